# Optimizing a Trainium2 kernel written in Bass

```python
import jax, jax.numpy as jnp
from jax import lax
import numpy as np

D_MODEL = 1024
BATCH = 32
SEQ = 2048
DEPTH = 1
DEC_BATCH = 8
DEC_SEQ = 4096
PAST_LEN = 128

FOURIER_WIDTH = D_MODEL // 2
FOURIER_GROUPS = 4
FOURIER_GROUP_DIM = FOURIER_WIDTH // FOURIER_GROUPS
HGRN_WIDTH = D_MODEL - FOURIER_WIDTH
HGRN_HEAD_DIM = 128
HGRN_HEADS = HGRN_WIDTH // HGRN_HEAD_DIM
CHUNK = 64
IN_WIDTH = FOURIER_WIDTH + 5 * HGRN_WIDTH
N_GROUPS = 4
EXPERTS_PER_GROUP = 8
N_EXPERTS = N_GROUPS * EXPERTS_PER_GROUP
TOP_K = 2
D_EXPERT = D_MODEL // 2
PLE_DIM = 256
EPS = 1e-6

kernel_name = "fnet_hgrn2_hmoe_parallel_encoder"


def _rmsnorm(x, g):
    xf = x.astype(jnp.float32)
    y = xf * lax.rsqrt(jnp.mean(xf * xf, axis=-1, keepdims=True) + EPS) * g.astype(jnp.float32)
    return y.astype(x.dtype)


def _fourier_mix(u, w):
    B, S, _ = u.shape
    u4 = u.astype(jnp.float32).reshape(B, S, FOURIER_GROUPS, FOURIER_GROUP_DIM)
    y = jnp.fft.fft2(u4, axes=(1, 3), norm="ortho").real
    y = jnp.einsum('bsgc,gcd->bsgd', y, w.astype(jnp.float32))
    return y.reshape(B, S, FOURIER_WIDTH).astype(u.dtype)


def _hgrn2_direction(q, k, v, log_f):
    B, S, H, D = q.shape
    N = S // CHUNK

    def to_chunks(t):
        return t.astype(jnp.float32).reshape(B, N, CHUNK, H, D).transpose(0, 3, 1, 2, 4)

    q, k, v, log_f = (to_chunks(t) for t in (q, k, v, log_f))
    b = jnp.cumsum(log_f, axis=3)
    b_last = b[:, :, :, -1:, :]
    q_dec = q * jnp.exp(b) * (D ** -0.5)
    scores = jnp.einsum('bhncd,bhnsd->bhncs', q_dec, k * jnp.exp(-b))
    lower = jnp.tril(jnp.ones((CHUNK, CHUNK), dtype=bool))
    scores = jnp.where(lower, scores, 0.0)
    o_intra = jnp.einsum('bhncs,bhnse->bhnce', scores, v)
    kv = jnp.einsum('bhncd,bhnce->bhnde', k * jnp.exp(b_last - b), v)
    decay = jnp.exp(b_last[:, :, :, 0, :])

    def step(state, xs):
        q_n, dec_n, kv_n = xs
        o_n = jnp.einsum('bhcd,bhde->bhce', q_n, state)
        return dec_n[..., None] * state + kv_n, o_n

    state0 = jnp.zeros((B, H, D, D), jnp.float32)
    _, o_inter = lax.scan(step, state0,
                          (jnp.moveaxis(q_dec, 2, 0), jnp.moveaxis(decay, 2, 0), jnp.moveaxis(kv, 2, 0)))
    o = o_intra + jnp.moveaxis(o_inter, 0, 2)
    return o.transpose(0, 2, 3, 1, 4).reshape(B, S, H, D)


def _gate_terms(f_raw, lb):
    fr = f_raw.astype(jnp.float32)
    f = lb + (1.0 - lb) * jax.nn.sigmoid(fr)
    k = (1.0 - lb) * jax.nn.sigmoid(-fr)
    return k, jnp.log(f)


def _hgrn2_bidirectional(q, v, f_raw_fwd, f_raw_bwd, lb_fwd, lb_bwd):
    B, S, _ = q.shape
    shp = (B, S, HGRN_HEADS, HGRN_HEAD_DIM)
    q = q.reshape(shp)
    v = v.reshape(shp)
    k_f, lf_f = _gate_terms(f_raw_fwd.reshape(shp), lb_fwd.reshape(HGRN_HEADS, HGRN_HEAD_DIM))
    k_b, lf_b = _gate_terms(f_raw_bwd.reshape(shp), lb_bwd.reshape(HGRN_HEADS, HGRN_HEAD_DIM))
    o_fwd = _hgrn2_direction(q, k_f, v, lf_f)
    o_bwd = _hgrn2_direction(q[:, ::-1], k_b[:, ::-1], v[:, ::-1], lf_b[:, ::-1])[:, ::-1]
    return o_fwd + o_bwd


def _hier_moe(h, w_rg, w_re, w_eg, w_eu, w_ed):
    B, S, D = h.shape
    t = h.reshape(-1, D)
    T = t.shape[0]
    p_group = jax.nn.softmax((t @ w_rg).astype(jnp.float32), axis=-1)
    p_top, g_idx = lax.top_k(p_group, 1)
    le = (t @ w_re).astype(jnp.float32).reshape(T, N_GROUPS, EXPERTS_PER_GROUP)
    le_sel = jnp.take_along_axis(le, g_idx[:, :, None], axis=1)[:, 0]
    v2, i2 = lax.top_k(le_sel, TOP_K)
    w2 = jax.nn.softmax(v2, axis=-1) * p_top
    ids = g_idx * EXPERTS_PER_GROUP + i2
    gates = jnp.sum(jax.nn.one_hot(ids, N_EXPERTS, dtype=jnp.float32) * w2[..., None], axis=1)
    y = jnp.zeros((T, D), jnp.float32)
    for e in range(N_EXPERTS):
        hid = jax.nn.silu(t @ w_eg[e]) * (t @ w_eu[e])
        y = y + gates[:, e:e + 1] * (hid @ w_ed[e]).astype(jnp.float32)
    return y.astype(h.dtype).reshape(B, S, D)


def _trunk(x, p, norm_mix, w_in, w_fourier, lb_logits, norm_o, w_out, norm_ffn,
           w_route_group, w_route_expert, w_exp_gate, w_exp_up, w_exp_down,
           norm_ple, w_ple_gate, w_ple_proj, norm_final):
    lb_all = jnp.cumsum(jax.nn.softmax(lb_logits.astype(jnp.float32), axis=0), axis=0)
    splits = [FOURIER_WIDTH + j * HGRN_WIDTH for j in range(5)]
    for i in range(DEPTH):
        B, S, _ = x.shape
        h = _rmsnorm(x, norm_mix[i])
        z = h @ w_in[i]
        u, q, v, fr_f, fr_b, og = jnp.split(z, splits, axis=-1)
        y_four = _fourier_mix(u, w_fourier[i])
        o = _hgrn2_bidirectional(q, v, fr_f, fr_b, lb_all[i, 0], lb_all[i, 1])
        o = _rmsnorm(o, norm_o[i]) * jax.nn.silu(og.astype(jnp.float32).reshape(B, S, HGRN_HEADS, HGRN_HEAD_DIM))
        o = o.reshape(B, S, HGRN_WIDTH).astype(x.dtype)
        x = x + jnp.concatenate([y_four, o], axis=-1) @ w_out[i]
        x = x + _hier_moe(_rmsnorm(x, norm_ffn[i]), w_route_group[i], w_route_expert[i],
                          w_exp_gate[i], w_exp_up[i], w_exp_down[i])
        gate = jax.nn.sigmoid(_rmsnorm(x, norm_ple[i]) @ w_ple_gate[i])
        x = x + (p[i] @ w_ple_proj[i]) * gate
    return _rmsnorm(x, norm_final)


def setup_inputs(seed: int = 0) -> dict:
    key = jax.random.key(seed)
    ks = jax.random.split(key, 20)
    f32 = jnp.float32

    def nrm(k, shape, fan_in):
        return jax.random.normal(k, shape, f32) * (fan_in ** -0.5)

    def gain(k, shape):
        return 1.0 + 0.05 * jax.random.normal(k, shape, f32)

    return {
        "x_prompt": jax.random.normal(ks[0], (BATCH, SEQ, D_MODEL), f32),
        "x_sample": jax.random.normal(ks[1], (DEC_BATCH, DEC_SEQ, D_MODEL), f32),
        "p_prompt": jax.random.normal(ks[2], (DEPTH, BATCH, SEQ, PLE_DIM), f32),
        "p_sample": jax.random.normal(ks[3], (DEPTH, DEC_BATCH, DEC_SEQ, PLE_DIM), f32),
        "norm_mix": gain(ks[4], (DEPTH, D_MODEL)),
        "w_in": nrm(ks[5], (DEPTH, D_MODEL, IN_WIDTH), D_MODEL),
        "w_fourier": nrm(ks[6], (DEPTH, FOURIER_GROUPS, FOURIER_GROUP_DIM, FOURIER_GROUP_DIM), FOURIER_GROUP_DIM),
        "lb_logits": 0.1 * jax.random.normal(ks[7], (DEPTH + 1, 2, HGRN_WIDTH), f32),
        "norm_o": gain(ks[8], (DEPTH, HGRN_HEAD_DIM)),
        "w_out": nrm(ks[9], (DEPTH, D_MODEL, D_MODEL), D_MODEL),
        "norm_ffn": gain(ks[10], (DEPTH, D_MODEL)),
        "w_route_group": nrm(ks[11], (DEPTH, D_MODEL, N_GROUPS), D_MODEL),
        "w_route_expert": nrm(ks[12], (DEPTH, D_MODEL, N_EXPERTS), D_MODEL),
        "w_exp_gate": nrm(ks[13], (DEPTH, N_EXPERTS, D_MODEL, D_EXPERT), D_MODEL),
        "w_exp_up": nrm(ks[14], (DEPTH, N_EXPERTS, D_MODEL, D_EXPERT), D_MODEL),
        "w_exp_down": nrm(ks[15], (DEPTH, N_EXPERTS, D_EXPERT, D_MODEL), D_EXPERT),
        "norm_ple": gain(ks[16], (DEPTH, D_MODEL)),
        "w_ple_gate": nrm(ks[17], (DEPTH, D_MODEL, D_MODEL), D_MODEL),
        "w_ple_proj": nrm(ks[18], (DEPTH, PLE_DIM, D_MODEL), PLE_DIM),
        "norm_final": gain(ks[19], (D_MODEL,)),
    }


def reference(x_prompt, x_sample, p_prompt, p_sample, norm_mix, w_in, w_fourier, lb_logits, norm_o,
              w_out, norm_ffn, w_route_group, w_route_expert, w_exp_gate, w_exp_up, w_exp_down,
              norm_ple, w_ple_gate, w_ple_proj, norm_final):
    y_prompt = _trunk(x_prompt, p_prompt, norm_mix, w_in, w_fourier, lb_logits, norm_o, w_out, norm_ffn,
                      w_route_group, w_route_expert, w_exp_gate, w_exp_up, w_exp_down,
                      norm_ple, w_ple_gate, w_ple_proj, norm_final)
    y_sample = _trunk(x_sample, p_sample, norm_mix, w_in, w_fourier, lb_logits, norm_o, w_out, norm_ffn,
                      w_route_group, w_route_expert, w_exp_gate, w_exp_up, w_exp_down,
                      norm_ple, w_ple_gate, w_ple_proj, norm_final)
    return (y_prompt, y_sample)
```

```python
import numpy as np
import ml_dtypes
from contextlib import ExitStack
import concourse.bass as bass
import concourse.mybir as mybir
from concourse.bass_utils import run_bass_kernel_spmd

F32 = mybir.dt.float32
BF16 = mybir.dt.bfloat16
I32 = mybir.dt.int32
AF = mybir.ActivationFunctionType
ALU = mybir.AluOpType
AX = mybir.AxisListType

D = 1024
NH = 4
HD = 128
NE = 32
DE = 512
PD = 256
INW = 3072
EPS = 1e-6
NTAB = 4096
BIG = 1.0e30


class Syn:
    EPOCH = 30000

    def __init__(self, nc, es, n_dma_sems=32):
        self.nc = nc
        self.es = es
        self.eng = {"pe": nc.tensor, "dve": nc.vector, "act": nc.scalar, "pool": nc.gpsimd, "sp": nc.sync}
        self.cnt = {e: 0 for e in self.eng}
        self.sems = {e: [] for e in self.eng}
        self.know = {e: {} for e in self.eng}
        self.snap = {}
        self.last_w = {}
        self.readers = {}
        self.dma_sems = [es.enter_context(nc.semaphore(f"dq{i}")) for i in range(n_dma_sems)]
        self.dma_val = [0] * n_dma_sems
        self.dma_last_ev = [None] * n_dma_sems
        self.dma_rr = 0
        self.n_wait = 0

    def _sem_for(self, e, count):
        ep = (count - 1) // self.EPOCH
        while len(self.sems[e]) <= ep:
            self.sems[e].append(self.es.enter_context(self.nc.semaphore(f"c_{e}_{len(self.sems[e])}")))
        return self.sems[e][ep], count - ep * self.EPOCH

    def _known(self, e, ev):
        return self.know[e].get((ev[0], ev[1]), 0) >= ev[2]

    def _learn(self, e, ev):
        k = self.know[e]
        key = (ev[0], ev[1])
        if k.get(key, 0) < ev[2]:
            k[key] = ev[2]
        sn = self.snap.get(ev)
        if sn:
            for kk, vv in sn.items():
                if k.get(kk, 0) < vv:
                    k[kk] = vv

    def _wait(self, e, ev):
        if ev is None or self._known(e, ev):
            return
        if ev[0] == "e":
            sem, val = self._sem_for(ev[1], ev[2])
            self.eng[e].wait_ge(sem, val)
        else:
            self.eng[e].wait_ge(self.dma_sems[ev[1]], ev[2])
        self.n_wait += 1
        self._learn(e, ev)

    PSUM_NAMES = {"pSx", "pSy", "pj", "ptrA", "pkA", "pfB", "pscC", "poC", "pkvC", "ptrC", "pA", "pB", "pC", "pD"}

    def _is_ps(self, r):
        return (r[0] if isinstance(r, tuple) else r) in self.PSUM_NAMES

    def _deps(self, e, reads, writes):
        evs = []
        for r in reads:
            ev = self.last_w.get(r)
            ps = self._is_ps(r)
            if ev is not None and not (ps and ev[1] == e):
                evs.append(ev)
            if ps:
                evs.extend(x for x in self.readers.get(r, ()) if x[1] != e)
        for w in writes:
            ps = self._is_ps(w)
            ev = self.last_w.get(w)
            if ev is not None and not (ps and ev[1] == e):
                evs.append(ev)
            evs.extend(x for x in self.readers.get(w, ()) if not (ps and x[1] == e))
        for ev in evs:
            if ev[0] == "e" and ev[1] == "pe" and e == "pe":
                continue
            self._wait(e, ev)

    def _record(self, ev, reads, writes):
        for r in reads:
            self.readers.setdefault(r, []).append(ev)
        for w in writes:
            self.last_w[w] = ev
            self.readers[w] = []

    def op(self, e, fn, reads=(), writes=()):
        self._deps(e, reads, writes)
        inst = fn()
        self.cnt[e] += 1
        c = self.cnt[e]
        sem, _ = self._sem_for(e, c)
        inst.then_inc(sem, 1)
        ev = ("e", e, c)
        self.snap[ev] = dict(self.know[e])
        self._record(ev, reads, writes)
        return ev

    def dma(self, q, out, in_, reads=(), writes=(), **kw):
        self._deps(q, reads, writes)
        i = self.dma_rr
        self.dma_rr = (self.dma_rr + 1) % len(self.dma_sems)
        self._wait(q, self.dma_last_ev[i])
        inst = self.eng[q].dma_start(out=out, in_=in_, **kw)
        self.dma_val[i] += 16
        inst.then_inc(self.dma_sems[i], 16)
        ev = ("d", i, self.dma_val[i])
        self.snap[ev] = dict(self.know[q])
        self.dma_last_ev[i] = ev
        self._record(ev, reads, writes)
        return ev

    def idma(self, q, fn, reads=(), writes=()):
        self._deps(q, reads, writes)
        i = self.dma_rr
        self.dma_rr = (self.dma_rr + 1) % len(self.dma_sems)
        self._wait(q, self.dma_last_ev[i])
        inst = fn()
        self.dma_val[i] += 16
        inst.then_inc(self.dma_sems[i], 16)
        ev = ("d", i, self.dma_val[i])
        self.snap[ev] = dict(self.know[q])
        self.dma_last_ev[i] = ev
        self._record(ev, reads, writes)
        return ev

    def barrier(self):
        for e in self.eng:
            for ev in self.dma_last_ev:
                self._wait(e, ev)
            for f in self.eng:
                if f != e and self.cnt[f] > 0:
                    self._wait(e, ("e", f, self.cnt[f]))
        self.last_w.clear()
        self.readers.clear()

    def finish(self, q="sp"):
        for ev in self.dma_last_ev:
            self._wait(q, ev)
        for e in self.eng:
            if self.cnt[e] > 0 and e != q:
                self._wait(q, ("e", e, self.cnt[e]))


SLOT = 512


def build(seq_lens, debug=False, phases="ABCE"):
    NTOK = sum(seq_lens)
    NSLOT = (2 * NTOK) // SLOT + NE
    NPOS = NSLOT * SLOT
    NTILE_ = NTOK // 128
    assert NTOK % 1024 == 0 and all(s % 512 == 0 for s in seq_lens)
    offs = [sum(seq_lens[:i]) for i in range(len(seq_lens))]
    nc = bass.Bass("TRN2", target_bir_lowering=False)

    def din(name, shape, dt=F32):
        return nc.dram_tensor(name, shape, dt, kind="ExternalInput").ap()

    x = din("x", [NTOK, D])
    p_in = din("p", [NTOK, PD])
    norm_mix = din("norm_mix", [D])
    w_in = din("w_in", [D, INW])
    w_four = din("w_fourier", [4, 128, 128])
    lb_logits = din("lb_logits", [2, 2 * 512])
    norm_o = din("norm_o", [HD])
    w_out = din("w_out", [D, D])
    norm_ffn = din("norm_ffn", [D])
    w_rg = din("w_route_group", [D, 4])
    w_re = din("w_route_expert", [D, NE])
    w_eg = din("w_exp_gate", [NE, D, DE])
    w_eu = din("w_exp_up", [NE, D, DE])
    w_ed = din("w_exp_down", [NE, DE, D])
    norm_ple = din("norm_ple", [D])
    w_pg = din("w_ple_gate", [D, D])
    w_pp = din("w_ple_proj", [PD, D])
    norm_final = din("norm_final", [D])
    c_ident = din("c_ident", [128, 128], BF16)
    c_maskf = din("c_maskf", [128, 128], BF16)
    c_maskb = din("c_maskb", [128, 128], BF16)
    c_cc = din("c_cc", [128, 256], BF16)
    c_ctab = din("c_ctab", [NTAB, NTAB], BF16)
    c_stab = din("c_stab", [NTAB, NTAB], BF16)
    c_recpad = din("c_recpad", [NPOS, 4], I32)
    c_L = din("c_L", [128, 128], BF16)
    c_ones = din("c_ones", [128, 128], BF16)
    c_mask96 = din("c_mask96", [128, NE * NTILE_], F32)
    c_tokid = din("c_tokid", [128, NTILE_], F32)
    c_slotstart = din("c_slotstart", [128, NSLOT], F32)
    c_thr = din("c_thr", [128, NTOK // SLOT], F32)
    c_addc = din("c_addc", [128, 6], F32)
    c_mulc = din("c_mulc", [128, 6], F32)

    y = nc.dram_tensor("y", [NTOK, D], F32, kind="ExternalOutput").ap()

    skind = "ExternalOutput" if debug else "Internal"

    def dscr(name, shape, dt=BF16):
        return nc.dram_tensor(name, shape, dt, kind=skind).ap()

    ab_s = dscr("ab_s", [NTOK, 1024])
    qd_s = dscr("qd_s", [2, NH, 128, NTOK])
    kd_s = dscr("kd_s", [2, NH, 128, NTOK])
    k2_s = dscr("k2_s", [NTOK, 1024])
    v_s = dscr("v_s", [NTOK, 512])
    og_s = dscr("og_s", [NTOK, 512])
    mixT_s = dscr("mixT_s", [D, NTOK])
    x1_s = dscr("x1_s", [NTOK, D], F32)
    hn_s = dscr("hn_s", [NTOK, D])
    rec_s = dscr("rec_s", [NPOS, 4], I32)
    ybuf = dscr("ybuf", [2 * NTOK + SLOT, D])
    wbg_s = dscr("wbg_s", [NE * 128, 8 * DE])
    wbu_s = dscr("wbu_s", [NE * 128, 8 * DE])
    wbd_s = dscr("wbd_s", [NE * 128, 4 * D])
    dbg_ofwd = dscr("dbg_ofwd", [NTOK, 512]) if debug else None
    dbg_scm = dscr("dbg_scm", [128, 512]) if debug else None
    dbg_kv = nc.dram_tensor("dbg_kv", [128, 512], F32, kind="ExternalOutput").ap() if debug else None

    NCH = NTOK // 64

    with ExitStack() as es:
        def sbg(name, shape, dt):
            return es.enter_context(nc.sbuf_tensor(name, shape, dt))

        es.enter_context(nc.Block())
        S = Syn(nc, es)
        V, A, P, T = nc.vector, nc.scalar, nc.gpsimd, nc.tensor

        ident = sbg("ident", [128, 128], BF16)
        maskf = sbg("maskf", [128, 128], BF16)
        maskb = sbg("maskb", [128, 128], BF16)
        gmix = sbg("gmix", [128, 8], F32)
        gffn = sbg("gffn", [128, 8], F32)
        gple = sbg("gple", [128, 8], F32)
        gfin = sbg("gfin", [128, D], F32)
        normo = sbg("normo", [128, 1], F32)
        lbt = sbg("lbt", [128, 8], F32)
        clb = sbg("clb", [128, 8], F32)
        lbtmp = sbg("lbtmp", [128, 16], F32)
        decS = sbg("decS", [128, 2, NH, NCH], F32)
        S.dma("sp", ident[:], c_ident, writes=["ident"])
        S.dma("sp", maskf[:], c_maskf, writes=["maskf"])
        S.dma("sp", maskb[:], c_maskb, writes=["maskb"])
        for (t_, src, nm) in ((gmix, norm_mix, "gmix"), (gffn, norm_ffn, "gffn"), (gple, norm_ple, "gple")):
            S.dma("sp", t_[:], src.rearrange("(k p) -> p k", p=128), writes=[nm], allow_slow_non_contiguous=True)
        S.dma("sp", gfin[:], norm_final.partition_broadcast(128), writes=["gfin"])
        S.dma("sp", normo[:], norm_o.rearrange("(p o) -> p o", o=1), writes=["normo"])
        S.dma("sp", lbtmp[:, 0:8], lb_logits[0].rearrange("(a p) -> p a", p=128), writes=["lbtmp"], allow_slow_non_contiguous=True)
        S.dma("sp", lbtmp[:, 8:16], lb_logits[1].rearrange("(a p) -> p a", p=128), writes=["lbtmp"], allow_slow_non_contiguous=True)
        S.op("dve", lambda: V.tensor_sub(out=lbt[:], in0=lbtmp[:, 8:16], in1=lbtmp[:, 0:8]), reads=["lbtmp"], writes=["lbt"])
        S.op("act", lambda: A.activation(out=lbt[:], in_=lbt[:], func=AF.Exp), reads=["lbt"], writes=["lbt"])
        S.op("dve", lambda: V.tensor_scalar_add(out=lbt[:], in0=lbt[:], scalar1=1.0), reads=["lbt"], writes=["lbt"])
        S.op("dve", lambda: V.reciprocal(out=lbt[:], in_=lbt[:]), reads=["lbt"], writes=["lbt"])
        S.op("act", lambda: A.activation(out=clb[:], in_=lbt[:], func=AF.Ln, scale=-1.0, bias=1.0), reads=["lbt"], writes=["clb"])

        def rstd_from_ssq(ssq_ap, out_ap, n, res_r, res_w):
            S.op("act", lambda: A.activation(out=out_ap, in_=ssq_ap, func=AF.Ln, scale=1.0 / n, bias=EPS), reads=res_r, writes=res_w)
            S.op("act", lambda: A.activation(out=out_ap, in_=out_ap, func=AF.Exp, scale=-0.5), reads=res_w, writes=res_w)

        if "A" in phases:
            with ExitStack() as ph:
                def sb(name, shape, dt):
                    return ph.enter_context(nc.sbuf_tensor(name, shape, dt))

                def ps(name, shape, dt=F32):
                    return ph.enter_context(nc.psum_tensor(name, shape, dt))

                winb = sb("winb", [128, 8, INW], BF16)
                mcs = sb("mcs", [128, 4, 256], BF16)
                wfb = sb("wfb", [128, 4, 128], BF16)
                ccb = sb("ccb", [128, 256], BF16)
                scanmask = sb("scanmask", [128, 512], F32)
                xg = [sb(f"xg{i}", [128, 4, D], F32) for i in range(2)]
                xs = [sb(f"xs{i}", [128, 4, D], BF16) for i in range(2)]
                junk = sb("junkA", [128, D], BF16)
                ssq = sb("ssqA", [128, 8], F32)
                rstd = sb("rstdA", [128, 8], F32)
                hT = [sb(f"hT{i}", [128, 8, 512], BF16) for i in range(2)]
                uT = sb("uT", [128, 4, 512], BF16)
                abst = sb("abst", [128, 4, 1024], BF16)
                vst = sb("vst", [128, 4, 512], BF16)
                ogst = sb("ogst", [128, 4, 512], BF16)
                k2T = sb("k2T", [128, 8, 512], BF16)
                k2st = sb("k2st", [128, 4, 1024], BF16)
                qdst = [sb(f"qdst{i}", [128, 512], BF16) for i in range(4)]
                kdst = [sb(f"kdst{i}", [128, 512], BF16) for i in range(4)]
                NTMP = 2
                tm = [{n: sb(f"t_{n}{i}", [128, 512], F32) for n in ("e", "L1", "L2", "P", "lk", "a1", "a2", "Eb")} for i in range(NTMP)]
                ptr = [ps(f"ptrA{i}", [128, 1024], BF16) for i in range(2)]
                pj = [ps(f"pjA{i}", [128, 512], F32) for i in range(5)]
                pk = ps("pkA", [128, 1024], BF16)

                for c in range(4):
                    S.dma("pool", winb[:, :, c * 768:(c + 1) * 768],
                          w_in[:, c * 768:(c + 1) * 768].rearrange("(k p) n -> p k n", p=128), writes=[("winb", c)])
                winb_res = [("winb", c) for c in range(4)]
                S.dma("pool", wfb[:], w_four.rearrange("g c d -> c g d"), writes=["wfb"])
                S.dma("sp", ccb[:], c_cc, writes=["ccb"])
                for g in range(4):
                    for hh in range(2):
                        S.op("pe", lambda g=g, hh=hh: T.matmul(pj[hh][:, g * 128:(g + 1) * 128],
                                                                ccb[:, hh * 128:(hh + 1) * 128], wfb[:, g, :], start=True, stop=True),
                             reads=["ccb", "wfb"], writes=[("pj", hh)])
                for hh in range(2):
                    S.op("dve", lambda hh=hh: V.tensor_copy(out=mcs[:, :, hh * 128:(hh + 1) * 128],
                                                            in_=pj[hh][:, :].rearrange("p (g d) -> p g d", g=4)),
                         reads=[("pj", hh)], writes=["mcs"])
                S.op("dve", lambda: V.memset(scanmask[:], 1.0), writes=["scanmask"])
                S.op("dve", lambda: V.memset(scanmask[:].rearrange("p (n c) -> p n c", c=64)[:, :, 0:1], 0.0), reads=["scanmask"], writes=["scanmask"])

                if "E" in phases:
                    for e in range(NE):
                        S.dma("pool", wbg_s[e * 128:(e + 1) * 128, :], w_eg[e].rearrange("(p k) n -> p (k n)", p=128), writes=[("wbg_s", e)])
                        S.dma("pool", wbu_s[e * 128:(e + 1) * 128, :], w_eu[e].rearrange("(p k) n -> p (k n)", p=128), writes=[("wbu_s", e)])
                        S.dma("pool", wbd_s[e * 128:(e + 1) * 128, :].rearrange("p (k n) -> p k n", k=4), w_ed[e].rearrange("(k p) n -> p k n", p=128),
                              writes=[("wbd_s", e)])
                pj_rr = [0]

                def next_pj():
                    i = pj_rr[0]
                    pj_rr[0] = (i + 1) % 5
                    return i

                NG = NTOK // 512
                ev_i = 0

                def a_load(G_):
                    S.dma("sp", xg[G_ % 2][:], x[G_ * 512:(G_ + 1) * 512, :].rearrange("(j p) d -> p j d", p=128), writes=[("xg", G_ % 2)])

                def a_front(G_):
                    b_ = G_ % 2
                    for j in range(4):
                        S.op("act", lambda j=j: A.activation(out=junk[:], in_=xg[b_][:, j, :], func=AF.Square, accum_out=ssq[:, b_ * 4 + j:b_ * 4 + j + 1]),
                             reads=[("xg", b_)], writes=["junkA", ("ssqA", b_)])
                    rstd_from_ssq(ssq[:, b_ * 4:(b_ + 1) * 4], rstd[:, b_ * 4:(b_ + 1) * 4], D, [("ssqA", b_)], [("rstdA", b_)])
                    for j in range(4):
                        if j % 2 == 0:
                            S.op("act", lambda j=j: A.activation(out=xs[b_][:, j, :], in_=xg[b_][:, j, :], func=AF.Copy, scale=rstd[:, b_ * 4 + j:b_ * 4 + j + 1]),
                                 reads=[("xg", b_), ("rstdA", b_)], writes=[("xs", b_, j)])
                        else:
                            S.op("dve", lambda j=j: V.tensor_scalar(out=xs[b_][:, j, :], in0=xg[b_][:, j, :], scalar1=rstd[:, b_ * 4 + j:b_ * 4 + j + 1], scalar2=None, op0=ALU.mult),
                                 reads=[("xg", b_), ("rstdA", b_)], writes=[("xs", b_, j)])
                    for k in range(8):
                        pt = ptr[(k // 2) % 2]
                        half = k % 2
                        for j in range(4):
                            S.op("pe", lambda j=j, k=k, pt=pt, half=half: T.transpose(pt[:, half * 512 + j * 128: half * 512 + (j + 1) * 128],
                                                                                      xs[b_][:, j, k * 128:(k + 1) * 128], ident[:]),
                                 reads=[("xs", b_, j), "ident"], writes=[("ptrA", (k // 2) % 2)])
                        if k % 2 == 0:
                            S.op("dve", lambda k=k, pt=pt, half=half: V.tensor_scalar(out=hT[b_][:, k, :], in0=pt[:, half * 512:(half + 1) * 512],
                                                                                      scalar1=gmix[:, k:k + 1], scalar2=None, op0=ALU.mult),
                                 reads=[("ptrA", (k // 2) % 2), "gmix"], writes=[("hT", b_, k)])
                        else:
                            S.op("act", lambda k=k, pt=pt, half=half: A.activation(out=hT[b_][:, k, :], in_=pt[:, half * 512:(half + 1) * 512],
                                                                                   func=AF.Copy, scale=gmix[:, k:k + 1]),
                                 reads=[("ptrA", (k // 2) % 2), "gmix"], writes=[("hT", b_, k)])

                a_load(0)
                if NG > 1:
                    a_load(1)
                a_front(0)
                for G in range(NG):
                    t0 = G * 512
                    b = G % 2
                    if G + 1 < NG:
                        a_front(G + 1)
                    if G + 2 < NG:
                        a_load(G + 2)
                    hT_res = [("hT", b, k) for k in range(8)]

                    def proj_fm(col0, pi):
                        for k in range(8):
                            S.op("pe", lambda k=k: T.matmul(pj[pi][:, :], winb[:, k, col0:col0 + 128], hT[b][:, k, :], start=(k == 0), stop=(k == 7)),
                                 reads=winb_res + hT_res, writes=[("pj", pi)])

                    for g in range(4):
                        pi = next_pj()
                        proj_fm(g * 128, pi)
                        eng = "act" if g % 2 else "dve"
                        if eng == "act":
                            S.op("act", lambda g=g, pi=pi: A.copy(out=uT[:, g, :], in_=pj[pi][:, :]), reads=[("pj", pi)], writes=[("uT", g)])
                        else:
                            S.op("dve", lambda g=g, pi=pi: V.tensor_copy(out=uT[:, g, :], in_=pj[pi][:, :]), reads=[("pj", pi)], writes=[("uT", g)])
                    for j in range(4):
                        for half in range(2):
                            pi = next_pj()
                            for gg in range(2):
                                g = half * 2 + gg
                                S.op("pe", lambda g=g, gg=gg, j=j, pi=pi: T.matmul(pj[pi][:, gg * 256:(gg + 1) * 256], uT[:, g, j * 128:(j + 1) * 128],
                                                                                mcs[:, g, :], start=True, stop=True),
                                     reads=[("uT", g), "mcs"], writes=[("pj", pi)])
                            if half == 0:
                                S.op("act", lambda j=j, half=half, pi=pi: A.copy(out=abst[:, j, half * 512:(half + 1) * 512], in_=pj[pi][:, :]),
                                     reads=[("pj", pi)], writes=[("abst", j, half)])
                            else:
                                S.op("dve", lambda j=j, half=half, pi=pi: V.tensor_copy(out=abst[:, j, half * 512:(half + 1) * 512], in_=pj[pi][:, :]),
                                     reads=[("pj", pi)], writes=[("abst", j, half)])
                    S.dma("sp", ab_s[t0:t0 + 512, :].rearrange("(j p) n -> p j n", p=128), abst[:],
                          reads=[("abst", j, hf) for j in range(4) for hf in range(2)], writes=[("ab_s", G)])

                    for (col0, st, nm) in ((1024, vst, "vst"), (2560, ogst, "ogst")):
                        for j in range(4):
                            pi = next_pj()
                            for k in range(8):
                                S.op("pe", lambda k=k, j=j, pi=pi, col0=col0: T.matmul(pj[pi][:, :], hT[b][:, k, j * 128:(j + 1) * 128],
                                                                                      winb[:, k, col0:col0 + 512], start=(k == 0), stop=(k == 7)),
                                     reads=winb_res + hT_res, writes=[("pj", pi)])
                            if j % 2 == 0:
                                S.op("act", lambda j=j, pi=pi, st=st: A.copy(out=st[:, j, :], in_=pj[pi][:, :]), reads=[("pj", pi)], writes=[(nm, j)])
                            else:
                                S.op("dve", lambda j=j, pi=pi, st=st: V.tensor_copy(out=st[:, j, :], in_=pj[pi][:, :]), reads=[("pj", pi)], writes=[(nm, j)])
                    S.dma("sp", v_s[t0:t0 + 512, :].rearrange("(j p) n -> p j n", p=128), vst[:],
                          reads=[("vst", j) for j in range(4)], writes=[("v_s", G)])
                    S.dma("sp", og_s[t0:t0 + 512, :].rearrange("(j p) n -> p j n", p=128), ogst[:],
                          reads=[("ogst", j) for j in range(4)], writes=[("og_s", G)])

                    for h in range(NH):
                        pq = next_pj()
                        proj_fm(512 + h * 128, pq)
                        pfr = [None, None]
                        pfr[0] = next_pj()
                        proj_fm(1536 + h * 128, pfr[0])
                        pfr[1] = next_pj()
                        proj_fm(2048 + h * 128, pfr[1])
                        def chain(dr):
                            tt = tm[dr]
                            ti = dr
                            R = lambda n: ("tm", ti, n)
                            fr = pj[pfr[dr]]
                            frr = ("pj", pfr[dr])
                            col = dr * 4 + h
                            qs = qdst[(2 * h + dr) % 4]
                            ks = kdst[(2 * h + dr) % 4]
                            qsr = ("qdst", (2 * h + dr) % 4)
                            ksr = ("kdst", (2 * h + dr) % 4)
                            S.op("act", lambda: A.activation(out=tt["e"][:], in_=fr[:, :], func=AF.Exp, scale=-1.0), reads=[frr], writes=[R("e")])
                            yield
                            S.op("act", lambda: A.activation(out=tt["L1"][:], in_=tt["e"][:], func=AF.Ln, bias=1.0), reads=[R("e")], writes=[R("L1")])
                            yield
                            S.op("act", lambda: A.activation(out=tt["L2"][:], in_=tt["e"][:], func=AF.Ln, scale=lbt[:, col:col + 1], bias=1.0),
                                 reads=[R("e"), "lbt"], writes=[R("L2")])
                            yield
                            S.op("dve", lambda: V.tensor_sub(out=tt["L2"][:], in0=tt["L2"][:], in1=tt["L1"][:]), reads=[R("L2"), R("L1")], writes=[R("L2")])
                            yield
                            S.op("dve", lambda: V.tensor_tensor_scan(out=tt["P"][:], data0=scanmask[:], data1=tt["L2"][:], initial=0.0, op0=ALU.mult, op1=ALU.add),
                                 reads=[R("L2"), "scanmask"], writes=[R("P")])
                            yield
                            S.op("dve", lambda: V.scalar_tensor_tensor(out=tt["lk"][:], in0=fr[:, :], scalar=-1.0, in1=tt["L1"][:], op0=ALU.mult, op1=ALU.subtract),
                                 reads=[frr, R("L1")], writes=[R("lk")])
                            yield
                            P3 = tt["P"][:].rearrange("p (n c) -> p n c", c=64)
                            Tb = P3[:, :, 63:64].to_broadcast([128, 8, 64])
                            v3 = lambda n: tt[n][:].rearrange("p (n c) -> p n c", c=64)
                            ch0 = t0 // 64
                            if dr == 0:
                                S.op("dve", lambda: V.tensor_sub(out=tt["a1"][:], in0=tt["lk"][:], in1=tt["P"][:]), reads=[R("lk"), R("P")], writes=[R("a1")])
                                yield
                                S.op("dve", lambda: V.tensor_tensor(out=v3("a2"), in0=v3("a1"), in1=Tb, op=ALU.add), reads=[R("a1"), R("P")], writes=[R("a2")])
                                yield
                                S.op("act", lambda: A.activation(out=tt["Eb"][:], in_=tt["P"][:], func=AF.Exp), reads=[R("P")], writes=[R("Eb")])
                                yield
                            else:
                                S.op("dve", lambda: V.tensor_sub(out=tt["a2"][:], in0=tt["P"][:], in1=tt["L2"][:]), reads=[R("P"), R("L2")], writes=[R("a2")])
                                yield
                                S.op("dve", lambda: V.tensor_tensor(out=v3("Eb"), in0=Tb, in1=v3("a2"), op=ALU.subtract), reads=[R("P"), R("a2")], writes=[R("Eb")])
                                yield
                                S.op("dve", lambda: V.tensor_sub(out=tt["a1"][:], in0=tt["lk"][:], in1=tt["Eb"][:]), reads=[R("lk"), R("Eb")], writes=[R("a1")])
                                yield
                                S.op("dve", lambda: V.tensor_tensor(out=tt["a2"][:], in0=tt["a2"][:], in1=tt["lk"][:], op=ALU.add), reads=[R("a2"), R("lk")], writes=[R("a2")])
                                yield
                                S.op("act", lambda: A.activation(out=tt["Eb"][:], in_=tt["Eb"][:], func=AF.Exp), reads=[R("Eb")], writes=[R("Eb")])
                                yield
                            S.op("act", lambda: A.activation(out=ks[:], in_=tt["a1"][:], func=AF.Exp, bias=clb[:, col:col + 1]), reads=[R("a1"), "clb"], writes=[ksr])
                            yield
                            S.op("act", lambda: A.activation(out=k2T[:, col, :], in_=tt["a2"][:], func=AF.Exp, bias=clb[:, col:col + 1]),
                                 reads=[R("a2"), "clb"], writes=[("k2T", col)])
                            yield
                            S.op("dve", lambda: V.scalar_tensor_tensor(out=qs[:], in0=pj[pq][:, :], scalar=float(HD) ** -0.5, in1=tt["Eb"][:], op0=ALU.mult, op1=ALU.mult),
                                 reads=[("pj", pq), R("Eb")], writes=[qsr])
                            yield
                            S.op("act", lambda: A.activation(out=decS[:, dr, h, ch0:ch0 + 8], in_=P3[:, :, 63], func=AF.Exp), reads=[R("P")], writes=[("decS", G)])
                            yield
                            S.dma("sp", qd_s[dr, h, :, t0:t0 + 512], qs[:], reads=[qsr], writes=[("qd_s", G)])
                            S.dma("sp", kd_s[dr, h, :, t0:t0 + 512], ks[:], reads=[ksr], writes=[("kd_s", G)])

                        gens_ = [chain(0), chain(1)]
                        alive_ = [True, True]
                        while any(alive_):
                            for d_ in range(2):
                                if alive_[d_]:
                                    try:
                                        next(gens_[d_])
                                    except StopIteration:
                                        alive_[d_] = False
                    for j in range(4):
                        for col in range(8):
                            S.op("pe", lambda j=j, col=col: T.transpose(pk[:, col * 128:(col + 1) * 128], k2T[:, col, j * 128:(j + 1) * 128], ident[:]),
                                 reads=[("k2T", col), "ident"], writes=["pkA"])
                        if j % 2 == 0:
                            S.op("dve", lambda j=j: V.tensor_copy(out=k2st[:, j, :], in_=pk[:, :]), reads=["pkA"], writes=[("k2st", j)])
                        else:
                            S.op("act", lambda j=j: A.copy(out=k2st[:, j, :], in_=pk[:, :]), reads=["pkA"], writes=[("k2st", j)])
                    S.dma("sp", k2_s[t0:t0 + 512, :].rearrange("(j p) n -> p j n", p=128), k2st[:],
                          reads=[("k2st", j) for j in range(4)], writes=[("k2_s", G)])
                S.barrier()

        if "B" in phases:
            with ExitStack() as ph:
                def sb(name, shape, dt):
                    return ph.enter_context(nc.sbuf_tensor(name, shape, dt))

                def ps(name, shape, dt=F32):
                    return ph.enter_context(nc.psum_tensor(name, shape, dt))

                NTmax = max(seq_lens) // 128
                abS = sb("abS", [128, NTmax, 1024], BF16)
                ctile = [sb(f"ctile{i}", [128, 16, 512], BF16) for i in range(2)]
                stile = [sb(f"stile{i}", [128, 16, 512], BF16) for i in range(2)]
                yst = [sb(f"ystB{i}", [128, 4, 512], BF16) for i in range(2)]
                pf = [ps(f"pfB{i}", [128, 512], F32) for i in range(8)]
                ti = 0
                cti = 0
                for si, (off, SL) in enumerate(zip(offs, seq_lens)):
                    NT = SL // 128
                    rs = NTAB // SL
                    TC = min(16, NT)
                    nkc = NT // TC
                    for q4 in range(0, NT, 8):
                        n4 = min(8, NT - q4)
                        S.dma("sp", abS[:, q4:q4 + n4, :], ab_s[off + q4 * 128: off + (q4 + n4) * 128, :].rearrange("(j p) n -> p j n", p=128),
                              reads=[("ab_s", g) for g in range(NTOK // 512)] if "A" in phases else [], writes=[("abS", q4 // 8)])
                    abS_res = [("abS", i) for i in range((NT + 7) // 8)]
                    ctab_v = c_ctab.rearrange("(r m) c -> r m c", m=rs)
                    stab_v = c_stab.rearrange("(r m) c -> r m c", m=rs)
                    for ct in range(SL // 512):
                        pset = (cti % 2) * 4
                        for kc in range(nkc):
                            tb = ti % 2
                            ti += 1
                            r0 = kc * TC * 128
                            S.dma("sp", ctile[tb][:, 0:TC, :], ctab_v[r0:r0 + TC * 128, 0, ct * 512:(ct + 1) * 512].rearrange("(j p) n -> p j n", p=128),
                                  writes=[("ctile", tb)])
                            S.dma("pool", stile[tb][:, 0:TC, :], stab_v[r0:r0 + TC * 128, 0, ct * 512:(ct + 1) * 512].rearrange("(j p) n -> p j n", p=128),
                                  writes=[("stile", tb)])
                            for t in range(TC):
                                tt_ = kc * TC + t
                                for g in range(4):
                                    S.op("pe", lambda g=g, t=t, tt_=tt_, tb=tb: T.matmul(pf[pset + g][:, :], abS[:, tt_, g * 256:g * 256 + 128], ctile[tb][:, t, :],
                                                                                   start=(tt_ == 0), stop=False),
                                         reads=abS_res + [("ctile", tb)], writes=[("pfB", pset + g)])
                                    S.op("pe", lambda g=g, t=t, tt_=tt_, tb=tb: T.matmul(pf[pset + g][:, :], abS[:, tt_, g * 256 + 128:g * 256 + 256], stile[tb][:, t, :],
                                                                                   start=False, stop=(tt_ == NT - 1)),
                                         reads=abS_res + [("stile", tb)], writes=[("pfB", pset + g)])
                        yb = cti % 2
                        sc = float((SL * 128.0) ** -0.5)
                        for g in range(4):
                            if g % 2 == 0:
                                S.op("act", lambda g=g: A.activation(out=yst[yb][:, g, :], in_=pf[pset + g][:, :], func=AF.Copy, scale=sc),
                                     reads=[("pfB", pset + g)], writes=[("ystB", yb)])
                            else:
                                S.op("dve", lambda g=g: V.tensor_scalar(out=yst[yb][:, g, :], in0=pf[pset + g][:, :], scalar1=sc, scalar2=None, op0=ALU.mult),
                                     reads=[("pfB", pset + g)], writes=[("ystB", yb)])
                        S.dma("sp", mixT_s[0:512, off + ct * 512: off + (ct + 1) * 512].rearrange("(g p) t -> p g t", p=128), yst[yb][:],
                              reads=[("ystB", yb)], writes=[("mixT_s", "four", si, ct)])
                        cti += 1
                S.barrier()

        if "C" in phases:
            with ExitStack() as ph:
                def sb(name, shape, dt):
                    return ph.enter_context(nc.sbuf_tensor(name, shape, dt))

                def ps(name, shape, dt=F32):
                    return ph.enter_context(nc.psum_tensor(name, shape, dt))

                NTmax = max(seq_lens) // 128
                osto = sb("osto", [128, NTmax, 512], BF16)
                qdb = [[sb(f"qdb{d}_{i}", [128, NH, 512], BF16) for i in range(2)] for d in range(2)]
                kdb = [[sb(f"kdb{d}_{i}", [128, NH, 512], BF16) for i in range(2)] for d in range(2)]
                k2b = [[sb(f"k2b{d}_{i}", [128, 4, 512], BF16) for i in range(2)] for d in range(2)]
                vb = [[sb(f"vb{d}_{i}", [128, 4, 512], BF16) for i in range(2)] for d in range(2)]
                ogb = [[sb(f"ogb{d}_{i}", [128, 4, 512], BF16) for i in range(2)] for d in range(2)]
                S32 = [[[sb(f"S32_{d}_{h}_{i}", [128, 128], F32) for i in range(2)] for h in range(NH)] for d in range(2)]
                Sbf = [[sb(f"Sbf_{d}_{h}", [128, 128], BF16) for h in range(NH)] for d in range(2)]
                scm = [sb(f"scm{d}", [128, 512], BF16) for d in range(2)]
                o32 = [sb(f"o32_{d}", [128, 512], F32) for d in range(2)]
                sg = [sb(f"sgC{d}", [128, 512], F32) for d in range(2)]
                mb = [sb(f"mbC{d}", [128, 512], BF16) for d in range(2)]
                junk = sb("junkC", [128, 128], BF16)
                ssq4 = [sb(f"ssq4_{d}", [128, 4], F32) for d in range(2)]
                mst = [[sb(f"mstC{d}_{i}", [128, NH, 512], BF16) for i in range(2)] for d in range(2)]
                psc = [ps(f"pscC{d}", [128, 512], F32) for d in range(2)]
                po = [ps(f"poC{d}", [128, 512], F32) for d in range(2)]
                pkv = [ps(f"pkvC{d}", [128, 512], F32) for d in range(2)]
                ptr = [ps(f"ptrC{d}", [128, 1024], BF16) for d in range(2)]
                blk_cnt = [0, 0]

                def c_step(dr, off, SL, t, second, cur):
                    NT = SL // 128
                    bi, j = t // 4, t % 4
                    tok0 = off + bi * 512
                    G = tok0 // 512
                    first_in_blk = (j == 0) if dr == 0 else (j == 3)
                    last_in_blk = (j == 3) if dr == 0 else (j == 0)
                    if first_in_blk:
                        blk_cnt[dr] += 1
                        cur["bb"] = blk_cnt[dr] % 2
                        bb = cur["bb"]
                        S.dma("sp", qdb[dr][bb][:], qd_s[dr, :, :, tok0:tok0 + 512].rearrange("h d t -> d h t"), writes=[("qdb", dr, bb)])
                        S.dma("sp", kdb[dr][bb][:], kd_s[dr, :, :, tok0:tok0 + 512].rearrange("h d t -> d h t"), writes=[("kdb", dr, bb)])
                        S.dma("sp", k2b[dr][bb][:], k2_s[tok0:tok0 + 512, dr * 512:(dr + 1) * 512].rearrange("(j p) n -> p j n", p=128), writes=[("k2b", dr, bb)])
                        S.dma("sp", vb[dr][bb][:], v_s[tok0:tok0 + 512, :].rearrange("(j p) n -> p j n", p=128), writes=[("vb", dr, bb)])
                        if (bi * 4 + 3 >= NT // 2) if dr == 0 else (bi * 4 < NT // 2):
                            S.dma("sp", ogb[dr][bb][:], og_s[tok0:tok0 + 512, :].rearrange("(j p) n -> p j n", p=128), writes=[("ogb", dr, bb)])
                    bb = cur["bb"]
                    mask = maskf if dr == 0 else maskb
                    Q, Kd, K2, Vv, OG = qdb[dr][bb], kdb[dr][bb], k2b[dr][bb], vb[dr][bb], ogb[dr][bb]
                    rq, rk, rk2, rv, rog = ("qdb", dr, bb), ("kdb", dr, bb), ("k2b", dr, bb), ("vb", dr, bb), ("ogb", dr, bb)
                    for h in range(NH):
                        S.op("pe", lambda h=h: T.matmul(psc[dr][:, h * 128:(h + 1) * 128], Kd[:, h, j * 128:(j + 1) * 128], Q[:, h, j * 128:(j + 1) * 128], start=True, stop=True),
                             reads=[rk, rq], writes=[("pscC", dr)])
                    yield
                    S.op("dve", lambda: V.tensor_tensor(out=scm[dr][:].rearrange("p (h c) -> p h c", h=NH), in0=psc[dr][:, :].rearrange("p (h c) -> p h c", h=NH),
                                                        in1=mask[:, :].unsqueeze(1).to_broadcast([128, NH, 128]), op=ALU.mult),
                         reads=[("pscC", dr), "maskf", "maskb"], writes=[("scm", dr)])
                    yield
                    corder = (0, 1) if dr == 0 else (1, 0)
                    for ci, c in enumerate(corder):
                        rows = slice(c * 64, (c + 1) * 64)
                        chunk = (tok0 + j * 128 + c * 64) // 64
                        for h in range(NH):
                            S.op("pe", lambda h=h, rows=rows, c=c: T.matmul(po[dr][rows, h * 128:(h + 1) * 128], scm[dr][rows, h * 128 + c * 64: h * 128 + (c + 1) * 64],
                                                                           Vv[rows, j, h * 128:(h + 1) * 128], start=True, stop=False),
                                 reads=[("scm", dr), rv], writes=[("poC", dr)])
                            S.op("pe", lambda h=h, rows=rows, c=c: T.matmul(po[dr][rows, h * 128:(h + 1) * 128], Q[:, h, j * 128 + c * 64: j * 128 + (c + 1) * 64],
                                                                           Sbf[dr][h][:, :], start=False, stop=True),
                                 reads=[rq, ("Sbf", dr, h)], writes=[("poC", dr)])
                            S.op("pe", lambda h=h, rows=rows: T.matmul(pkv[dr][:, h * 128:(h + 1) * 128], K2[rows, j, h * 128:(h + 1) * 128],
                                                                      Vv[rows, j, h * 128:(h + 1) * 128], start=True, stop=True),
                                 reads=[rk2, rv], writes=[("pkvC", dr)])
                        yield
                        for h in range(NH):
                            a = cur["spp"][h]
                            S.op("dve", lambda h=h, a=a, chunk=chunk: V.scalar_tensor_tensor(
                                out=Sbf[dr][h][:], in0=S32[dr][h][a][:], scalar=decS[:, dr, h, chunk:chunk + 1],
                                in1=pkv[dr][:, h * 128:(h + 1) * 128], op0=ALU.mult, op1=ALU.add),
                                 reads=[("S32", dr, h, a), ("pkvC", dr)], writes=[("Sbf", dr, h)])
                            S.op("dve", lambda h=h, a=a, chunk=chunk: V.scalar_tensor_tensor(
                                out=S32[dr][h][1 - a][:], in0=S32[dr][h][a][:], scalar=decS[:, dr, h, chunk:chunk + 1],
                                in1=pkv[dr][:, h * 128:(h + 1) * 128], op0=ALU.mult, op1=ALU.add),
                                 reads=[("S32", dr, h, a), ("pkvC", dr)], writes=[("S32", dr, h, 1 - a)])
                            cur["spp"][h] = 1 - a
                        yield
                    if not second:
                        S.op("act", lambda: A.copy(out=osto[:, t, :], in_=po[dr][:, :]), reads=[("poC", dr)], writes=[("osto", t)])
                        return
                    mi = cur["bb"]
                    S.op("dve", lambda: V.tensor_tensor(out=o32[dr][:], in0=po[dr][:, :], in1=osto[:, t, :], op=ALU.add),
                         reads=[("poC", dr), ("osto", t)], writes=[("o32", dr)])
                    for h in range(NH):
                        S.op("act", lambda h=h: A.activation(out=junk[:], in_=o32[dr][:, h * 128:(h + 1) * 128], func=AF.Square, accum_out=ssq4[dr][:, h:h + 1]),
                             reads=[("o32", dr)], writes=["junkC", ("ssq4", dr)])
                    rstd_from_ssq(ssq4[dr][:], ssq4[dr][:], HD, [("ssq4", dr)], [("ssq4", dr)])
                    S.op("act", lambda: A.activation(out=sg[dr][:], in_=OG[:, j, :], func=AF.Exp, scale=-1.0), reads=[rog], writes=[("sgC", dr)])
                    yield
                    S.op("act", lambda: A.activation(out=sg[dr][:], in_=sg[dr][:], func=AF.Ln, bias=1.0), reads=[("sgC", dr)], writes=[("sgC", dr)])
                    S.op("act", lambda: A.activation(out=sg[dr][:], in_=sg[dr][:], func=AF.Exp, scale=-1.0), reads=[("sgC", dr)], writes=[("sgC", dr)])
                    S.op("dve", lambda: V.tensor_tensor(out=o32[dr][:], in0=o32[dr][:], in1=OG[:, j, :], op=ALU.mult), reads=[("o32", dr), rog], writes=[("o32", dr)])
                    S.op("dve", lambda: V.tensor_tensor(out=sg[dr][:], in0=sg[dr][:], in1=o32[dr][:], op=ALU.mult), reads=[("sgC", dr), ("o32", dr)], writes=[("sgC", dr)])
                    S.op("dve", lambda: V.tensor_tensor(out=mb[dr][:].rearrange("p (h c) -> p h c", h=NH), in0=sg[dr][:].rearrange("p (h c) -> p h c", h=NH),
                                                        in1=ssq4[dr][:, :].unsqueeze(2).to_broadcast([128, NH, 128]), op=ALU.mult),
                         reads=[("sgC", dr), ("ssq4", dr)], writes=[("mbC", dr)])
                    yield
                    for h in range(NH):
                        S.op("pe", lambda h=h: T.transpose(ptr[dr][:, h * 128:(h + 1) * 128], mb[dr][:, h * 128:(h + 1) * 128], ident[:]),
                             reads=[("mbC", dr), "ident"], writes=[("ptrC", dr)])
                    yield
                    S.op("act", lambda: A.activation(out=mst[dr][mi][:, :, j * 128:(j + 1) * 128], in_=ptr[dr][:, 0:512].rearrange("p (h t) -> p h t", h=NH),
                                                     func=AF.Copy, scale=normo[:, 0:1]),
                         reads=[("ptrC", dr), "normo"], writes=[("mstC", dr, mi)])
                    whole = (bi * 4 >= NT // 2) if dr == 0 else (bi * 4 + 3 < NT // 2)
                    if whole:
                        if last_in_blk:
                            S.dma("sp", mixT_s[512:1024, tok0:tok0 + 512].rearrange("(h p) t -> p h t", p=128), mst[dr][mi][:],
                                  reads=[("mstC", dr, mi)], writes=[("mixT_s", "hg", G)])
                    else:
                        S.dma("sp", mixT_s[512:1024, tok0 + j * 128:tok0 + (j + 1) * 128].rearrange("(h p) t -> p h t", p=128), mst[dr][mi][:, :, j * 128:(j + 1) * 128],
                              reads=[("mstC", dr, mi)], writes=[("mixT_s", "hg", G, j)])

                for si, (off, SL) in enumerate(zip(offs, seq_lens)):
                    NT = SL // 128
                    curs = [{"spp": [0] * NH, "bb": 0}, {"spp": [0] * NH, "bb": 0}]
                    for d in range(2):
                        for h in range(NH):
                            S.op("dve", lambda d=d, h=h: V.memset(S32[d][h][0][:], 0.0), writes=[("S32", d, h, 0)])
                            S.op("dve", lambda d=d, h=h: V.memset(Sbf[d][h][:], 0.0), writes=[("Sbf", d, h)])
                    for k in range(NT):
                        tf, tb_ = k, NT - 1 - k
                        gens = [c_step(0, off, SL, tf, tf >= NT // 2, curs[0]), c_step(1, off, SL, tb_, tb_ < NT // 2, curs[1])]
                        alive = [True, True]
                        while any(alive):
                            for d in range(2):
                                if alive[d]:
                                    try:
                                        next(gens[d])
                                    except StopIteration:
                                        alive[d] = False
                S.barrier()

        if "D" in phases:
            with ExitStack() as ph:
                def sb(name, shape, dt):
                    return ph.enter_context(nc.sbuf_tensor(name, shape, dt))

                def ps(name, shape, dt=F32):
                    return ph.enter_context(nc.psum_tensor(name, shape, dt))

                TS = 1024
                NTI = TS // 128
                woutb = sb("woutb", [128, 8, D], BF16)
                wpgb = sb("wpgb", [128, 8, D], BF16)
                wppb = sb("wppb", [128, 2, D], BF16)
                wrb = sb("wrb", [128, 8, 36], BF16)
                x1 = sb("x1", [128, NTI, D], F32)
                hnT = sb("hnT", [128, 8, TS], BF16)
                mt = [sb(f"mtD{i}", [128, 8, 512], BF16) for i in range(2)]
                NWB = 2
                wgb = [sb(f"wgb{i}", [128, 8, DE], BF16) for i in range(NWB)]
                wub = [sb(f"wub{i}", [128, 8, DE], BF16) for i in range(NWB)]
                wdb = [sb(f"wdb{i}", [128, 4, D], BF16) for i in range(NWB)]
                hid = [sb(f"hidD{i}", [128, 4, 512], BF16) for i in range(2)]
                sgt = [sb(f"sgD{i}", [128, 512], F32) for i in range(2)]
                xsb = [sb(f"xsD{i}", [128, D], BF16) for i in range(2)]
                junk = sb("junkD", [128, D], BF16)
                ssq = sb("ssqD", [128, NTI], F32)
                rstd = sb("rstdD", [128, NTI], F32)
                ssq3 = sb("ssq3D", [128, NTI], F32)
                rstd3 = sb("rstd3D", [128, NTI], F32)
                rl = sb("rlD", [128, NTI, 36], F32)
                gates = sb("gatesD", [128, NTI, NE], F32)
                r_mg = sb("r_mg", [128, NTI], F32)
                r_gm = sb("r_gm", [128, NTI, 4], F32)
                r_eg = sb("r_eg", [128, NTI, 4], F32)
                r_sg = sb("r_sg", [128, NTI], F32)
                r_lem = sb("r_lem", [128, NTI, NE], F32)
                r_lem2 = sb("r_lem2", [128, NTI, NE], F32)
                r_oh1 = sb("r_oh1", [128, NTI, NE], F32)
                r_oh2 = sb("r_oh2", [128, NTI, NE], F32)
                r_m1 = sb("r_m1", [128, NTI], F32)
                r_m2 = sb("r_m2", [128, NTI], F32)
                r_w1 = sb("r_w1", [128, NTI], F32)
                r_w2 = sb("r_w2", [128, NTI], F32)
                pld = sb("pld", [128, NTI, PD], F32)
                pbf = [sb(f"pbf{i}", [128, PD], BF16) for i in range(2)]
                ptT = [sb(f"ptT{i}", [128, 2, 128], BF16) for i in range(2)]
                hpT = [sb(f"hpT{i}", [128, 8, 128], BF16) for i in range(2)]
                sig = [sb(f"sigD{i}", [128, D], F32) for i in range(2)]
                pA = ps("pA", [128, 1024], F32)
                pB = ps("pB", [128, 1024], F32)
                pC = ps("pC", [128, 1024], F32)
                pD_ = ps("pD", [128, 1024], F32)
                pCb = pC[:, :].bitcast(BF16)
                pDb = pD_[:, :].bitcast(BF16)

                S.dma("pool", woutb[:], w_out.rearrange("(k p) n -> p k n", p=128), writes=["woutb"])
                S.dma("pool", wpgb[:], w_pg.rearrange("(k p) n -> p k n", p=128), writes=["wpgb"])
                S.dma("pool", wppb[:], w_pp.rearrange("(k p) n -> p k n", p=128), writes=["wppb"])
                S.dma("pool", wrb[:, :, 0:4], w_rg.rearrange("(k p) n -> p k n", p=128), writes=["wrb"])
                S.dma("pool", wrb[:, :, 4:36], w_re.rearrange("(k p) n -> p k n", p=128), writes=["wrb"])

                def norm_T(xin_ap, xin_res, rstd_col, rstd_res, gain, gain_res, out_ap, out_res, pbank, pbank_res, xsi):
                    S.op("act", lambda: A.activation(out=xsb[xsi][:], in_=xin_ap, func=AF.Copy, scale=rstd_col),
                         reads=xin_res + [rstd_res], writes=[("xsD", xsi)])
                    for k in range(8):
                        S.op("pe", lambda k=k: T.transpose(pbank[:, k * 128:(k + 1) * 128], xsb[xsi][:, k * 128:(k + 1) * 128], ident[:]),
                             reads=[("xsD", xsi), "ident"], writes=[pbank_res])
                    S.op("dve", lambda: V.tensor_tensor(out=out_ap, in0=pbank.rearrange("p (k t) -> p k t", k=8),
                                                        in1=gain[:, :].unsqueeze(2).to_broadcast([128, 8, 128]), op=ALU.mult),
                         reads=[pbank_res, gain_res], writes=[out_res])

                def sumsq(i, col_t, res):
                    S.op("act", lambda: A.activation(out=junk[:], in_=x1[:, i, :], func=AF.Square, accum_out=col_t[:, i:i + 1]),
                         reads=[("x1", i)], writes=["junkD", res])

                NST = NTOK // TS
                wload_i = [0]
                nxt_w = [None]

                def load_expert(e):
                    wb = wload_i[0] % NWB
                    wload_i[0] += 1
                    S.dma("pool", wgb[wb][:], w_eg[e].rearrange("(k p) n -> p k n", p=128), writes=[("wgb", wb)])
                    S.dma("pool", wub[wb][:], w_eu[e].rearrange("(k p) n -> p k n", p=128), writes=[("wub", wb)])
                    S.dma("pool", wdb[wb][:], w_ed[e].rearrange("(k p) n -> p k n", p=128), writes=[("wdb", wb)])
                    return wb

                for st in range(NST):
                    T0 = st * TS
                    G0 = T0 // 512
                    mix_reads = [("mixT_s", "hg", G0), ("mixT_s", "hg", G0 + 1)] if "C" in phases else []
                    S.dma("sp", pld[:], p_in[T0:T0 + TS, :].rearrange("(j p) n -> p j n", p=128), writes=["pld"])
                    for hf in range(2):
                        S.dma("sp", mt[hf][:], mixT_s[:, T0 + hf * 512:T0 + (hf + 1) * 512].rearrange("(k p) t -> p k t", p=128),
                              reads=mix_reads, writes=[("mtD", hf)])
                    for i in range(NTI):
                        S.dma("sp", x1[:, i, :], x[T0 + i * 128:T0 + (i + 1) * 128, :], writes=[("x1", i)])
                    for i in range(NTI):
                        pO = pA if i % 2 == 0 else pB
                        pOr = "pA" if i % 2 == 0 else "pB"
                        hf = i // 4
                        jj = i % 4
                        for half in range(2):
                            for k in range(8):
                                S.op("pe", lambda k=k, half=half: T.matmul(pO[:, half * 512:(half + 1) * 512], mt[hf][:, k, jj * 128:(jj + 1) * 128],
                                                                          woutb[:, k, half * 512:(half + 1) * 512], start=(k == 0), stop=(k == 7)),
                                     reads=[("mtD", hf), "woutb"], writes=[(pOr, half)])
                        S.op("dve", lambda i=i: V.tensor_tensor(out=x1[:, i, :], in0=pO[:, :], in1=x1[:, i, :], op=ALU.add),
                             reads=[(pOr, 0), (pOr, 1), ("x1", i)], writes=[("x1", i)])
                        sumsq(i, ssq, "ssqD")
                    rstd_from_ssq(ssq[:], rstd[:], D, ["ssqD"], ["rstdD"])
                    for i in range(NTI):
                        pbk = pCb[:, (i % 2) * 1024:(i % 2 + 1) * 1024]
                        norm_T(x1[:, i, :], [("x1", i)], rstd[:, i:i + 1], "rstdD", gffn, "gffn",
                               hnT[:, :, i * 128:(i + 1) * 128], ("hnT", i), pbk, ("pC", i % 2), i % 2)
                        for k in range(8):
                            S.op("pe", lambda k=k, i=i: T.matmul(pD_[:, (i % 2) * 512:(i % 2) * 512 + 36], hnT[:, k, i * 128:(i + 1) * 128], wrb[:, k, :],
                                                                start=(k == 0), stop=(k == 7)),
                                 reads=[("hnT", i), "wrb"], writes=[("pD", i % 2)])
                        S.op("act", lambda i=i: A.copy(out=rl[:, i, :], in_=pD_[:, (i % 2) * 512:(i % 2) * 512 + 36]), reads=[("pD", i % 2)], writes=[("rl", i)])
                    rl_res = [("rl", i) for i in range(NTI)]
                    lg = rl[:, :, 0:4]
                    le = rl[:, :, 4:36]
                    S.op("dve", lambda: V.tensor_reduce(out=r_mg[:], in_=lg, axis=AX.X, op=ALU.max), reads=rl_res, writes=["r_mg"])
                    mgb = r_mg[:, :].unsqueeze(2).to_broadcast([128, NTI, 4])
                    S.op("dve", lambda: V.tensor_tensor(out=r_gm[:], in0=lg, in1=mgb, op=ALU.is_equal), reads=rl_res + ["r_mg"], writes=["r_gm"])
                    S.op("dve", lambda: V.tensor_tensor(out=r_eg[:], in0=lg, in1=mgb, op=ALU.subtract), reads=rl_res + ["r_mg"], writes=["r_eg"])
                    S.op("act", lambda: A.activation(out=r_eg[:], in_=r_eg[:], func=AF.Exp), reads=["r_eg"], writes=["r_eg"])
                    S.op("dve", lambda: V.tensor_reduce(out=r_sg[:], in_=r_eg[:], axis=AX.X, op=ALU.add), reads=["r_eg"], writes=["r_sg"])
                    S.op("dve", lambda: V.reciprocal(out=r_sg[:], in_=r_sg[:]), reads=["r_sg"], writes=["r_sg"])
                    S.op("dve", lambda: V.tensor_scalar(out=r_gm[:], in0=r_gm[:], scalar1=1.0, scalar2=BIG, op0=ALU.subtract, op1=ALU.mult), reads=["r_gm"], writes=["r_gm"])
                    for i in range(NTI):
                        S.op("dve", lambda i=i: V.tensor_tensor(out=r_lem[:, i, :].rearrange("p (g j) -> p g j", g=4),
                                                                in0=rl[:, i, 4:36].rearrange("p (g j) -> p g j", g=4),
                                                                in1=r_gm[:, i, :].unsqueeze(2).to_broadcast([128, 4, 8]), op=ALU.add),
                             reads=rl_res + ["r_gm"], writes=["r_lem"])
                    S.op("dve", lambda: V.tensor_reduce(out=r_m1[:], in_=r_lem[:], axis=AX.X, op=ALU.max), reads=["r_lem"], writes=["r_m1"])
                    S.op("dve", lambda: V.tensor_tensor(out=r_oh1[:], in0=r_lem[:], in1=r_m1[:, :].unsqueeze(2).to_broadcast([128, NTI, NE]), op=ALU.is_equal),
                         reads=["r_lem", "r_m1"], writes=["r_oh1"])
                    S.op("dve", lambda: V.scalar_tensor_tensor(out=r_lem2[:], in0=r_oh1[:], scalar=-BIG, in1=r_lem[:], op0=ALU.mult, op1=ALU.add),
                         reads=["r_oh1", "r_lem"], writes=["r_lem2"])
                    S.op("dve", lambda: V.tensor_reduce(out=r_m2[:], in_=r_lem2[:], axis=AX.X, op=ALU.max), reads=["r_lem2"], writes=["r_m2"])
                    S.op("dve", lambda: V.tensor_tensor(out=r_oh2[:], in0=r_lem2[:], in1=r_m2[:, :].unsqueeze(2).to_broadcast([128, NTI, NE]), op=ALU.is_equal),
                         reads=["r_lem2", "r_m2"], writes=["r_oh2"])
                    S.op("dve", lambda: V.tensor_sub(out=r_m2[:], in0=r_m2[:], in1=r_m1[:]), reads=["r_m2", "r_m1"], writes=["r_m2"])
                    S.op("act", lambda: A.activation(out=r_m2[:], in_=r_m2[:], func=AF.Exp), reads=["r_m2"], writes=["r_m2"])
                    S.op("dve", lambda: V.tensor_scalar_add(out=r_w1[:], in0=r_m2[:], scalar1=1.0), reads=["r_m2"], writes=["r_w1"])
                    S.op("dve", lambda: V.reciprocal(out=r_w1[:], in_=r_w1[:]), reads=["r_w1"], writes=["r_w1"])
                    S.op("dve", lambda: V.tensor_mul(out=r_w1[:], in0=r_w1[:], in1=r_sg[:]), reads=["r_w1", "r_sg"], writes=["r_w1"])
                    S.op("dve", lambda: V.tensor_mul(out=r_w2[:], in0=r_w1[:], in1=r_m2[:]), reads=["r_w1", "r_m2"], writes=["r_w2"])
                    S.op("dve", lambda: V.tensor_tensor(out=r_oh1[:], in0=r_oh1[:], in1=r_w1[:, :].unsqueeze(2).to_broadcast([128, NTI, NE]), op=ALU.mult),
                         reads=["r_oh1", "r_w1"], writes=["r_oh1"])
                    S.op("dve", lambda: V.tensor_tensor(out=r_oh2[:], in0=r_oh2[:], in1=r_w2[:, :].unsqueeze(2).to_broadcast([128, NTI, NE]), op=ALU.mult),
                         reads=["r_oh2", "r_w2"], writes=["r_oh2"])
                    S.op("dve", lambda: V.tensor_add(out=gates[:], in0=r_oh1[:], in1=r_oh2[:]), reads=["r_oh1", "r_oh2"], writes=["gates"])

                    hnT_res = [("hnT", i) for i in range(NTI)]
                    hi = 0
                    di = 0
                    for e in range(NE):
                        if nxt_w[0] is None:
                            nxt_w[0] = load_expert(e)
                        wb = nxt_w[0]
                        if e + 1 < NE:
                            nxt_w[0] = load_expert(e + 1)
                        elif st + 1 < NST:
                            nxt_w[0] = load_expert(0)
                        else:
                            nxt_w[0] = None
                        for hf in range(2):
                            hb = hi % 2
                            hi += 1
                            for c in range(4):
                                pGU = pA if c % 2 == 0 else pB
                                pGUr = "pA" if c % 2 == 0 else "pB"
                                for k in range(8):
                                    S.op("pe", lambda k=k, c=c: T.matmul(pGU[:, 0:512], wgb[wb][:, k, c * 128:(c + 1) * 128], hnT[:, k, hf * 512:(hf + 1) * 512],
                                                                        start=(k == 0), stop=(k == 7)),
                                         reads=[("wgb", wb)] + hnT_res, writes=[(pGUr, 0)])
                                for k in range(8):
                                    S.op("pe", lambda k=k, c=c: T.matmul(pGU[:, 512:1024], wub[wb][:, k, c * 128:(c + 1) * 128], hnT[:, k, hf * 512:(hf + 1) * 512],
                                                                        start=(k == 0), stop=(k == 7)),
                                         reads=[("wub", wb)] + hnT_res, writes=[(pGUr, 1)])
                                S.op("act", lambda c=c: A.activation(out=sgt[c % 2][:], in_=pGU[:, 0:512], func=AF.Silu), reads=[(pGUr, 0)], writes=[("sgD", c % 2)])
                                S.op("dve", lambda c=c: V.tensor_tensor(out=hid[hb][:, c, :], in0=pGU[:, 512:1024], in1=sgt[c % 2][:], op=ALU.mult),
                                     reads=[(pGUr, 1), ("sgD", c % 2)], writes=[("hid", hb, c)])
                            for jj in range(4):
                                i = hf * 4 + jj
                                pDn = pC if di % 2 == 0 else pD_
                                pDr = "pC" if di % 2 == 0 else "pD"
                                di += 1
                                for half in range(2):
                                    for c in range(4):
                                        S.op("pe", lambda c=c, half=half, jj=jj: T.matmul(pDn[:, half * 512:(half + 1) * 512], hid[hb][:, c, jj * 128:(jj + 1) * 128],
                                                                                       wdb[wb][:, c, half * 512:(half + 1) * 512], start=(c == 0), stop=(c == 3)),
                                             reads=[("hid", hb, c), ("wdb", wb)], writes=[(pDr, half)])
                                S.op("dve", lambda i=i, e=e: V.scalar_tensor_tensor(out=x1[:, i, :], in0=pDn[:, :], scalar=gates[:, i, e:e + 1], in1=x1[:, i, :],
                                                                                   op0=ALU.mult, op1=ALU.add),
                                     reads=[(pDr, 0), (pDr, 1), "gates", ("x1", i)], writes=[("x1", i)])
                    for i in range(NTI):
                        sumsq(i, ssq, "ssqD")
                    rstd_from_ssq(ssq[:], rstd[:], D, ["ssqD"], ["rstdD"])
                    for i in range(NTI):
                        b2 = i % 2
                        pbk = pCb[:, b2 * 1024:(b2 + 1) * 1024]
                        norm_T(x1[:, i, :], [("x1", i)], rstd[:, i:i + 1], "rstdD", gple, "gple",
                               hpT[b2][:], ("hpT", b2), pbk, ("pC", b2), b2)
                        S.op("act", lambda i=i: A.copy(out=pbf[b2][:], in_=pld[:, i, :]), reads=["pld"], writes=[("pbf", b2)])
                        for k in range(2):
                            S.op("pe", lambda k=k: T.transpose(pDb[:, b2 * 1024 + k * 128: b2 * 1024 + (k + 1) * 128], pbf[b2][:, k * 128:(k + 1) * 128], ident[:]),
                                 reads=[("pbf", b2), "ident"], writes=[("pD", b2)])
                        S.op("act", lambda: A.copy(out=ptT[b2][:], in_=pDb[:, b2 * 1024: b2 * 1024 + 256].rearrange("p (k t) -> p k t", k=2)),
                             reads=[("pD", b2)], writes=[("ptT", b2)])
                        for half in range(2):
                            for k in range(8):
                                S.op("pe", lambda k=k, half=half: T.matmul(pA[:, half * 512:(half + 1) * 512], hpT[b2][:, k, :], wpgb[:, k, half * 512:(half + 1) * 512],
                                                                          start=(k == 0), stop=(k == 7)),
                                     reads=[("hpT", b2), "wpgb"], writes=[("pA", half)])
                        for half in range(2):
                            for k in range(2):
                                S.op("pe", lambda k=k, half=half: T.matmul(pB[:, half * 512:(half + 1) * 512], ptT[b2][:, k, :], wppb[:, k, half * 512:(half + 1) * 512],
                                                                          start=(k == 0), stop=(k == 1)),
                                     reads=[("ptT", b2), "wppb"], writes=[("pB", half)])
                        S.op("act", lambda: A.activation(out=sig[b2][:], in_=pA[:, :], func=AF.Sigmoid), reads=[("pA", 0), ("pA", 1)], writes=[("sigD", b2)])
                        S.op("dve", lambda: V.tensor_tensor(out=sig[b2][:], in0=pB[:, :], in1=sig[b2][:], op=ALU.mult),
                             reads=[("pB", 0), ("pB", 1), ("sigD", b2)], writes=[("sigD", b2)])
                        S.op("dve", lambda i=i: V.tensor_tensor(out=x1[:, i, :], in0=sig[b2][:], in1=x1[:, i, :], op=ALU.add),
                             reads=[("sigD", b2), ("x1", i)], writes=[("x1", i)])
                        sumsq(i, ssq3, "ssq3D")
                    rstd_from_ssq(ssq3[:], rstd3[:], D, ["ssq3D"], ["rstd3D"])
                    for i in range(NTI):
                        b2 = i % 2
                        S.op("dve", lambda i=i: V.scalar_tensor_tensor(out=sig[b2][:], in0=x1[:, i, :], scalar=rstd3[:, i:i + 1], in1=gfin[:], op0=ALU.mult, op1=ALU.mult),
                             reads=[("x1", i), "rstd3D", "gfin"], writes=[("sigD", b2)])
                        S.dma("sp", y[T0 + i * 128:T0 + (i + 1) * 128, :], sig[b2][:], reads=[("sigD", b2)], writes=[("y", st, i)])
                S.barrier()

        if "E" in phases:
            esub = [c for c in phases if c.isdigit()] or ["0", "1", "2", "3"]
            TS = 1024
            NTI = TS // 128
            NTILE = NTOK // 128
            NST = NTOK // TS
            with ExitStack() as phE:
                def sbE(name, shape, dt):
                    return phE.enter_context(nc.sbuf_tensor(name, shape, dt))

                OH1 = sbE("OH1", [128, NTILE, NE], F32)
                OH2 = sbE("OH2", [128, NTILE, NE], F32)
                W12 = sbE("W12", [128, NTILE, 2], F32)
                WI = sbE("WI", [128, NSLOT, 6], I32)
                S.dma("sp", rec_s, c_recpad, writes=["rec_s"])

                with ExitStack() as ph:
                    if "0" in esub:
                        def sb(name, shape, dt):
                            return ph.enter_context(nc.sbuf_tensor(name + '_e0', shape, dt))

                        def ps(name, shape, dt=F32):
                            return ph.enter_context(nc.psum_tensor(name + '_e0', shape, dt))

                        woutb = sb("woutb", [128, 8, D], BF16)
                        wrb = sb("wrb", [128, 8, 36], BF16)
                        gffb = sb("gffb", [128, D], F32)
                        x1 = sb("x1", [128, 2 * NTI, D], F32)
                        mt = [sb(f"mtD{i}", [128, 8, 512], BF16) for i in range(4)]
                        hnb = [sb(f"hnb{i}", [128, D], BF16) for i in range(2)]
                        hnT = [sb(f"hnTt{i}", [128, 8, 128], BF16) for i in range(2)]
                        junk = sb("junkD", [128, D], BF16)
                        ssq = sb("ssqD", [128, NTI], F32)
                        rstd = sb("rstdD", [128, NTI], F32)
                        rl = sb("rlD", [128, NTI, 36], F32)
                        r_mg = sb("r_mg", [128, NTI], F32)
                        r_gm = sb("r_gm", [128, NTI, 4], F32)
                        r_eg = sb("r_eg", [128, NTI, 4], F32)
                        r_sg = sb("r_sg", [128, NTI], F32)
                        r_lem = sb("r_lem", [128, NTI, NE], F32)
                        r_lem2 = sb("r_lem2", [128, NTI, NE], F32)
                        r_m1 = sb("r_m1", [128, NTI], F32)
                        r_m2 = sb("r_m2", [128, NTI], F32)
                        r_w1 = sb("r_w1", [128, NTI], F32)
                        pA = ps("pA", [128, 1024], F32)
                        pB = ps("pB", [128, 1024], F32)
                        pC = ps("pC", [128, 1024], F32)
                        pD_ = ps("pD", [128, 1024], F32)
                        pCb = pC[:, :].bitcast(BF16)
                        S.dma("pool", woutb[:], w_out.rearrange("(k p) n -> p k n", p=128), writes=["woutb"])
                        S.dma("pool", wrb[:, :, 0:4], w_rg.rearrange("(k p) n -> p k n", p=128), writes=["wrb"])
                        S.dma("pool", wrb[:, :, 4:36], w_re.rearrange("(k p) n -> p k n", p=128), writes=["wrb"])
                        S.dma("sp", gffb[:], norm_ffn.partition_broadcast(128), writes=["gffb"])
                        def e0_loads(st_):
                            T0_ = st_ * TS
                            xo_ = (st_ % 2) * NTI
                            for hf_ in range(2):
                                S.dma("pool", mt[(st_ % 2) * 2 + hf_][:], mixT_s[:, T0_ + hf_ * 512:T0_ + (hf_ + 1) * 512].rearrange("(k p) t -> p k t", p=128),
                                      writes=[("mtD", (st_ % 2) * 2 + hf_)])
                            for i_ in range(NTI):
                                S.dma("pool", x1[:, xo_ + i_, :], x[T0_ + i_ * 128:T0_ + (i_ + 1) * 128, :], writes=[("x1", xo_ + i_)])

                        e0_loads(0)
                        for st in range(NST):
                            T0 = st * TS
                            xo = (st % 2) * NTI
                            mo = (st % 2) * 2
                            if st + 1 < NST:
                                e0_loads(st + 1)
                            for i in range(NTI):
                                pO = pA if i % 2 == 0 else pB
                                pOr = "pA" if i % 2 == 0 else "pB"
                                hf = i // 4
                                jj = i % 4
                                for half in range(2):
                                    for k in range(8):
                                        S.op("pe", lambda k=k, half=half: T.matmul(pO[:, half * 512:(half + 1) * 512], mt[mo + hf][:, k, jj * 128:(jj + 1) * 128],
                                                                                  woutb[:, k, half * 512:(half + 1) * 512], start=(k == 0), stop=(k == 7)),
                                             reads=[("mtD", mo + hf), "woutb"], writes=[(pOr, half)])
                                S.op("dve", lambda i=i: V.tensor_tensor(out=x1[:, xo + i, :], in0=pO[:, :], in1=x1[:, xo + i, :], op=ALU.add),
                                     reads=[(pOr, 0), (pOr, 1), ("x1", xo + i)], writes=[("x1", xo + i)])
                                S.op("act", lambda i=i: A.activation(out=junk[:], in_=x1[:, xo + i, :], func=AF.Square, accum_out=ssq[:, i:i + 1]),
                                     reads=[("x1", xo + i)], writes=["junkD", "ssqD"])
                                S.dma("sp", x1_s[T0 + i * 128:T0 + (i + 1) * 128, :], x1[:, xo + i, :], reads=[("x1", xo + i)], writes=[("x1_s", st, i)])
                            rstd_from_ssq(ssq[:], rstd[:], D, ["ssqD"], ["rstdD"])
                            for i in range(NTI):
                                b2 = i % 2
                                S.op("dve", lambda i=i: V.scalar_tensor_tensor(out=hnb[b2][:], in0=x1[:, xo + i, :], scalar=rstd[:, i:i + 1], in1=gffb[:], op0=ALU.mult, op1=ALU.mult),
                                     reads=[("x1", xo + i), "rstdD", "gffb"], writes=[("hnb", b2)])
                                S.dma("sp", hn_s[T0 + i * 128:T0 + (i + 1) * 128, :], hnb[b2][:], reads=[("hnb", b2)], writes=[("hn_s", st, i)])
                                pbk = pCb[:, b2 * 1024:(b2 + 1) * 1024]
                                for k in range(8):
                                    S.op("pe", lambda k=k: T.transpose(pbk[:, k * 128:(k + 1) * 128], hnb[b2][:, k * 128:(k + 1) * 128], ident[:]),
                                         reads=[("hnb", b2), "ident"], writes=[("pC", b2)])
                                S.op("act", lambda: A.copy(out=hnT[b2][:], in_=pbk.rearrange("p (k t) -> p k t", k=8)), reads=[("pC", b2)], writes=[("hnTt", b2)])
                                for k in range(8):
                                    S.op("pe", lambda k=k: T.matmul(pD_[:, b2 * 512:b2 * 512 + 36], hnT[b2][:, k, :], wrb[:, k, :], start=(k == 0), stop=(k == 7)),
                                         reads=[("hnTt", b2), "wrb"], writes=[("pD", b2)])
                                S.op("act", lambda i=i: A.copy(out=rl[:, i, :], in_=pD_[:, b2 * 512:b2 * 512 + 36]), reads=[("pD", b2)], writes=[("rl", i)])
                            tsl = slice(st * NTI, (st + 1) * NTI)
                            oh1 = OH1[:, tsl, :]
                            oh2 = OH2[:, tsl, :]
                            rl_res = [("rl", i) for i in range(NTI)]
                            lg = rl[:, :, 0:4]
                            S.op("dve", lambda: V.tensor_reduce(out=r_mg[:], in_=lg, axis=AX.X, op=ALU.max), reads=rl_res, writes=["r_mg"])
                            mgb = r_mg[:, :].unsqueeze(2).to_broadcast([128, NTI, 4])
                            S.op("dve", lambda: V.tensor_tensor(out=r_gm[:], in0=lg, in1=mgb, op=ALU.is_equal), reads=rl_res + ["r_mg"], writes=["r_gm"])
                            S.op("dve", lambda: V.tensor_tensor(out=r_eg[:], in0=lg, in1=mgb, op=ALU.subtract), reads=rl_res + ["r_mg"], writes=["r_eg"])
                            S.op("act", lambda: A.activation(out=r_eg[:], in_=r_eg[:], func=AF.Exp), reads=["r_eg"], writes=["r_eg"])
                            S.op("dve", lambda: V.tensor_reduce(out=r_sg[:], in_=r_eg[:], axis=AX.X, op=ALU.add), reads=["r_eg"], writes=["r_sg"])
                            S.op("dve", lambda: V.reciprocal(out=r_sg[:], in_=r_sg[:]), reads=["r_sg"], writes=["r_sg"])
                            S.op("dve", lambda: V.tensor_scalar(out=r_gm[:], in0=r_gm[:], scalar1=1.0, scalar2=BIG, op0=ALU.subtract, op1=ALU.mult), reads=["r_gm"], writes=["r_gm"])
                            for i in range(NTI):
                                S.op("dve", lambda i=i: V.tensor_tensor(out=r_lem[:, i, :].rearrange("p (g j) -> p g j", g=4),
                                                                        in0=rl[:, i, 4:36].rearrange("p (g j) -> p g j", g=4),
                                                                        in1=r_gm[:, i, :].unsqueeze(2).to_broadcast([128, 4, 8]), op=ALU.add),
                                     reads=rl_res + ["r_gm"], writes=["r_lem"])
                            S.op("dve", lambda: V.tensor_reduce(out=r_m1[:], in_=r_lem[:], axis=AX.X, op=ALU.max), reads=["r_lem"], writes=["r_m1"])
                            S.op("dve", lambda: V.tensor_tensor(out=oh1, in0=r_lem[:], in1=r_m1[:, :].unsqueeze(2).to_broadcast([128, NTI, NE]), op=ALU.is_equal),
                                 reads=["r_lem", "r_m1"], writes=[("OH1", st)])
                            S.op("dve", lambda: V.scalar_tensor_tensor(out=r_lem2[:], in0=oh1, scalar=-BIG, in1=r_lem[:], op0=ALU.mult, op1=ALU.add),
                                 reads=[("OH1", st), "r_lem"], writes=["r_lem2"])
                            S.op("dve", lambda: V.tensor_reduce(out=r_m2[:], in_=r_lem2[:], axis=AX.X, op=ALU.max), reads=["r_lem2"], writes=["r_m2"])
                            S.op("dve", lambda: V.tensor_tensor(out=oh2, in0=r_lem2[:], in1=r_m2[:, :].unsqueeze(2).to_broadcast([128, NTI, NE]), op=ALU.is_equal),
                                 reads=["r_lem2", "r_m2"], writes=[("OH2", st)])
                            S.op("dve", lambda: V.tensor_sub(out=r_m2[:], in0=r_m2[:], in1=r_m1[:]), reads=["r_m2", "r_m1"], writes=["r_m2"])
                            S.op("act", lambda: A.activation(out=r_m2[:], in_=r_m2[:], func=AF.Exp), reads=["r_m2"], writes=["r_m2"])
                            S.op("dve", lambda: V.tensor_scalar_add(out=r_w1[:], in0=r_m2[:], scalar1=1.0), reads=["r_m2"], writes=["r_w1"])
                            S.op("dve", lambda: V.reciprocal(out=r_w1[:], in_=r_w1[:]), reads=["r_w1"], writes=["r_w1"])
                            S.op("dve", lambda: V.tensor_mul(out=W12[:, tsl, 0], in0=r_w1[:], in1=r_sg[:]), reads=["r_w1", "r_sg"], writes=[("W12", st, 0)])
                            S.op("dve", lambda: V.tensor_mul(out=W12[:, tsl, 1], in0=W12[:, tsl, 0], in1=r_m2[:]), reads=[("W12", st, 0), "r_m2"], writes=[("W12", st, 1)])
                        S.barrier()

                with ExitStack() as ph:
                    if "1" in esub:
                        def sb(name, shape, dt):
                            return ph.enter_context(nc.sbuf_tensor(name + '_e1', shape, dt))

                        def ps(name, shape, dt=F32):
                            return ph.enter_context(nc.psum_tensor(name + '_e1', shape, dt))

                        NTHR = NTOK // SLOT
                        Lb = sb("Lb", [128, 128], BF16)
                        onesb = sb("onesb", [128, 128], BF16)
                        mask96 = sb("mask96", [128, NE * NTILE], F32)
                        tokid = sb("tokid", [128, NTILE], F32)
                        slotst = sb("slotst", [128, NSLOT], F32)
                        thr = sb("thr", [128, NTHR], F32)
                        addc = sb("addc", [128, 6], F32)
                        mulc = sb("mulc", [128, 6], F32)
                        ones32 = sb("ones32", [128, NE], F32)
                        ohE = sb("ohE", [128, NE, NTILE], F32)
                        incl = sb("incl", [128, NE, NTILE], F32)
                        ohb = sb("ohb", [128, NTILE, NE], BF16)
                        ohcb = sb("ohcb", [128, NE, NTILE], BF16)
                        inclb = sb("inclb", [128, NE], BF16)
                        C_all = sb("C_all", [128, NTILE, NE], F32)
                        prod = sb("prodE", [128, NTILE, NE], F32)
                        cnt = sb("cntE", [128, NE], F32)
                        cmpt = sb("cmpt", [128, NE, NTHR], F32)
                        padded = sb("padded", [128, NE], F32)
                        inclp = sb("inclp", [128, NE], F32)
                        base = sb("baseE", [128, NE], F32)
                        POS = sb("POS", [128, NTILE, 2], F32)
                        POSI = sb("POSI", [128, NTILE, 2], I32)
                        REC = sb("REC", [128, NTILE, 2, 4], I32)
                        cmps = sb("cmps", [128, NSLOT, NE], F32)
                        slot_e = sb("slot_e", [128, NSLOT], F32)
                        WIf = sb("WIf", [128, NSLOT, 6], F32)
                        pS = [ps(f"pA{i}", [128, 512], F32) if False else ps(f"pSx{i}", [128, 512], F32) for i in range(2)]
                        pS2 = ps("pSy", [128, 512], F32)
                        for (t_, src, nm) in ((Lb, c_L, "Lb"), (onesb, c_ones, "onesb"), (mask96, c_mask96, "mask96"), (tokid, c_tokid, "tokid"),
                                              (slotst, c_slotstart, "slotst"), (thr, c_thr, "thr"), (addc, c_addc, "addc"), (mulc, c_mulc, "mulc")):
                            S.dma("sp", t_[:], src, writes=[nm])
                        S.op("dve", lambda: V.memset(ones32[:], 1.0), writes=["ones32"])
                        S.op("dve", lambda: V.tensor_tensor(out=ohE[:].rearrange("p e t -> p t e"), in0=OH1[:], in1=OH2[:], op=ALU.add), writes=["ohE"])
                        S.op("dve", lambda: V.tensor_tensor(out=ohb[:], in0=OH1[:], in1=OH2[:], op=ALU.add), writes=["ohb"])
                        S.op("dve", lambda: V.tensor_tensor_scan(out=incl[:].rearrange("p e t -> p (e t)"), data0=mask96[:], data1=ohE[:].rearrange("p e t -> p (e t)"),
                                                                 initial=0.0, op0=ALU.mult, op1=ALU.add), reads=["ohE", "mask96"], writes=["incl"])
                        S.op("dve", lambda: V.tensor_tensor(out=ohcb[:], in0=incl[:], in1=ohE[:], op=ALU.subtract), reads=["incl", "ohE"], writes=["ohcb"])
                        S.op("dve", lambda: V.tensor_copy(out=inclb[:], in_=incl[:, :, NTILE - 1]), reads=["incl"], writes=["inclb"])
                        for i in range(NTILE):
                            pb = (i // 16) % 2
                            sl = (i % 16) * NE
                            S.op("pe", lambda i=i, pb=pb, sl=sl: T.matmul(pS[pb][:, sl:sl + NE], Lb[:], ohb[:, i, :], start=True, stop=False),
                                 reads=["Lb", "ohb"], writes=[("pSx", pb)])
                            S.op("pe", lambda i=i, pb=pb, sl=sl: T.matmul(pS[pb][:, sl:sl + NE], onesb[:], ohcb[:, :, i], start=False, stop=True),
                                 reads=["onesb", "ohcb"], writes=[("pSx", pb)])
                            if i % 16 == 15 or i == NTILE - 1:
                                i0 = (i // 16) * 16
                                n_ = i - i0 + 1
                                S.op("dve", lambda i0=i0, n_=n_, pb=pb: V.tensor_copy(out=C_all[:, i0:i0 + n_, :], in_=pS[pb][:, 0:n_ * NE].rearrange("p (t e) -> p t e", e=NE)),
                                     reads=[("pSx", pb)], writes=["C_all"])
                        S.op("pe", lambda: T.matmul(pS2[:, 0:NE], onesb[:], inclb[:], start=True, stop=True), reads=["onesb", "inclb"], writes=["pSy"])
                        S.op("dve", lambda: V.tensor_copy(out=cnt[:], in_=pS2[:, 0:NE]), reads=["pSy"], writes=["cntE"])
                        S.op("dve", lambda: V.tensor_tensor(out=cmpt[:], in0=cnt[:, :].unsqueeze(2).to_broadcast([128, NE, NTHR]),
                                                            in1=thr[:, :].unsqueeze(1).to_broadcast([128, NE, NTHR]), op=ALU.is_gt),
                             reads=["cntE", "thr"], writes=["cmpt"])
                        S.op("dve", lambda: V.tensor_reduce(out=padded[:], in_=cmpt[:], axis=AX.X, op=ALU.add), reads=["cmpt"], writes=["padded"])
                        S.op("dve", lambda: V.tensor_scalar_mul(out=padded[:], in0=padded[:], scalar1=float(SLOT)), reads=["padded"], writes=["padded"])
                        S.op("dve", lambda: V.tensor_tensor_scan(out=inclp[:], data0=ones32[:], data1=padded[:], initial=0.0, op0=ALU.mult, op1=ALU.add),
                             reads=["ones32", "padded"], writes=["inclp"])
                        S.op("dve", lambda: V.tensor_sub(out=base[:], in0=inclp[:], in1=padded[:]), reads=["inclp", "padded"], writes=["baseE"])
                        S.op("dve", lambda: V.tensor_tensor(out=C_all[:], in0=C_all[:], in1=base[:, :].unsqueeze(1).to_broadcast([128, NTILE, NE]), op=ALU.add),
                             reads=["C_all", "baseE"], writes=["C_all"])
                        for r, OH in enumerate((OH1, OH2)):
                            S.op("dve", lambda OH=OH: V.tensor_tensor(out=prod[:], in0=C_all[:], in1=OH[:], op=ALU.mult), reads=["C_all"], writes=["prodE"])
                            S.op("dve", lambda r=r: V.tensor_reduce(out=POS[:, :, r], in_=prod[:], axis=AX.X, op=ALU.add), reads=["prodE"], writes=["POS"])
                        S.op("dve", lambda: V.tensor_copy(out=POSI[:], in_=POS[:]), reads=["POS"], writes=["POSI"])
                        S.op("dve", lambda: V.memset(REC[:], 0), writes=["REC"])
                        for r in range(2):
                            S.op("dve", lambda r=r: V.tensor_copy(out=REC[:, :, r, 0], in_=tokid[:]), reads=["tokid", "REC"], writes=["REC"])
                            S.op("dve", lambda r=r: V.tensor_scalar_add(out=POS[:, :, r], in0=tokid[:], scalar1=float(r * NTOK)), reads=["tokid", "POSI"], writes=["POS"])
                            S.op("dve", lambda r=r: V.tensor_copy(out=REC[:, :, r, 1], in_=POS[:, :, r]), reads=["POS", "REC"], writes=["REC"])
                            S.op("dve", lambda r=r: V.tensor_copy(out=REC[:, :, r, 2].bitcast(F32), in_=W12[:, :, r]), reads=["REC"], writes=["REC"])
                        ev_sc = []
                        for i in range(NTILE):
                            for r in range(2):
                                S.idma("pool", lambda: P.indirect_dma_start(out=rec_s, out_offset=bass.IndirectOffsetOnAxis(ap=POSI[:, i, r:r + 1], axis=0), in_=REC[:, i, r, :], in_offset=None),
                                       reads=["REC", "POSI", "rec_s"], writes=[("rec_sc", i, r)])
                        S.op("dve", lambda: V.tensor_tensor(out=cmps[:], in0=inclp[:, :].unsqueeze(1).to_broadcast([128, NSLOT, NE]),
                                                            in1=slotst[:, :].unsqueeze(2).to_broadcast([128, NSLOT, NE]), op=ALU.is_le),
                             reads=["inclp", "slotst"], writes=["cmps"])
                        S.op("dve", lambda: V.tensor_reduce(out=slot_e[:], in_=cmps[:], axis=AX.X, op=ALU.add), reads=["cmps"], writes=["slot_e"])
                        S.op("dve", lambda: V.tensor_scalar_min(out=slot_e[:], in0=slot_e[:], scalar1=float(NE - 1)), reads=["slot_e"], writes=["slot_e"])
                        S.op("dve", lambda: V.tensor_tensor(out=WIf[:], in0=slot_e[:, :].unsqueeze(2).to_broadcast([128, NSLOT, 6]),
                                                            in1=mulc[:, :].unsqueeze(1).to_broadcast([128, NSLOT, 6]), op=ALU.mult),
                             reads=["slot_e", "mulc"], writes=["WIf"])
                        S.op("dve", lambda: V.tensor_tensor(out=WIf[:], in0=WIf[:], in1=addc[:, :].unsqueeze(1).to_broadcast([128, NSLOT, 6]), op=ALU.add),
                             reads=["WIf", "addc"], writes=["WIf"])
                        S.op("dve", lambda: V.tensor_copy(out=WI[:], in_=WIf[:]), reads=["WIf"], writes=["WI"])
                        S.barrier()

                with ExitStack() as ph:
                    if "2" in esub:
                        def sb(name, shape, dt):
                            return ph.enter_context(nc.sbuf_tensor(name + '_e2', shape, dt))

                        def ps(name, shape, dt=F32):
                            return ph.enter_context(nc.psum_tensor(name + '_e2', shape, dt))

                        NB2 = 2
                        recb = [sb(f"recb{i}", [128, 4, 4], I32) for i in range(NB2)]
                        hng = [[sb(f"hng{i}_{j}", [128, D], BF16) for j in range(4)] for i in range(NB2)]
                        hnTs = [sb(f"hnTs{i}", [128, 8, 512], BF16) for i in range(NB2)]
                        wgb = [sb(f"wgb{i}", [128, 8, DE], BF16) for i in range(NB2)]
                        wub = [sb(f"wub{i}", [128, 8, DE], BF16) for i in range(NB2)]
                        wdb = [sb(f"wdb{i}", [128, 4, D], BF16) for i in range(NB2)]
                        hid = [sb(f"hidD{i}", [128, 4, 512], BF16) for i in range(2)]
                        sgt = [sb(f"sgD{i}", [128, 512], F32) for i in range(2)]
                        yrow = [sb(f"yrow{i}", [128, D], BF16) for i in range(4)]
                        pA = ps("pA", [128, 1024], F32)
                        pB = ps("pB", [128, 1024], F32)
                        pC = ps("pC", [128, 1024], F32)
                        pD_ = ps("pD", [128, 1024], F32)
                        pCb = pC[:, :].bitcast(BF16)
                        pDb = pD_[:, :].bitcast(BF16)
                        wg_v = w_eg.rearrange("e (p a k) n -> (e p a) (k n)", p=128, a=2)
                        wu_v = w_eu.rearrange("e (p a k) n -> (e p a) (k n)", p=128, a=2)
                        wd_v = w_ed.rearrange("e f n -> (e f) n")
                        rec_scatter_res = [("rec_sc", i, r) for i in range(NTILE) for r in range(2)]

                        def load_slot(s):
                            b = s % NB2
                            S.dma("sp", recb[b][:], rec_s[s * SLOT:(s + 1) * SLOT, :].rearrange("(j p) c -> p j c", p=128), writes=[("recb", b)])
                            for j in range(4):
                                S.idma("pool", lambda: P.indirect_dma_start(out=hng[b][j][:], out_offset=None, in_=hn_s, in_offset=bass.IndirectOffsetOnAxis(ap=recb[b][:, j, 0:1], axis=0)),
                                       reads=[("recb", b)], writes=[("hng", b, j)])
                            for (wt_, src_, nm_) in ((wgb, wbg_s, "wgb"), (wub, wbu_s, "wub"), (wdb, wbd_s, "wdb")):
                                S.idma("pool", lambda wt_=wt_, src_=src_: P.indirect_dma_start(out=wt_[b][:, :, :].rearrange("p k n -> p (k n)"), out_offset=None, in_=src_,
                                                                                              in_offset=bass.IndirectOffsetOnAxis(ap=WI[:, s, 0:1], axis=0)),
                                       reads=["WI"], writes=[(nm_, b)])

                        load_slot(0)
                        yi = 0
                        for s in range(NSLOT):
                            b = s % NB2
                            if s + 1 < NSLOT:
                                load_slot(s + 1)
                            for j in range(4):
                                pbk = pCb[:, (j % 2) * 1024:(j % 2 + 1) * 1024]
                                for k in range(8):
                                    S.op("pe", lambda k=k, j=j, pbk=pbk: T.transpose(pbk[:, k * 128:(k + 1) * 128],
                                                                                    hng[b][j][:, :].rearrange("p (q k) -> p k q", k=8)[:, k, :], ident[:]),
                                         reads=[("hng", b, j), "ident"], writes=[("pC", j % 2)])
                                if j % 2 == 0:
                                    S.op("act", lambda j=j, pbk=pbk: A.copy(out=hnTs[b][:, :, j * 128:(j + 1) * 128], in_=pbk.rearrange("p (k t) -> p k t", k=8)),
                                         reads=[("pC", j % 2)], writes=[("hnTs", b, j)])
                                else:
                                    S.op("dve", lambda j=j, pbk=pbk: V.tensor_copy(out=hnTs[b][:, :, j * 128:(j + 1) * 128], in_=pbk.rearrange("p (k t) -> p k t", k=8)),
                                         reads=[("pC", j % 2)], writes=[("hnTs", b, j)])
                            hnTs_res = [("hnTs", b, j) for j in range(4)]
                            hb = s % 2
                            for c in range(4):
                                pGU = pA if c % 2 == 0 else pB
                                pGUr = "pA" if c % 2 == 0 else "pB"
                                for k in range(8):
                                    S.op("pe", lambda k=k, c=c: T.matmul(pGU[:, 0:512], wgb[b][:, k, c * 128:(c + 1) * 128], hnTs[b][:, k, :], start=(k == 0), stop=(k == 7)),
                                         reads=[("wgb", b)] + hnTs_res, writes=[(pGUr, 0)])
                                for k in range(8):
                                    S.op("pe", lambda k=k, c=c: T.matmul(pGU[:, 512:1024], wub[b][:, k, c * 128:(c + 1) * 128], hnTs[b][:, k, :], start=(k == 0), stop=(k == 7)),
                                         reads=[("wub", b)] + hnTs_res, writes=[(pGUr, 1)])
                                S.op("act", lambda c=c: A.activation(out=sgt[c % 2][:], in_=pGU[:, 0:512], func=AF.Silu), reads=[(pGUr, 0)], writes=[("sgD", c % 2)])
                                S.op("dve", lambda c=c: V.tensor_tensor(out=hid[hb][:, c, :], in0=pGU[:, 512:1024], in1=sgt[c % 2][:], op=ALU.mult),
                                     reads=[(pGUr, 1), ("sgD", c % 2)], writes=[("hid", hb, c)])
                            for j in range(4):
                                pDn = pC if j % 2 == 0 else pD_
                                pDr = "pC" if j % 2 == 0 else "pD"
                                for half in range(2):
                                    for c in range(4):
                                        S.op("pe", lambda c=c, half=half, j=j: T.matmul(pDn[:, half * 512:(half + 1) * 512], hid[hb][:, c, j * 128:(j + 1) * 128],
                                                                                     wdb[b][:, c, half * 512:(half + 1) * 512], start=(c == 0), stop=(c == 3)),
                                             reads=[("hid", hb, c), ("wdb", b)], writes=[(pDr, half)])
                                yb = yi % 4
                                yi += 1
                                if j % 2 == 0:
                                    S.op("act", lambda j=j, yb=yb: A.activation(out=yrow[yb][:], in_=pDn[:, :], func=AF.Copy, scale=recb[b][:, j, 2:3].bitcast(F32)),
                                         reads=[(pDr, 0), (pDr, 1), ("recb", b)], writes=[("yrow", yb)])
                                else:
                                    S.op("dve", lambda j=j, yb=yb: V.tensor_scalar(out=yrow[yb][:], in0=pDn[:, :], scalar1=recb[b][:, j, 2:3].bitcast(F32), scalar2=None, op0=ALU.mult),
                                         reads=[(pDr, 0), (pDr, 1), ("recb", b)], writes=[("yrow", yb)])
                                S.idma("pool", lambda: P.indirect_dma_start(out=ybuf, out_offset=bass.IndirectOffsetOnAxis(ap=recb[b][:, j, 1:2], axis=0), in_=yrow[yb][:], in_offset=None),
                                       reads=[("yrow", yb), ("recb", b)], writes=[("ybuf", s, j)])
                        S.barrier()

                with ExitStack() as ph:
                    if "3" in esub:
                        def sb(name, shape, dt):
                            return ph.enter_context(nc.sbuf_tensor(name + '_e3', shape, dt))

                        def ps(name, shape, dt=F32):
                            return ph.enter_context(nc.psum_tensor(name + '_e3', shape, dt))

                        wpgb = sb("wpgb", [128, 8, D], BF16)
                        wppb = sb("wppb", [128, 2, D], BF16)
                        x1 = sb("x1", [128, 2 * NTI, D], F32)
                        ya = [sb(f"yaE{i}", [128, 2, D], BF16) for i in range(4)]
                        xsb = [sb(f"xsD{i}", [128, D], BF16) for i in range(2)]
                        junk = sb("junkD", [128, D], BF16)
                        ssq = sb("ssqD", [128, NTI], F32)
                        rstd = sb("rstdD", [128, NTI], F32)
                        ssq3 = sb("ssq3D", [128, NTI], F32)
                        rstd3 = sb("rstd3D", [128, NTI], F32)
                        pld = sb("pld", [128, 2 * NTI, PD], F32)
                        pbf = [sb(f"pbf{i}", [128, PD], BF16) for i in range(2)]
                        ptT = [sb(f"ptT{i}", [128, 2, 128], BF16) for i in range(2)]
                        hpT = [sb(f"hpT{i}", [128, 8, 128], BF16) for i in range(2)]
                        sig = [sb(f"sigD{i}", [128, D], F32) for i in range(2)]
                        pA = ps("pA", [128, 1024], F32)
                        pB = ps("pB", [128, 1024], F32)
                        pC = ps("pC", [128, 1024], F32)
                        pD_ = ps("pD", [128, 1024], F32)
                        pCb = pC[:, :].bitcast(BF16)
                        pDb = pD_[:, :].bitcast(BF16)
                        S.dma("pool", wpgb[:], w_pg.rearrange("(k p) n -> p k n", p=128), writes=["wpgb"])
                        S.dma("pool", wppb[:], w_pp.rearrange("(k p) n -> p k n", p=128), writes=["wppb"])

                        xo_box = [0]

                        def sumsq(i, col_t, res):
                            S.op("act", lambda: A.activation(out=junk[:], in_=x1[:, xo_box[0] + i, :], func=AF.Square, accum_out=col_t[:, i:i + 1]),
                                 reads=[("x1", xo_box[0] + i)], writes=["junkD", res])

                        def norm_T(xin_ap, xin_res, rstd_col, rstd_res, gain, gain_res, out_ap, out_res, pbank, pbank_res, xsi):
                            S.op("act", lambda: A.activation(out=xsb[xsi][:], in_=xin_ap, func=AF.Copy, scale=rstd_col),
                                 reads=xin_res + [rstd_res], writes=[("xsD", xsi)])
                            for k in range(8):
                                S.op("pe", lambda k=k: T.transpose(pbank[:, k * 128:(k + 1) * 128], xsb[xsi][:, k * 128:(k + 1) * 128], ident[:]),
                                     reads=[("xsD", xsi), "ident"], writes=[pbank_res])
                            S.op("dve", lambda: V.tensor_tensor(out=out_ap, in0=pbank.rearrange("p (k t) -> p k t", k=8),
                                                                in1=gain[:, :].unsqueeze(2).to_broadcast([128, 8, 128]), op=ALU.mult),
                                 reads=[pbank_res, gain_res], writes=[out_res])

                        yb_v = ybuf[0:2 * NTOK, :].rearrange("(r t) d -> t r d", r=2)

                        def e3_loads(st_):
                            T0_ = st_ * TS
                            xo_ = (st_ % 2) * NTI
                            S.dma("pool", pld[:, xo_:xo_ + NTI, :], p_in[T0_:T0_ + TS, :].rearrange("(j p) n -> p j n", p=128), writes=[("pld", st_ % 2)])
                            for i_ in range(NTI):
                                S.dma("pool", x1[:, xo_ + i_, :], x1_s[T0_ + i_ * 128:T0_ + (i_ + 1) * 128, :], writes=[("x1", xo_ + i_)])

                        def ya_load(g_):
                            S.dma("pool", ya[g_ % 4][:], yb_v[g_ * 128:(g_ + 1) * 128, :, :], writes=[("yaE", g_ % 4)])

                        e3_loads(0)
                        for g_ in range(4):
                            ya_load(g_)
                        for st in range(NST):
                            T0 = st * TS
                            xo = (st % 2) * NTI
                            xo_box[0] = xo
                            if st + 1 < NST:
                                e3_loads(st + 1)
                            for i in range(NTI):
                                b2 = i % 2
                                yq = (st * NTI + i) % 4
                                S.op("dve", lambda i=i: V.tensor_tensor(out=x1[:, xo + i, :], in0=x1[:, xo + i, :], in1=ya[yq][:, 0, :], op=ALU.add),
                                     reads=[("x1", xo + i), ("yaE", yq)], writes=[("x1", xo + i)])
                                S.op("dve", lambda i=i: V.tensor_tensor(out=x1[:, xo + i, :], in0=x1[:, xo + i, :], in1=ya[yq][:, 1, :], op=ALU.add),
                                     reads=[("x1", xo + i), ("yaE", yq)], writes=[("x1", xo + i)])
                                if st * NTI + i + 4 < NTOK // 128:
                                    ya_load(st * NTI + i + 4)
                                sumsq(i, ssq, "ssqD")
                            rstd_from_ssq(ssq[:], rstd[:], D, ["ssqD"], ["rstdD"])
                            def e3_front(i):
                                b2 = i % 2
                                pbk = pCb[:, b2 * 1024:(b2 + 1) * 1024]
                                norm_T(x1[:, xo + i, :], [("x1", xo + i)], rstd[:, i:i + 1], "rstdD", gple, "gple",
                                       hpT[b2][:], ("hpT", b2), pbk, ("pC", b2), b2)
                                S.op("act", lambda i=i: A.copy(out=pbf[b2][:], in_=pld[:, xo + i, :]), reads=[("pld", st % 2)], writes=[("pbf", b2)])
                                for k in range(2):
                                    S.op("pe", lambda k=k: T.transpose(pDb[:, b2 * 1024 + k * 128: b2 * 1024 + (k + 1) * 128], pbf[b2][:, k * 128:(k + 1) * 128], ident[:]),
                                         reads=[("pbf", b2), "ident"], writes=[("pD", b2)])
                                S.op("act", lambda: A.copy(out=ptT[b2][:], in_=pDb[:, b2 * 1024: b2 * 1024 + 256].rearrange("p (k t) -> p k t", k=2)),
                                     reads=[("pD", b2)], writes=[("ptT", b2)])

                            def e3_back(i):
                                b2 = i % 2
                                for half in range(2):
                                    for k in range(8):
                                        S.op("pe", lambda k=k, half=half: T.matmul(pA[:, half * 512:(half + 1) * 512], hpT[b2][:, k, :], wpgb[:, k, half * 512:(half + 1) * 512],
                                                                                  start=(k == 0), stop=(k == 7)),
                                             reads=[("hpT", b2), "wpgb"], writes=[("pA", half)])
                                for half in range(2):
                                    for k in range(2):
                                        S.op("pe", lambda k=k, half=half: T.matmul(pB[:, half * 512:(half + 1) * 512], ptT[b2][:, k, :], wppb[:, k, half * 512:(half + 1) * 512],
                                                                                  start=(k == 0), stop=(k == 1)),
                                             reads=[("ptT", b2), "wppb"], writes=[("pB", half)])
                                for half in range(2):
                                    hs = slice(half * 512, (half + 1) * 512)
                                    S.op("act", lambda hs=hs: A.activation(out=sig[b2][:, hs], in_=pA[:, hs], func=AF.Sigmoid), reads=[("pA", half)], writes=[("sigD", b2, half)])
                                    S.op("dve", lambda hs=hs: V.tensor_tensor(out=sig[b2][:, hs], in0=pB[:, hs], in1=sig[b2][:, hs], op=ALU.mult),
                                         reads=[("pB", half), ("sigD", b2, half)], writes=[("sigD", b2, half)])
                                S.op("dve", lambda i=i: V.tensor_tensor(out=x1[:, xo + i, :], in0=sig[b2][:], in1=x1[:, xo + i, :], op=ALU.add),
                                     reads=[("sigD", b2, 0), ("sigD", b2, 1), ("x1", xo + i)], writes=[("x1", xo + i)])
                                sumsq(i, ssq3, "ssq3D")

                            e3_front(0)
                            for i in range(NTI):
                                if i + 1 < NTI:
                                    e3_front(i + 1)
                                e3_back(i)
                            rstd_from_ssq(ssq3[:], rstd3[:], D, ["ssq3D"], ["rstd3D"])
                            for i in range(NTI):
                                b2 = i % 2
                                S.op("dve", lambda i=i: V.scalar_tensor_tensor(out=x1[:, xo + i, :], in0=x1[:, xo + i, :], scalar=rstd3[:, i:i + 1], in1=gfin[:], op0=ALU.mult, op1=ALU.mult),
                                     reads=[("x1", xo + i), "rstd3D", "gfin"], writes=[("x1", xo + i)])
                                S.dma("sp", y[T0 + i * 128:T0 + (i + 1) * 128, :], x1[:, xo + i, :], reads=[("x1", xo + i)], writes=[("y", st, i)])
                        S.barrier()

        S.finish("sp")
        build.stats = dict(cnt=dict(S.cnt), waits=S.n_wait)
    return nc


_CONST = {}


def _consts():
    if _CONST:
        return _CONST
    bf = ml_dtypes.bfloat16
    idx = np.arange(128)
    same = (idx[:, None] // 64) == (idx[None, :] // 64)
    _CONST["c_ident"] = np.eye(128, dtype=np.float32).astype(bf)
    _CONST["c_maskf"] = (same & (idx[:, None] <= idx[None, :])).astype(np.float32).astype(bf)
    _CONST["c_maskb"] = (same & (idx[:, None] >= idx[None, :])).astype(np.float32).astype(bf)
    ang = 2.0 * np.pi * ((idx[:, None] * idx[None, :]) % 128) / 128.0
    _CONST["c_cc"] = np.concatenate([np.cos(ang), np.sin(ang)], axis=1).astype(np.float32).astype(bf)
    n = np.arange(NTAB, dtype=np.int64)
    m = (n[:, None] * n[None, :]) % NTAB
    tab = np.cos(2.0 * np.pi * np.arange(NTAB) / NTAB)
    tabs = -np.sin(2.0 * np.pi * np.arange(NTAB) / NTAB)
    _CONST["c_ctab"] = tab[m].astype(np.float32).astype(bf)
    _CONST["c_stab"] = tabs[m].astype(np.float32).astype(bf)
    return _CONST


def _consts_n(NTOK):
    bf = ml_dtypes.bfloat16
    NSLOT = (2 * NTOK) // SLOT + NE
    NPOS = NSLOT * SLOT
    NTILE = NTOK // 128
    c = {}
    pos = np.arange(NPOS)
    rp = np.zeros((NPOS, 4), np.int32)
    rp[:, 1] = 2 * NTOK + (pos % SLOT)
    c["c_recpad"] = rp
    idx = np.arange(128)
    c["c_L"] = (idx[:, None] < idx[None, :]).astype(np.float32).astype(bf)
    c["c_ones"] = np.ones((128, 128), np.float32).astype(bf)
    m = np.ones((NE, NTILE), np.float32)
    m[:, 0] = 0.0
    c["c_mask96"] = np.ascontiguousarray(np.broadcast_to(m.reshape(1, -1), (128, NE * NTILE)))
    c["c_tokid"] = (np.arange(NTILE)[None, :] * 128 + idx[:, None]).astype(np.float32)
    c["c_slotstart"] = np.ascontiguousarray(np.broadcast_to((np.arange(NSLOT) * SLOT).astype(np.float32)[None, :], (128, NSLOT)))
    c["c_thr"] = np.ascontiguousarray(np.broadcast_to((np.arange(NTOK // SLOT) * SLOT).astype(np.float32)[None, :], (128, NTOK // SLOT)))
    addc = np.stack([idx, idx, idx, idx, idx, idx], axis=1).astype(np.float32)
    c["c_addc"] = addc
    c["c_mulc"] = np.ascontiguousarray(np.broadcast_to(np.array([128, 128, 128, 128, 128, 128], np.float32)[None, :], (128, 6)))
    return c


_WNAMES = ["norm_mix", "w_in", "w_fourier", "lb_logits", "norm_o", "w_out", "norm_ffn", "w_route_group", "w_route_expert",
           "w_exp_gate", "w_exp_up", "w_exp_down", "norm_ple", "w_ple_gate", "w_ple_proj", "norm_final"]


def _prep_weights(inp):
    f = lambda a: np.ascontiguousarray(np.asarray(a, dtype=np.float32))
    return {
        "norm_mix": f(inp["norm_mix"]).reshape(D),
        "w_in": f(inp["w_in"]).reshape(D, INW),
        "w_fourier": f(inp["w_fourier"]).reshape(4, 128, 128),
        "lb_logits": f(inp["lb_logits"]).reshape(2, 1024),
        "norm_o": f(inp["norm_o"]).reshape(HD),
        "w_out": f(inp["w_out"]).reshape(D, D),
        "norm_ffn": f(inp["norm_ffn"]).reshape(D),
        "w_route_group": f(inp["w_route_group"]).reshape(D, 4),
        "w_route_expert": f(inp["w_route_expert"]).reshape(D, NE),
        "w_exp_gate": f(inp["w_exp_gate"]).reshape(NE, D, DE),
        "w_exp_up": f(inp["w_exp_up"]).reshape(NE, D, DE),
        "w_exp_down": f(inp["w_exp_down"]).reshape(NE, DE, D),
        "norm_ple": f(inp["norm_ple"]).reshape(D),
        "w_ple_gate": f(inp["w_ple_gate"]).reshape(D, D),
        "w_ple_proj": f(inp["w_ple_proj"]).reshape(PD, D),
        "norm_final": f(inp["norm_final"]).reshape(D),
    }


_NC_CACHE = {}


def kernel(**inputs):
    ncores = 8
    xp = np.asarray(inputs["x_prompt"], dtype=np.float32)
    xs = np.asarray(inputs["x_sample"], dtype=np.float32)
    pp = np.asarray(inputs["p_prompt"], dtype=np.float32)[0]
    psm = np.asarray(inputs["p_sample"], dtype=np.float32)[0]
    B, SQ, _ = xp.shape
    DB, DS, _ = xs.shape
    npc = B // ncores
    nsc = DB // ncores
    seq_lens = tuple([SQ] * npc + [DS] * nsc)
    if seq_lens not in _NC_CACHE:
        _NC_CACHE[seq_lens] = build(list(seq_lens))
    nc = _NC_CACHE[seq_lens]
    w = _prep_weights(inputs)
    cst = dict(_consts())
    cst.update(_consts_n(sum(seq_lens)))
    in_maps = []
    for c in range(ncores):
        xc = np.concatenate([xp[c * npc:(c + 1) * npc].reshape(-1, D), xs[c * nsc:(c + 1) * nsc].reshape(-1, D)], axis=0)
        pc = np.concatenate([pp[c * npc:(c + 1) * npc].reshape(-1, PD), psm[c * nsc:(c + 1) * nsc].reshape(-1, PD)], axis=0)
        m = {"x": np.ascontiguousarray(xc), "p": np.ascontiguousarray(pc)}
        m.update(w)
        m.update(cst)
        in_maps.append(m)
    res = run_bass_kernel_spmd(nc, in_maps, core_ids=list(range(ncores)))
    yp = np.empty((B, SQ, D), np.float32)
    ys = np.empty((DB, DS, D), np.float32)
    for c in range(ncores):
        yc = np.asarray(res.results[c]["y"], dtype=np.float32)
        yp[c * npc:(c + 1) * npc] = yc[:npc * SQ].reshape(npc, SQ, D)
        ys[c * nsc:(c + 1) * nsc] = yc[npc * SQ:].reshape(nsc, DS, D)
    return (yp, ys)
```

```python
import numpy as np
import ml_dtypes
from contextlib import ExitStack
import concourse.bass as bass
import concourse.mybir as mybir
from concourse.bass_utils import run_bass_kernel_spmd

F32 = mybir.dt.float32
BF16 = mybir.dt.bfloat16
I32 = mybir.dt.int32
AF = mybir.ActivationFunctionType
ALU = mybir.AluOpType
AX = mybir.AxisListType

D = 1024
NH = 4
HD = 128
NE = 32
DE = 512
PD = 256
INW = 3072
EPS = 1e-6
NTAB = 4096
BIG = 1.0e30


class Syn:
    EPOCH = 30000

    def __init__(self, nc, es, n_dma_sems=32):
        self.nc = nc
        self.es = es
        self.eng = {"pe": nc.tensor, "dve": nc.vector, "act": nc.scalar, "pool": nc.gpsimd, "sp": nc.sync}
        self.cnt = {e: 0 for e in self.eng}
        self.sems = {e: [] for e in self.eng}
        self.know = {e: {} for e in self.eng}
        self.snap = {}
        self.last_w = {}
        self.readers = {}
        self.dma_sems = [es.enter_context(nc.semaphore(f"dq{i}")) for i in range(n_dma_sems)]
        self.dma_val = [0] * n_dma_sems
        self.dma_last_ev = [None] * n_dma_sems
        self.dma_rr = 0
        self.n_wait = 0

    def _sem_for(self, e, count):
        ep = (count - 1) // self.EPOCH
        while len(self.sems[e]) <= ep:
            self.sems[e].append(self.es.enter_context(self.nc.semaphore(f"c_{e}_{len(self.sems[e])}")))
        return self.sems[e][ep], count - ep * self.EPOCH

    def _known(self, e, ev):
        return self.know[e].get((ev[0], ev[1]), 0) >= ev[2]

    def _learn(self, e, ev):
        k = self.know[e]
        key = (ev[0], ev[1])
        if k.get(key, 0) < ev[2]:
            k[key] = ev[2]
        sn = self.snap.get(ev)
        if sn:
            for kk, vv in sn.items():
                if k.get(kk, 0) < vv:
                    k[kk] = vv

    def _wait(self, e, ev):
        if ev is None or self._known(e, ev):
            return
        if ev[0] == "e":
            sem, val = self._sem_for(ev[1], ev[2])
            self.eng[e].wait_ge(sem, val)
        else:
            self.eng[e].wait_ge(self.dma_sems[ev[1]], ev[2])
        self.n_wait += 1
        self._learn(e, ev)

    PSUM_NAMES = {"pSx", "pSy", "pj", "ptrA", "pkA", "pfB", "pscC", "poC", "pkvC", "ptrC", "pA", "pB", "pC", "pD"}

    def _is_ps(self, r):
        return (r[0] if isinstance(r, tuple) else r) in self.PSUM_NAMES

    def _deps(self, e, reads, writes):
        evs = []
        for r in reads:
            ev = self.last_w.get(r)
            ps = self._is_ps(r)
            if ev is not None and not (ps and ev[1] == e):
                evs.append(ev)
            if ps:
                evs.extend(x for x in self.readers.get(r, ()) if x[1] != e)
        for w in writes:
            ps = self._is_ps(w)
            ev = self.last_w.get(w)
            if ev is not None and not (ps and ev[1] == e):
                evs.append(ev)
            evs.extend(x for x in self.readers.get(w, ()) if not (ps and x[1] == e))
        for ev in evs:
            if ev[0] == "e" and ev[1] == "pe" and e == "pe":
                continue
            self._wait(e, ev)

    def _record(self, ev, reads, writes):
        for r in reads:
            self.readers.setdefault(r, []).append(ev)
        for w in writes:
            self.last_w[w] = ev
            self.readers[w] = []

    def op(self, e, fn, reads=(), writes=()):
        self._deps(e, reads, writes)
        inst = fn()
        self.cnt[e] += 1
        c = self.cnt[e]
        sem, _ = self._sem_for(e, c)
        inst.then_inc(sem, 1)
        ev = ("e", e, c)
        self.snap[ev] = dict(self.know[e])
        self._record(ev, reads, writes)
        return ev

    def dma(self, q, out, in_, reads=(), writes=(), **kw):
        self._deps(q, reads, writes)
        i = self.dma_rr
        self.dma_rr = (self.dma_rr + 1) % len(self.dma_sems)
        self._wait(q, self.dma_last_ev[i])
        inst = self.eng[q].dma_start(out=out, in_=in_, **kw)
        self.dma_val[i] += 16
        inst.then_inc(self.dma_sems[i], 16)
        ev = ("d", i, self.dma_val[i])
        self.snap[ev] = dict(self.know[q])
        self.dma_last_ev[i] = ev
        self._record(ev, reads, writes)
        return ev

    def idma(self, q, fn, reads=(), writes=()):
        self._deps(q, reads, writes)
        i = self.dma_rr
        self.dma_rr = (self.dma_rr + 1) % len(self.dma_sems)
        self._wait(q, self.dma_last_ev[i])
        inst = fn()
        self.dma_val[i] += 16
        inst.then_inc(self.dma_sems[i], 16)
        ev = ("d", i, self.dma_val[i])
        self.snap[ev] = dict(self.know[q])
        self.dma_last_ev[i] = ev
        self._record(ev, reads, writes)
        return ev

    def barrier(self):
        for e in self.eng:
            for ev in self.dma_last_ev:
                self._wait(e, ev)
            for f in self.eng:
                if f != e and self.cnt[f] > 0:
                    self._wait(e, ("e", f, self.cnt[f]))
        self.last_w.clear()
        self.readers.clear()

    def finish(self, q="sp"):
        for ev in self.dma_last_ev:
            self._wait(q, ev)
        for e in self.eng:
            if self.cnt[e] > 0 and e != q:
                self._wait(q, ("e", e, self.cnt[e]))


SLOT = 512


def build(seq_lens, debug=False, phases="ABCE"):
    NTOK = sum(seq_lens)
    NSLOT = (2 * NTOK) // SLOT + NE
    NPOS = NSLOT * SLOT
    NTILE_ = NTOK // 128
    assert NTOK % 1024 == 0 and all(s % 512 == 0 for s in seq_lens)
    offs = [sum(seq_lens[:i]) for i in range(len(seq_lens))]
    nc = bass.Bass("TRN2", target_bir_lowering=False)

    def din(name, shape, dt=F32):
        return nc.dram_tensor(name, shape, dt, kind="ExternalInput").ap()

    x = din("x", [NTOK, D])
    p_in = din("p", [NTOK, PD])
    norm_mix = din("norm_mix", [D])
    w_in = din("w_in", [D, INW])
    w_four = din("w_fourier", [4, 128, 128])
    lb_logits = din("lb_logits", [2, 2 * 512])
    norm_o = din("norm_o", [HD])
    w_out = din("w_out", [D, D])
    norm_ffn = din("norm_ffn", [D])
    w_rg = din("w_route_group", [D, 4])
    w_re = din("w_route_expert", [D, NE])
    w_eg = din("w_exp_gate", [NE, D, DE])
    w_eu = din("w_exp_up", [NE, D, DE])
    w_ed = din("w_exp_down", [NE, DE, D])
    norm_ple = din("norm_ple", [D])
    w_pg = din("w_ple_gate", [D, D])
    w_pp = din("w_ple_proj", [PD, D])
    norm_final = din("norm_final", [D])
    c_ident = din("c_ident", [128, 128], BF16)
    c_maskf = din("c_maskf", [128, 128], BF16)
    c_maskb = din("c_maskb", [128, 128], BF16)
    c_cc = din("c_cc", [128, 256], BF16)
    c_ctab = din("c_ctab", [NTAB, NTAB], BF16)
    c_stab = din("c_stab", [NTAB, NTAB], BF16)
    c_recpad = din("c_recpad", [NPOS, 4], I32)
    c_L = din("c_L", [128, 128], BF16)
    c_ones = din("c_ones", [128, 128], BF16)
    c_mask96 = din("c_mask96", [128, NE * NTILE_], F32)
    c_tokid = din("c_tokid", [128, NTILE_], F32)
    c_slotstart = din("c_slotstart", [128, NSLOT], F32)
    c_thr = din("c_thr", [128, NTOK // SLOT], F32)
    c_addc = din("c_addc", [128, 6], F32)
    c_mulc = din("c_mulc", [128, 6], F32)

    y = nc.dram_tensor("y", [NTOK, D], F32, kind="ExternalOutput").ap()

    skind = "ExternalOutput" if debug else "Internal"

    def dscr(name, shape, dt=BF16):
        return nc.dram_tensor(name, shape, dt, kind=skind).ap()

    ab_s = dscr("ab_s", [NTOK, 1024])
    qd_s = dscr("qd_s", [2, NH, 128, NTOK])
    kd_s = dscr("kd_s", [2, NH, 128, NTOK])
    k2_s = dscr("k2_s", [NTOK, 1024])
    v_s = dscr("v_s", [NTOK, 512])
    og_s = dscr("og_s", [NTOK, 512])
    mixT_s = dscr("mixT_s", [D, NTOK])
    x1_s = dscr("x1_s", [NTOK, D], F32)
    hn_s = dscr("hn_s", [NTOK, D])
    rec_s = dscr("rec_s", [NPOS, 4], I32)
    ybuf = dscr("ybuf", [2 * NTOK + SLOT, D])
    wbg_s = dscr("wbg_s", [NE * 128, 8 * DE])
    wbu_s = dscr("wbu_s", [NE * 128, 8 * DE])
    wbd_s = dscr("wbd_s", [NE * 128, 4 * D])
    dbg_ofwd = dscr("dbg_ofwd", [NTOK, 512]) if debug else None
    dbg_scm = dscr("dbg_scm", [128, 512]) if debug else None
    dbg_kv = nc.dram_tensor("dbg_kv", [128, 512], F32, kind="ExternalOutput").ap() if debug else None

    NCH = NTOK // 64

    with ExitStack() as es:
        def sbg(name, shape, dt):
            return es.enter_context(nc.sbuf_tensor(name, shape, dt))

        es.enter_context(nc.Block())
        S = Syn(nc, es)
        V, A, P, T = nc.vector, nc.scalar, nc.gpsimd, nc.tensor

        ident = sbg("ident", [128, 128], BF16)
        maskf = sbg("maskf", [128, 128], BF16)
        maskb = sbg("maskb", [128, 128], BF16)
        gmix = sbg("gmix", [128, 8], F32)
        gffn = sbg("gffn", [128, 8], F32)
        gple = sbg("gple", [128, 8], F32)
        gfin = sbg("gfin", [128, D], F32)
        normo = sbg("normo", [128, 1], F32)
        lbt = sbg("lbt", [128, 8], F32)
        clb = sbg("clb", [128, 8], F32)
        lbtmp = sbg("lbtmp", [128, 16], F32)
        decS = sbg("decS", [128, 2, NH, NCH], F32)
        S.dma("sp", ident[:], c_ident, writes=["ident"])
        S.dma("sp", maskf[:], c_maskf, writes=["maskf"])
        S.dma("sp", maskb[:], c_maskb, writes=["maskb"])
        for (t_, src, nm) in ((gmix, norm_mix, "gmix"), (gffn, norm_ffn, "gffn"), (gple, norm_ple, "gple")):
            S.dma("sp", t_[:], src.rearrange("(k p) -> p k", p=128), writes=[nm], allow_slow_non_contiguous=True)
        S.dma("sp", gfin[:], norm_final.partition_broadcast(128), writes=["gfin"])
        S.dma("sp", normo[:], norm_o.rearrange("(p o) -> p o", o=1), writes=["normo"])
        S.dma("sp", lbtmp[:, 0:8], lb_logits[0].rearrange("(a p) -> p a", p=128), writes=["lbtmp"], allow_slow_non_contiguous=True)
        S.dma("sp", lbtmp[:, 8:16], lb_logits[1].rearrange("(a p) -> p a", p=128), writes=["lbtmp"], allow_slow_non_contiguous=True)
        S.op("dve", lambda: V.tensor_sub(out=lbt[:], in0=lbtmp[:, 8:16], in1=lbtmp[:, 0:8]), reads=["lbtmp"], writes=["lbt"])
        S.op("act", lambda: A.activation(out=lbt[:], in_=lbt[:], func=AF.Exp), reads=["lbt"], writes=["lbt"])
        S.op("dve", lambda: V.tensor_scalar_add(out=lbt[:], in0=lbt[:], scalar1=1.0), reads=["lbt"], writes=["lbt"])
        S.op("dve", lambda: V.reciprocal(out=lbt[:], in_=lbt[:]), reads=["lbt"], writes=["lbt"])
        S.op("act", lambda: A.activation(out=clb[:], in_=lbt[:], func=AF.Ln, scale=-1.0, bias=1.0), reads=["lbt"], writes=["clb"])

        def rstd_from_ssq(ssq_ap, out_ap, n, res_r, res_w):
            S.op("act", lambda: A.activation(out=out_ap, in_=ssq_ap, func=AF.Ln, scale=1.0 / n, bias=EPS), reads=res_r, writes=res_w)
            S.op("act", lambda: A.activation(out=out_ap, in_=out_ap, func=AF.Exp, scale=-0.5), reads=res_w, writes=res_w)

        if "A" in phases:
            with ExitStack() as ph:
                def sb(name, shape, dt):
                    return ph.enter_context(nc.sbuf_tensor(name, shape, dt))

                def ps(name, shape, dt=F32):
                    return ph.enter_context(nc.psum_tensor(name, shape, dt))

                winb = sb("winb", [128, 8, INW], BF16)
                mcs = sb("mcs", [128, 4, 256], BF16)
                wfb = sb("wfb", [128, 4, 128], BF16)
                ccb = sb("ccb", [128, 256], BF16)
                scanmask = sb("scanmask", [128, 512], F32)
                xg = [sb(f"xg{i}", [128, 4, D], F32) for i in range(2)]
                xs = [sb(f"xs{i}", [128, 4, D], BF16) for i in range(2)]
                junk = sb("junkA", [128, D], BF16)
                ssq = sb("ssqA", [128, 8], F32)
                rstd = sb("rstdA", [128, 8], F32)
                hT = [sb(f"hT{i}", [128, 8, 512], BF16) for i in range(2)]
                uT = sb("uT", [128, 4, 512], BF16)
                abst = sb("abst", [128, 4, 1024], BF16)
                vst = sb("vst", [128, 4, 512], BF16)
                ogst = sb("ogst", [128, 4, 512], BF16)
                k2T = sb("k2T", [128, 8, 512], BF16)
                k2st = sb("k2st", [128, 4, 1024], BF16)
                qdst = [sb(f"qdst{i}", [128, 512], BF16) for i in range(4)]
                kdst = [sb(f"kdst{i}", [128, 512], BF16) for i in range(4)]
                NTMP = 2
                tm = [{n: sb(f"t_{n}{i}", [128, 512], F32) for n in ("e", "L1", "L2", "P", "lk", "a1", "a2", "Eb")} for i in range(NTMP)]
                ptr = [ps(f"ptrA{i}", [128, 1024], BF16) for i in range(2)]
                pj = [ps(f"pjA{i}", [128, 512], F32) for i in range(5)]
                pk = ps("pkA", [128, 1024], BF16)

                for c in range(4):
                    S.dma("pool", winb[:, :, c * 768:(c + 1) * 768],
                          w_in[:, c * 768:(c + 1) * 768].rearrange("(k p) n -> p k n", p=128), writes=[("winb", c)])
                winb_res = [("winb", c) for c in range(4)]
                S.dma("pool", wfb[:], w_four.rearrange("g c d -> c g d"), writes=["wfb"])
                S.dma("sp", ccb[:], c_cc, writes=["ccb"])
                for g in range(4):
                    for hh in range(2):
                        S.op("pe", lambda g=g, hh=hh: T.matmul(pj[hh][:, g * 128:(g + 1) * 128],
                                                                ccb[:, hh * 128:(hh + 1) * 128], wfb[:, g, :], start=True, stop=True),
                             reads=["ccb", "wfb"], writes=[("pj", hh)])
                for hh in range(2):
                    S.op("dve", lambda hh=hh: V.tensor_copy(out=mcs[:, :, hh * 128:(hh + 1) * 128],
                                                            in_=pj[hh][:, :].rearrange("p (g d) -> p g d", g=4)),
                         reads=[("pj", hh)], writes=["mcs"])
                S.op("dve", lambda: V.memset(scanmask[:], 1.0), writes=["scanmask"])
                S.op("dve", lambda: V.memset(scanmask[:].rearrange("p (n c) -> p n c", c=64)[:, :, 0:1], 0.0), reads=["scanmask"], writes=["scanmask"])

                if "E" in phases:
                    for e in range(NE):
                        S.dma("pool", wbg_s[e * 128:(e + 1) * 128, :], w_eg[e].rearrange("(p k) n -> p (k n)", p=128), writes=[("wbg_s", e)])
                        S.dma("pool", wbu_s[e * 128:(e + 1) * 128, :], w_eu[e].rearrange("(p k) n -> p (k n)", p=128), writes=[("wbu_s", e)])
                        S.dma("pool", wbd_s[e * 128:(e + 1) * 128, :].rearrange("p (k n) -> p k n", k=4), w_ed[e].rearrange("(k p) n -> p k n", p=128),
                              writes=[("wbd_s", e)])
                pj_rr = [0]

                def next_pj():
                    i = pj_rr[0]
                    pj_rr[0] = (i + 1) % 5
                    return i

                NG = NTOK // 512
                ev_i = 0

                def a_load(G_):
                    S.dma("sp", xg[G_ % 2][:], x[G_ * 512:(G_ + 1) * 512, :].rearrange("(j p) d -> p j d", p=128), writes=[("xg", G_ % 2)])

                def a_front(G_):
                    b_ = G_ % 2
                    for j in range(4):
                        S.op("act", lambda j=j: A.activation(out=junk[:], in_=xg[b_][:, j, :], func=AF.Square, accum_out=ssq[:, b_ * 4 + j:b_ * 4 + j + 1]),
                             reads=[("xg", b_)], writes=["junkA", ("ssqA", b_)])
                    rstd_from_ssq(ssq[:, b_ * 4:(b_ + 1) * 4], rstd[:, b_ * 4:(b_ + 1) * 4], D, [("ssqA", b_)], [("rstdA", b_)])
                    for j in range(4):
                        if j % 2 == 0:
                            S.op("act", lambda j=j: A.activation(out=xs[b_][:, j, :], in_=xg[b_][:, j, :], func=AF.Copy, scale=rstd[:, b_ * 4 + j:b_ * 4 + j + 1]),
                                 reads=[("xg", b_), ("rstdA", b_)], writes=[("xs", b_, j)])
                        else:
                            S.op("dve", lambda j=j: V.tensor_scalar(out=xs[b_][:, j, :], in0=xg[b_][:, j, :], scalar1=rstd[:, b_ * 4 + j:b_ * 4 + j + 1], scalar2=None, op0=ALU.mult),
                                 reads=[("xg", b_), ("rstdA", b_)], writes=[("xs", b_, j)])
                    for k in range(8):
                        pt = ptr[(k // 2) % 2]
                        half = k % 2
                        for j in range(4):
                            S.op("pe", lambda j=j, k=k, pt=pt, half=half: T.transpose(pt[:, half * 512 + j * 128: half * 512 + (j + 1) * 128],
                                                                                      xs[b_][:, j, k * 128:(k + 1) * 128], ident[:]),
                                 reads=[("xs", b_, j), "ident"], writes=[("ptrA", (k // 2) % 2)])
                        if k % 2 == 0:
                            S.op("dve", lambda k=k, pt=pt, half=half: V.tensor_scalar(out=hT[b_][:, k, :], in0=pt[:, half * 512:(half + 1) * 512],
                                                                                      scalar1=gmix[:, k:k + 1], scalar2=None, op0=ALU.mult),
                                 reads=[("ptrA", (k // 2) % 2), "gmix"], writes=[("hT", b_, k)])
                        else:
                            S.op("act", lambda k=k, pt=pt, half=half: A.activation(out=hT[b_][:, k, :], in_=pt[:, half * 512:(half + 1) * 512],
                                                                                   func=AF.Copy, scale=gmix[:, k:k + 1]),
                                 reads=[("ptrA", (k // 2) % 2), "gmix"], writes=[("hT", b_, k)])

                a_load(0)
                if NG > 1:
                    a_load(1)
                a_front(0)
                for G in range(NG):
                    t0 = G * 512
                    b = G % 2
                    if G + 1 < NG:
                        a_front(G + 1)
                    if G + 2 < NG:
                        a_load(G + 2)
                    hT_res = [("hT", b, k) for k in range(8)]

                    def proj_fm(col0, pi):
                        for k in range(8):
                            S.op("pe", lambda k=k: T.matmul(pj[pi][:, :], winb[:, k, col0:col0 + 128], hT[b][:, k, :], start=(k == 0), stop=(k == 7)),
                                 reads=winb_res + hT_res, writes=[("pj", pi)])

                    for g in range(4):
                        pi = next_pj()
                        proj_fm(g * 128, pi)
                        eng = "act" if g % 2 else "dve"
                        if eng == "act":
                            S.op("act", lambda g=g, pi=pi: A.copy(out=uT[:, g, :], in_=pj[pi][:, :]), reads=[("pj", pi)], writes=[("uT", g)])
                        else:
                            S.op("dve", lambda g=g, pi=pi: V.tensor_copy(out=uT[:, g, :], in_=pj[pi][:, :]), reads=[("pj", pi)], writes=[("uT", g)])
                    for j in range(4):
                        for half in range(2):
                            pi = next_pj()
                            for gg in range(2):
                                g = half * 2 + gg
                                S.op("pe", lambda g=g, gg=gg, j=j, pi=pi: T.matmul(pj[pi][:, gg * 256:(gg + 1) * 256], uT[:, g, j * 128:(j + 1) * 128],
                                                                                mcs[:, g, :], start=True, stop=True),
                                     reads=[("uT", g), "mcs"], writes=[("pj", pi)])
                            if half == 0:
                                S.op("act", lambda j=j, half=half, pi=pi: A.copy(out=abst[:, j, half * 512:(half + 1) * 512], in_=pj[pi][:, :]),
                                     reads=[("pj", pi)], writes=[("abst", j, half)])
                            else:
                                S.op("dve", lambda j=j, half=half, pi=pi: V.tensor_copy(out=abst[:, j, half * 512:(half + 1) * 512], in_=pj[pi][:, :]),
                                     reads=[("pj", pi)], writes=[("abst", j, half)])
                    S.dma("sp", ab_s[t0:t0 + 512, :].rearrange("(j p) n -> p j n", p=128), abst[:],
                          reads=[("abst", j, hf) for j in range(4) for hf in range(2)], writes=[("ab_s", G)])

                    for (col0, st, nm) in ((1024, vst, "vst"), (2560, ogst, "ogst")):
                        for j in range(4):
                            pi = next_pj()
                            for k in range(8):
                                S.op("pe", lambda k=k, j=j, pi=pi, col0=col0: T.matmul(pj[pi][:, :], hT[b][:, k, j * 128:(j + 1) * 128],
                                                                                      winb[:, k, col0:col0 + 512], start=(k == 0), stop=(k == 7)),
                                     reads=winb_res + hT_res, writes=[("pj", pi)])
                            if j % 2 == 0:
                                S.op("act", lambda j=j, pi=pi, st=st: A.copy(out=st[:, j, :], in_=pj[pi][:, :]), reads=[("pj", pi)], writes=[(nm, j)])
                            else:
                                S.op("dve", lambda j=j, pi=pi, st=st: V.tensor_copy(out=st[:, j, :], in_=pj[pi][:, :]), reads=[("pj", pi)], writes=[(nm, j)])
                    S.dma("sp", v_s[t0:t0 + 512, :].rearrange("(j p) n -> p j n", p=128), vst[:],
                          reads=[("vst", j) for j in range(4)], writes=[("v_s", G)])
                    S.dma("sp", og_s[t0:t0 + 512, :].rearrange("(j p) n -> p j n", p=128), ogst[:],
                          reads=[("ogst", j) for j in range(4)], writes=[("og_s", G)])

                    for h in range(NH):
                        pq = next_pj()
                        proj_fm(512 + h * 128, pq)
                        pfr = [None, None]
                        pfr[0] = next_pj()
                        proj_fm(1536 + h * 128, pfr[0])
                        pfr[1] = next_pj()
                        proj_fm(2048 + h * 128, pfr[1])
                        def chain(dr):
                            tt = tm[dr]
                            ti = dr
                            R = lambda n: ("tm", ti, n)
                            fr = pj[pfr[dr]]
                            frr = ("pj", pfr[dr])
                            col = dr * 4 + h
                            qs = qdst[(2 * h + dr) % 4]
                            ks = kdst[(2 * h + dr) % 4]
                            qsr = ("qdst", (2 * h + dr) % 4)
                            ksr = ("kdst", (2 * h + dr) % 4)
                            S.op("act", lambda: A.activation(out=tt["e"][:], in_=fr[:, :], func=AF.Exp, scale=-1.0), reads=[frr], writes=[R("e")])
                            yield
                            S.op("act", lambda: A.activation(out=tt["L1"][:], in_=tt["e"][:], func=AF.Ln, bias=1.0), reads=[R("e")], writes=[R("L1")])
                            yield
                            S.op("act", lambda: A.activation(out=tt["L2"][:], in_=tt["e"][:], func=AF.Ln, scale=lbt[:, col:col + 1], bias=1.0),
                                 reads=[R("e"), "lbt"], writes=[R("L2")])
                            yield
                            S.op("dve", lambda: V.tensor_sub(out=tt["L2"][:], in0=tt["L2"][:], in1=tt["L1"][:]), reads=[R("L2"), R("L1")], writes=[R("L2")])
                            yield
                            S.op("dve", lambda: V.tensor_tensor_scan(out=tt["P"][:], data0=scanmask[:], data1=tt["L2"][:], initial=0.0, op0=ALU.mult, op1=ALU.add),
                                 reads=[R("L2"), "scanmask"], writes=[R("P")])
                            yield
                            S.op("dve", lambda: V.scalar_tensor_tensor(out=tt["lk"][:], in0=fr[:, :], scalar=-1.0, in1=tt["L1"][:], op0=ALU.mult, op1=ALU.subtract),
                                 reads=[frr, R("L1")], writes=[R("lk")])
                            yield
                            P3 = tt["P"][:].rearrange("p (n c) -> p n c", c=64)
                            Tb = P3[:, :, 63:64].to_broadcast([128, 8, 64])
                            v3 = lambda n: tt[n][:].rearrange("p (n c) -> p n c", c=64)
                            ch0 = t0 // 64
                            if dr == 0:
                                S.op("dve", lambda: V.tensor_sub(out=tt["a1"][:], in0=tt["lk"][:], in1=tt["P"][:]), reads=[R("lk"), R("P")], writes=[R("a1")])
                                yield
                                S.op("dve", lambda: V.tensor_tensor(out=v3("a2"), in0=v3("a1"), in1=Tb, op=ALU.add), reads=[R("a1"), R("P")], writes=[R("a2")])
                                yield
                                S.op("act", lambda: A.activation(out=tt["Eb"][:], in_=tt["P"][:], func=AF.Exp), reads=[R("P")], writes=[R("Eb")])
                                yield
                            else:
                                S.op("dve", lambda: V.tensor_sub(out=tt["a2"][:], in0=tt["P"][:], in1=tt["L2"][:]), reads=[R("P"), R("L2")], writes=[R("a2")])
                                yield
                                S.op("dve", lambda: V.tensor_tensor(out=v3("Eb"), in0=Tb, in1=v3("a2"), op=ALU.subtract), reads=[R("P"), R("a2")], writes=[R("Eb")])
                                yield
                                S.op("dve", lambda: V.tensor_sub(out=tt["a1"][:], in0=tt["lk"][:], in1=tt["Eb"][:]), reads=[R("lk"), R("Eb")], writes=[R("a1")])
                                yield
                                S.op("dve", lambda: V.tensor_tensor(out=tt["a2"][:], in0=tt["a2"][:], in1=tt["lk"][:], op=ALU.add), reads=[R("a2"), R("lk")], writes=[R("a2")])
                                yield
                                S.op("act", lambda: A.activation(out=tt["Eb"][:], in_=tt["Eb"][:], func=AF.Exp), reads=[R("Eb")], writes=[R("Eb")])
                                yield
                            S.op("act", lambda: A.activation(out=ks[:], in_=tt["a1"][:], func=AF.Exp, bias=clb[:, col:col + 1]), reads=[R("a1"), "clb"], writes=[ksr])
                            yield
                            S.op("act", lambda: A.activation(out=k2T[:, col, :], in_=tt["a2"][:], func=AF.Exp, bias=clb[:, col:col + 1]),
                                 reads=[R("a2"), "clb"], writes=[("k2T", col)])
                            yield
                            S.op("dve", lambda: V.scalar_tensor_tensor(out=qs[:], in0=pj[pq][:, :], scalar=float(HD) ** -0.5, in1=tt["Eb"][:], op0=ALU.mult, op1=ALU.mult),
                                 reads=[("pj", pq), R("Eb")], writes=[qsr])
                            yield
                            S.op("act", lambda: A.activation(out=decS[:, dr, h, ch0:ch0 + 8], in_=P3[:, :, 63], func=AF.Exp), reads=[R("P")], writes=[("decS", G)])
                            yield
                            S.dma("sp", qd_s[dr, h, :, t0:t0 + 512], qs[:], reads=[qsr], writes=[("qd_s", G)])
                            S.dma("sp", kd_s[dr, h, :, t0:t0 + 512], ks[:], reads=[ksr], writes=[("kd_s", G)])

                        gens_ = [chain(0), chain(1)]
                        alive_ = [True, True]
                        while any(alive_):
                            for d_ in range(2):
                                if alive_[d_]:
                                    try:
                                        next(gens_[d_])
                                    except StopIteration:
                                        alive_[d_] = False
                    for j in range(4):
                        for col in range(8):
                            S.op("pe", lambda j=j, col=col: T.transpose(pk[:, col * 128:(col + 1) * 128], k2T[:, col, j * 128:(j + 1) * 128], ident[:]),
                                 reads=[("k2T", col), "ident"], writes=["pkA"])
                        if j % 2 == 0:
                            S.op("dve", lambda j=j: V.tensor_copy(out=k2st[:, j, :], in_=pk[:, :]), reads=["pkA"], writes=[("k2st", j)])
                        else:
                            S.op("act", lambda j=j: A.copy(out=k2st[:, j, :], in_=pk[:, :]), reads=["pkA"], writes=[("k2st", j)])
                    S.dma("sp", k2_s[t0:t0 + 512, :].rearrange("(j p) n -> p j n", p=128), k2st[:],
                          reads=[("k2st", j) for j in range(4)], writes=[("k2_s", G)])
                S.barrier()

        if "B" in phases:
            with ExitStack() as ph:
                def sb(name, shape, dt):
                    return ph.enter_context(nc.sbuf_tensor(name, shape, dt))

                def ps(name, shape, dt=F32):
                    return ph.enter_context(nc.psum_tensor(name, shape, dt))

                NTmax = max(seq_lens) // 128
                abS = sb("abS", [128, NTmax, 1024], BF16)
                ctile = [sb(f"ctile{i}", [128, 16, 512], BF16) for i in range(2)]
                stile = [sb(f"stile{i}", [128, 16, 512], BF16) for i in range(2)]
                yst = [sb(f"ystB{i}", [128, 4, 512], BF16) for i in range(2)]
                pf = [ps(f"pfB{i}", [128, 512], F32) for i in range(8)]
                ti = 0
                cti = 0
                for si, (off, SL) in enumerate(zip(offs, seq_lens)):
                    NT = SL // 128
                    rs = NTAB // SL
                    TC = min(16, NT)
                    nkc = NT // TC
                    for q4 in range(0, NT, 8):
                        n4 = min(8, NT - q4)
                        S.dma("sp", abS[:, q4:q4 + n4, :], ab_s[off + q4 * 128: off + (q4 + n4) * 128, :].rearrange("(j p) n -> p j n", p=128),
                              reads=[("ab_s", g) for g in range(NTOK // 512)] if "A" in phases else [], writes=[("abS", q4 // 8)])
                    abS_res = [("abS", i) for i in range((NT + 7) // 8)]
                    ctab_v = c_ctab.rearrange("(r m) c -> r m c", m=rs)
                    stab_v = c_stab.rearrange("(r m) c -> r m c", m=rs)
                    for ct in range(SL // 512):
                        pset = (cti % 2) * 4
                        for kc in range(nkc):
                            tb = ti % 2
                            ti += 1
                            r0 = kc * TC * 128
                            S.dma("sp", ctile[tb][:, 0:TC, :], ctab_v[r0:r0 + TC * 128, 0, ct * 512:(ct + 1) * 512].rearrange("(j p) n -> p j n", p=128),
                                  writes=[("ctile", tb)])
                            S.dma("pool", stile[tb][:, 0:TC, :], stab_v[r0:r0 + TC * 128, 0, ct * 512:(ct + 1) * 512].rearrange("(j p) n -> p j n", p=128),
                                  writes=[("stile", tb)])
                            for t in range(TC):
                                tt_ = kc * TC + t
                                for g in range(4):
                                    S.op("pe", lambda g=g, t=t, tt_=tt_, tb=tb: T.matmul(pf[pset + g][:, :], abS[:, tt_, g * 256:g * 256 + 128], ctile[tb][:, t, :],
                                                                                   start=(tt_ == 0), stop=False),
                                         reads=abS_res + [("ctile", tb)], writes=[("pfB", pset + g)])
                                    S.op("pe", lambda g=g, t=t, tt_=tt_, tb=tb: T.matmul(pf[pset + g][:, :], abS[:, tt_, g * 256 + 128:g * 256 + 256], stile[tb][:, t, :],
                                                                                   start=False, stop=(tt_ == NT - 1)),
                                         reads=abS_res + [("stile", tb)], writes=[("pfB", pset + g)])
                        yb = cti % 2
                        sc = float((SL * 128.0) ** -0.5)
                        for g in range(4):
                            if g % 2 == 0:
                                S.op("act", lambda g=g: A.activation(out=yst[yb][:, g, :], in_=pf[pset + g][:, :], func=AF.Copy, scale=sc),
                                     reads=[("pfB", pset + g)], writes=[("ystB", yb)])
                            else:
                                S.op("dve", lambda g=g: V.tensor_scalar(out=yst[yb][:, g, :], in0=pf[pset + g][:, :], scalar1=sc, scalar2=None, op0=ALU.mult),
                                     reads=[("pfB", pset + g)], writes=[("ystB", yb)])
                        S.dma("sp", mixT_s[0:512, off + ct * 512: off + (ct + 1) * 512].rearrange("(g p) t -> p g t", p=128), yst[yb][:],
                              reads=[("ystB", yb)], writes=[("mixT_s", "four", si, ct)])
                        cti += 1
                S.barrier()

        if "C" in phases:
            with ExitStack() as ph:
                def sb(name, shape, dt):
                    return ph.enter_context(nc.sbuf_tensor(name, shape, dt))

                def ps(name, shape, dt=F32):
                    return ph.enter_context(nc.psum_tensor(name, shape, dt))

                NTmax = max(seq_lens) // 128
                osto = sb("osto", [128, NTmax, 512], BF16)
                qdb = [[sb(f"qdb{d}_{i}", [128, NH, 512], BF16) for i in range(2)] for d in range(2)]
                kdb = [[sb(f"kdb{d}_{i}", [128, NH, 512], BF16) for i in range(2)] for d in range(2)]
                k2b = [[sb(f"k2b{d}_{i}", [128, 4, 512], BF16) for i in range(2)] for d in range(2)]
                vb = [[sb(f"vb{d}_{i}", [128, 4, 512], BF16) for i in range(2)] for d in range(2)]
                ogb = [[sb(f"ogb{d}_{i}", [128, 4, 512], BF16) for i in range(2)] for d in range(2)]
                S32 = [[[sb(f"S32_{d}_{h}_{i}", [128, 128], F32) for i in range(2)] for h in range(NH)] for d in range(2)]
                Sbf = [[sb(f"Sbf_{d}_{h}", [128, 128], BF16) for h in range(NH)] for d in range(2)]
                scm = [sb(f"scm{d}", [128, 512], BF16) for d in range(2)]
                o32 = [sb(f"o32_{d}", [128, 512], F32) for d in range(2)]
                sg = [sb(f"sgC{d}", [128, 512], F32) for d in range(2)]
                mb = [sb(f"mbC{d}", [128, 512], BF16) for d in range(2)]
                junk = sb("junkC", [128, 128], BF16)
                ssq4 = [sb(f"ssq4_{d}", [128, 4], F32) for d in range(2)]
                mst = [[sb(f"mstC{d}_{i}", [128, NH, 512], BF16) for i in range(2)] for d in range(2)]
                psc = [ps(f"pscC{d}", [128, 512], F32) for d in range(2)]
                po = [ps(f"poC{d}", [128, 512], F32) for d in range(2)]
                pkv = [ps(f"pkvC{d}", [128, 512], F32) for d in range(2)]
                ptr = [ps(f"ptrC{d}", [128, 1024], BF16) for d in range(2)]
                blk_cnt = [0, 0]

                def c_step(dr, off, SL, t, second, cur):
                    NT = SL // 128
                    bi, j = t // 4, t % 4
                    tok0 = off + bi * 512
                    G = tok0 // 512
                    first_in_blk = (j == 0) if dr == 0 else (j == 3)
                    last_in_blk = (j == 3) if dr == 0 else (j == 0)
                    if first_in_blk:
                        blk_cnt[dr] += 1
                        cur["bb"] = blk_cnt[dr] % 2
                        bb = cur["bb"]
                        S.dma("sp", qdb[dr][bb][:], qd_s[dr, :, :, tok0:tok0 + 512].rearrange("h d t -> d h t"), writes=[("qdb", dr, bb)])
                        S.dma("sp", kdb[dr][bb][:], kd_s[dr, :, :, tok0:tok0 + 512].rearrange("h d t -> d h t"), writes=[("kdb", dr, bb)])
                        S.dma("sp", k2b[dr][bb][:], k2_s[tok0:tok0 + 512, dr * 512:(dr + 1) * 512].rearrange("(j p) n -> p j n", p=128), writes=[("k2b", dr, bb)])
                        S.dma("sp", vb[dr][bb][:], v_s[tok0:tok0 + 512, :].rearrange("(j p) n -> p j n", p=128), writes=[("vb", dr, bb)])
                        if (bi * 4 + 3 >= NT // 2) if dr == 0 else (bi * 4 < NT // 2):
                            S.dma("sp", ogb[dr][bb][:], og_s[tok0:tok0 + 512, :].rearrange("(j p) n -> p j n", p=128), writes=[("ogb", dr, bb)])
                    bb = cur["bb"]
                    mask = maskf if dr == 0 else maskb
                    Q, Kd, K2, Vv, OG = qdb[dr][bb], kdb[dr][bb], k2b[dr][bb], vb[dr][bb], ogb[dr][bb]
                    rq, rk, rk2, rv, rog = ("qdb", dr, bb), ("kdb", dr, bb), ("k2b", dr, bb), ("vb", dr, bb), ("ogb", dr, bb)
                    for h in range(NH):
                        S.op("pe", lambda h=h: T.matmul(psc[dr][:, h * 128:(h + 1) * 128], Kd[:, h, j * 128:(j + 1) * 128], Q[:, h, j * 128:(j + 1) * 128], start=True, stop=True),
                             reads=[rk, rq], writes=[("pscC", dr)])
                    yield
                    S.op("dve", lambda: V.tensor_tensor(out=scm[dr][:].rearrange("p (h c) -> p h c", h=NH), in0=psc[dr][:, :].rearrange("p (h c) -> p h c", h=NH),
                                                        in1=mask[:, :].unsqueeze(1).to_broadcast([128, NH, 128]), op=ALU.mult),
                         reads=[("pscC", dr), "maskf", "maskb"], writes=[("scm", dr)])
                    yield
                    corder = (0, 1) if dr == 0 else (1, 0)
                    for ci, c in enumerate(corder):
                        rows = slice(c * 64, (c + 1) * 64)
                        chunk = (tok0 + j * 128 + c * 64) // 64
                        for h in range(NH):
                            S.op("pe", lambda h=h, rows=rows, c=c: T.matmul(po[dr][rows, h * 128:(h + 1) * 128], scm[dr][rows, h * 128 + c * 64: h * 128 + (c + 1) * 64],
                                                                           Vv[rows, j, h * 128:(h + 1) * 128], start=True, stop=False),
                                 reads=[("scm", dr), rv], writes=[("poC", dr)])
                            S.op("pe", lambda h=h, rows=rows, c=c: T.matmul(po[dr][rows, h * 128:(h + 1) * 128], Q[:, h, j * 128 + c * 64: j * 128 + (c + 1) * 64],
                                                                           Sbf[dr][h][:, :], start=False, stop=True),
                                 reads=[rq, ("Sbf", dr, h)], writes=[("poC", dr)])
                            S.op("pe", lambda h=h, rows=rows: T.matmul(pkv[dr][:, h * 128:(h + 1) * 128], K2[rows, j, h * 128:(h + 1) * 128],
                                                                      Vv[rows, j, h * 128:(h + 1) * 128], start=True, stop=True),
                                 reads=[rk2, rv], writes=[("pkvC", dr)])
                        yield
                        for h in range(NH):
                            a = cur["spp"][h]
                            S.op("dve", lambda h=h, a=a, chunk=chunk: V.scalar_tensor_tensor(
                                out=Sbf[dr][h][:], in0=S32[dr][h][a][:], scalar=decS[:, dr, h, chunk:chunk + 1],
                                in1=pkv[dr][:, h * 128:(h + 1) * 128], op0=ALU.mult, op1=ALU.add),
                                 reads=[("S32", dr, h, a), ("pkvC", dr)], writes=[("Sbf", dr, h)])
                            S.op("dve", lambda h=h, a=a, chunk=chunk: V.scalar_tensor_tensor(
                                out=S32[dr][h][1 - a][:], in0=S32[dr][h][a][:], scalar=decS[:, dr, h, chunk:chunk + 1],
                                in1=pkv[dr][:, h * 128:(h + 1) * 128], op0=ALU.mult, op1=ALU.add),
                                 reads=[("S32", dr, h, a), ("pkvC", dr)], writes=[("S32", dr, h, 1 - a)])
                            cur["spp"][h] = 1 - a
                        yield
                    if not second:
                        S.op("act", lambda: A.copy(out=osto[:, t, :], in_=po[dr][:, :]), reads=[("poC", dr)], writes=[("osto", t)])
                        return
                    mi = cur["bb"]
                    S.op("dve", lambda: V.tensor_tensor(out=o32[dr][:], in0=po[dr][:, :], in1=osto[:, t, :], op=ALU.add),
                         reads=[("poC", dr), ("osto", t)], writes=[("o32", dr)])
                    for h in range(NH):
                        S.op("act", lambda h=h: A.activation(out=junk[:], in_=o32[dr][:, h * 128:(h + 1) * 128], func=AF.Square, accum_out=ssq4[dr][:, h:h + 1]),
                             reads=[("o32", dr)], writes=["junkC", ("ssq4", dr)])
                    rstd_from_ssq(ssq4[dr][:], ssq4[dr][:], HD, [("ssq4", dr)], [("ssq4", dr)])
                    S.op("act", lambda: A.activation(out=sg[dr][:], in_=OG[:, j, :], func=AF.Exp, scale=-1.0), reads=[rog], writes=[("sgC", dr)])
                    yield
                    S.op("act", lambda: A.activation(out=sg[dr][:], in_=sg[dr][:], func=AF.Ln, bias=1.0), reads=[("sgC", dr)], writes=[("sgC", dr)])
                    S.op("act", lambda: A.activation(out=sg[dr][:], in_=sg[dr][:], func=AF.Exp, scale=-1.0), reads=[("sgC", dr)], writes=[("sgC", dr)])
                    S.op("dve", lambda: V.tensor_tensor(out=o32[dr][:], in0=o32[dr][:], in1=OG[:, j, :], op=ALU.mult), reads=[("o32", dr), rog], writes=[("o32", dr)])
                    S.op("dve", lambda: V.tensor_tensor(out=sg[dr][:], in0=sg[dr][:], in1=o32[dr][:], op=ALU.mult), reads=[("sgC", dr), ("o32", dr)], writes=[("sgC", dr)])
                    S.op("dve", lambda: V.tensor_tensor(out=mb[dr][:].rearrange("p (h c) -> p h c", h=NH), in0=sg[dr][:].rearrange("p (h c) -> p h c", h=NH),
                                                        in1=ssq4[dr][:, :].unsqueeze(2).to_broadcast([128, NH, 128]), op=ALU.mult),
                         reads=[("sgC", dr), ("ssq4", dr)], writes=[("mbC", dr)])
                    yield
                    for h in range(NH):
                        S.op("pe", lambda h=h: T.transpose(ptr[dr][:, h * 128:(h + 1) * 128], mb[dr][:, h * 128:(h + 1) * 128], ident[:]),
                             reads=[("mbC", dr), "ident"], writes=[("ptrC", dr)])
                    yield
                    S.op("act", lambda: A.activation(out=mst[dr][mi][:, :, j * 128:(j + 1) * 128], in_=ptr[dr][:, 0:512].rearrange("p (h t) -> p h t", h=NH),
                                                     func=AF.Copy, scale=normo[:, 0:1]),
                         reads=[("ptrC", dr), "normo"], writes=[("mstC", dr, mi)])
                    whole = (bi * 4 >= NT // 2) if dr == 0 else (bi * 4 + 3 < NT // 2)
                    if whole:
                        if last_in_blk:
                            S.dma("sp", mixT_s[512:1024, tok0:tok0 + 512].rearrange("(h p) t -> p h t", p=128), mst[dr][mi][:],
                                  reads=[("mstC", dr, mi)], writes=[("mixT_s", "hg", G)])
                    else:
                        S.dma("sp", mixT_s[512:1024, tok0 + j * 128:tok0 + (j + 1) * 128].rearrange("(h p) t -> p h t", p=128), mst[dr][mi][:, :, j * 128:(j + 1) * 128],
                              reads=[("mstC", dr, mi)], writes=[("mixT_s", "hg", G, j)])

                for si, (off, SL) in enumerate(zip(offs, seq_lens)):
                    NT = SL // 128
                    curs = [{"spp": [0] * NH, "bb": 0}, {"spp": [0] * NH, "bb": 0}]
                    for d in range(2):
                        for h in range(NH):
                            S.op("dve", lambda d=d, h=h: V.memset(S32[d][h][0][:], 0.0), writes=[("S32", d, h, 0)])
                            S.op("dve", lambda d=d, h=h: V.memset(Sbf[d][h][:], 0.0), writes=[("Sbf", d, h)])
                    for k in range(NT):
                        tf, tb_ = k, NT - 1 - k
                        gens = [c_step(0, off, SL, tf, tf >= NT // 2, curs[0]), c_step(1, off, SL, tb_, tb_ < NT // 2, curs[1])]
                        alive = [True, True]
                        while any(alive):
                            for d in range(2):
                                if alive[d]:
                                    try:
                                        next(gens[d])
                                    except StopIteration:
                                        alive[d] = False
                S.barrier()

        if "D" in phases:
            with ExitStack() as ph:
                def sb(name, shape, dt):
                    return ph.enter_context(nc.sbuf_tensor(name, shape, dt))

                def ps(name, shape, dt=F32):
                    return ph.enter_context(nc.psum_tensor(name, shape, dt))

                TS = 1024
                NTI = TS // 128
                woutb = sb("woutb", [128, 8, D], BF16)
                wpgb = sb("wpgb", [128, 8, D], BF16)
                wppb = sb("wppb", [128, 2, D], BF16)
                wrb = sb("wrb", [128, 8, 36], BF16)
                x1 = sb("x1", [128, NTI, D], F32)
                hnT = sb("hnT", [128, 8, TS], BF16)
                mt = [sb(f"mtD{i}", [128, 8, 512], BF16) for i in range(2)]
                NWB = 2
                wgb = [sb(f"wgb{i}", [128, 8, DE], BF16) for i in range(NWB)]
                wub = [sb(f"wub{i}", [128, 8, DE], BF16) for i in range(NWB)]
                wdb = [sb(f"wdb{i}", [128, 4, D], BF16) for i in range(NWB)]
                hid = [sb(f"hidD{i}", [128, 4, 512], BF16) for i in range(2)]
                sgt = [sb(f"sgD{i}", [128, 512], F32) for i in range(2)]
                xsb = [sb(f"xsD{i}", [128, D], BF16) for i in range(2)]
                junk = sb("junkD", [128, D], BF16)
                ssq = sb("ssqD", [128, NTI], F32)
                rstd = sb("rstdD", [128, NTI], F32)
                ssq3 = sb("ssq3D", [128, NTI], F32)
                rstd3 = sb("rstd3D", [128, NTI], F32)
                rl = sb("rlD", [128, NTI, 36], F32)
                gates = sb("gatesD", [128, NTI, NE], F32)
                r_mg = sb("r_mg", [128, NTI], F32)
                r_gm = sb("r_gm", [128, NTI, 4], F32)
                r_eg = sb("r_eg", [128, NTI, 4], F32)
                r_sg = sb("r_sg", [128, NTI], F32)
                r_lem = sb("r_lem", [128, NTI, NE], F32)
                r_lem2 = sb("r_lem2", [128, NTI, NE], F32)
                r_oh1 = sb("r_oh1", [128, NTI, NE], F32)
                r_oh2 = sb("r_oh2", [128, NTI, NE], F32)
                r_m1 = sb("r_m1", [128, NTI], F32)
                r_m2 = sb("r_m2", [128, NTI], F32)
                r_w1 = sb("r_w1", [128, NTI], F32)
                r_w2 = sb("r_w2", [128, NTI], F32)
                pld = sb("pld", [128, NTI, PD], F32)
                pbf = [sb(f"pbf{i}", [128, PD], BF16) for i in range(2)]
                ptT = [sb(f"ptT{i}", [128, 2, 128], BF16) for i in range(2)]
                hpT = [sb(f"hpT{i}", [128, 8, 128], BF16) for i in range(2)]
                sig = [sb(f"sigD{i}", [128, D], F32) for i in range(2)]
                pA = ps("pA", [128, 1024], F32)
                pB = ps("pB", [128, 1024], F32)
                pC = ps("pC", [128, 1024], F32)
                pD_ = ps("pD", [128, 1024], F32)
                pCb = pC[:, :].bitcast(BF16)
                pDb = pD_[:, :].bitcast(BF16)

                S.dma("pool", woutb[:], w_out.rearrange("(k p) n -> p k n", p=128), writes=["woutb"])
                S.dma("pool", wpgb[:], w_pg.rearrange("(k p) n -> p k n", p=128), writes=["wpgb"])
                S.dma("pool", wppb[:], w_pp.rearrange("(k p) n -> p k n", p=128), writes=["wppb"])
                S.dma("pool", wrb[:, :, 0:4], w_rg.rearrange("(k p) n -> p k n", p=128), writes=["wrb"])
                S.dma("pool", wrb[:, :, 4:36], w_re.rearrange("(k p) n -> p k n", p=128), writes=["wrb"])

                def norm_T(xin_ap, xin_res, rstd_col, rstd_res, gain, gain_res, out_ap, out_res, pbank, pbank_res, xsi):
                    S.op("act", lambda: A.activation(out=xsb[xsi][:], in_=xin_ap, func=AF.Copy, scale=rstd_col),
                         reads=xin_res + [rstd_res], writes=[("xsD", xsi)])
                    for k in range(8):
                        S.op("pe", lambda k=k: T.transpose(pbank[:, k * 128:(k + 1) * 128], xsb[xsi][:, k * 128:(k + 1) * 128], ident[:]),
                             reads=[("xsD", xsi), "ident"], writes=[pbank_res])
                    S.op("dve", lambda: V.tensor_tensor(out=out_ap, in0=pbank.rearrange("p (k t) -> p k t", k=8),
                                                        in1=gain[:, :].unsqueeze(2).to_broadcast([128, 8, 128]), op=ALU.mult),
                         reads=[pbank_res, gain_res], writes=[out_res])

                def sumsq(i, col_t, res):
                    S.op("act", lambda: A.activation(out=junk[:], in_=x1[:, i, :], func=AF.Square, accum_out=col_t[:, i:i + 1]),
                         reads=[("x1", i)], writes=["junkD", res])

                NST = NTOK // TS
                wload_i = [0]
                nxt_w = [None]

                def load_expert(e):
                    wb = wload_i[0] % NWB
                    wload_i[0] += 1
                    S.dma("pool", wgb[wb][:], w_eg[e].rearrange("(k p) n -> p k n", p=128), writes=[("wgb", wb)])
                    S.dma("pool", wub[wb][:], w_eu[e].rearrange("(k p) n -> p k n", p=128), writes=[("wub", wb)])
                    S.dma("pool", wdb[wb][:], w_ed[e].rearrange("(k p) n -> p k n", p=128), writes=[("wdb", wb)])
                    return wb

                for st in range(NST):
                    T0 = st * TS
                    G0 = T0 // 512
                    mix_reads = [("mixT_s", "hg", G0), ("mixT_s", "hg", G0 + 1)] if "C" in phases else []
                    S.dma("sp", pld[:], p_in[T0:T0 + TS, :].rearrange("(j p) n -> p j n", p=128), writes=["pld"])
                    for hf in range(2):
                        S.dma("sp", mt[hf][:], mixT_s[:, T0 + hf * 512:T0 + (hf + 1) * 512].rearrange("(k p) t -> p k t", p=128),
                              reads=mix_reads, writes=[("mtD", hf)])
                    for i in range(NTI):
                        S.dma("sp", x1[:, i, :], x[T0 + i * 128:T0 + (i + 1) * 128, :], writes=[("x1", i)])
                    for i in range(NTI):
                        pO = pA if i % 2 == 0 else pB
                        pOr = "pA" if i % 2 == 0 else "pB"
                        hf = i // 4
                        jj = i % 4
                        for half in range(2):
                            for k in range(8):
                                S.op("pe", lambda k=k, half=half: T.matmul(pO[:, half * 512:(half + 1) * 512], mt[hf][:, k, jj * 128:(jj + 1) * 128],
                                                                          woutb[:, k, half * 512:(half + 1) * 512], start=(k == 0), stop=(k == 7)),
                                     reads=[("mtD", hf), "woutb"], writes=[(pOr, half)])
                        S.op("dve", lambda i=i: V.tensor_tensor(out=x1[:, i, :], in0=pO[:, :], in1=x1[:, i, :], op=ALU.add),
                             reads=[(pOr, 0), (pOr, 1), ("x1", i)], writes=[("x1", i)])
                        sumsq(i, ssq, "ssqD")
                    rstd_from_ssq(ssq[:], rstd[:], D, ["ssqD"], ["rstdD"])
                    for i in range(NTI):
                        pbk = pCb[:, (i % 2) * 1024:(i % 2 + 1) * 1024]
                        norm_T(x1[:, i, :], [("x1", i)], rstd[:, i:i + 1], "rstdD", gffn, "gffn",
                               hnT[:, :, i * 128:(i + 1) * 128], ("hnT", i), pbk, ("pC", i % 2), i % 2)
                        for k in range(8):
                            S.op("pe", lambda k=k, i=i: T.matmul(pD_[:, (i % 2) * 512:(i % 2) * 512 + 36], hnT[:, k, i * 128:(i + 1) * 128], wrb[:, k, :],
                                                                start=(k == 0), stop=(k == 7)),
                                 reads=[("hnT", i), "wrb"], writes=[("pD", i % 2)])
                        S.op("act", lambda i=i: A.copy(out=rl[:, i, :], in_=pD_[:, (i % 2) * 512:(i % 2) * 512 + 36]), reads=[("pD", i % 2)], writes=[("rl", i)])
                    rl_res = [("rl", i) for i in range(NTI)]
                    lg = rl[:, :, 0:4]
                    le = rl[:, :, 4:36]
                    S.op("dve", lambda: V.tensor_reduce(out=r_mg[:], in_=lg, axis=AX.X, op=ALU.max), reads=rl_res, writes=["r_mg"])
                    mgb = r_mg[:, :].unsqueeze(2).to_broadcast([128, NTI, 4])
                    S.op("dve", lambda: V.tensor_tensor(out=r_gm[:], in0=lg, in1=mgb, op=ALU.is_equal), reads=rl_res + ["r_mg"], writes=["r_gm"])
                    S.op("dve", lambda: V.tensor_tensor(out=r_eg[:], in0=lg, in1=mgb, op=ALU.subtract), reads=rl_res + ["r_mg"], writes=["r_eg"])
                    S.op("act", lambda: A.activation(out=r_eg[:], in_=r_eg[:], func=AF.Exp), reads=["r_eg"], writes=["r_eg"])
                    S.op("dve", lambda: V.tensor_reduce(out=r_sg[:], in_=r_eg[:], axis=AX.X, op=ALU.add), reads=["r_eg"], writes=["r_sg"])
                    S.op("dve", lambda: V.reciprocal(out=r_sg[:], in_=r_sg[:]), reads=["r_sg"], writes=["r_sg"])
                    S.op("dve", lambda: V.tensor_scalar(out=r_gm[:], in0=r_gm[:], scalar1=1.0, scalar2=BIG, op0=ALU.subtract, op1=ALU.mult), reads=["r_gm"], writes=["r_gm"])
                    for i in range(NTI):
                        S.op("dve", lambda i=i: V.tensor_tensor(out=r_lem[:, i, :].rearrange("p (g j) -> p g j", g=4),
                                                                in0=rl[:, i, 4:36].rearrange("p (g j) -> p g j", g=4),
                                                                in1=r_gm[:, i, :].unsqueeze(2).to_broadcast([128, 4, 8]), op=ALU.add),
                             reads=rl_res + ["r_gm"], writes=["r_lem"])
                    S.op("dve", lambda: V.tensor_reduce(out=r_m1[:], in_=r_lem[:], axis=AX.X, op=ALU.max), reads=["r_lem"], writes=["r_m1"])
                    S.op("dve", lambda: V.tensor_tensor(out=r_oh1[:], in0=r_lem[:], in1=r_m1[:, :].unsqueeze(2).to_broadcast([128, NTI, NE]), op=ALU.is_equal),
                         reads=["r_lem", "r_m1"], writes=["r_oh1"])
                    S.op("dve", lambda: V.scalar_tensor_tensor(out=r_lem2[:], in0=r_oh1[:], scalar=-BIG, in1=r_lem[:], op0=ALU.mult, op1=ALU.add),
                         reads=["r_oh1", "r_lem"], writes=["r_lem2"])
                    S.op("dve", lambda: V.tensor_reduce(out=r_m2[:], in_=r_lem2[:], axis=AX.X, op=ALU.max), reads=["r_lem2"], writes=["r_m2"])
                    S.op("dve", lambda: V.tensor_tensor(out=r_oh2[:], in0=r_lem2[:], in1=r_m2[:, :].unsqueeze(2).to_broadcast([128, NTI, NE]), op=ALU.is_equal),
                         reads=["r_lem2", "r_m2"], writes=["r_oh2"])
                    S.op("dve", lambda: V.tensor_sub(out=r_m2[:], in0=r_m2[:], in1=r_m1[:]), reads=["r_m2", "r_m1"], writes=["r_m2"])
                    S.op("act", lambda: A.activation(out=r_m2[:], in_=r_m2[:], func=AF.Exp), reads=["r_m2"], writes=["r_m2"])
                    S.op("dve", lambda: V.tensor_scalar_add(out=r_w1[:], in0=r_m2[:], scalar1=1.0), reads=["r_m2"], writes=["r_w1"])
                    S.op("dve", lambda: V.reciprocal(out=r_w1[:], in_=r_w1[:]), reads=["r_w1"], writes=["r_w1"])
                    S.op("dve", lambda: V.tensor_mul(out=r_w1[:], in0=r_w1[:], in1=r_sg[:]), reads=["r_w1", "r_sg"], writes=["r_w1"])
                    S.op("dve", lambda: V.tensor_mul(out=r_w2[:], in0=r_w1[:], in1=r_m2[:]), reads=["r_w1", "r_m2"], writes=["r_w2"])
                    S.op("dve", lambda: V.tensor_tensor(out=r_oh1[:], in0=r_oh1[:], in1=r_w1[:, :].unsqueeze(2).to_broadcast([128, NTI, NE]), op=ALU.mult),
                         reads=["r_oh1", "r_w1"], writes=["r_oh1"])
                    S.op("dve", lambda: V.tensor_tensor(out=r_oh2[:], in0=r_oh2[:], in1=r_w2[:, :].unsqueeze(2).to_broadcast([128, NTI, NE]), op=ALU.mult),
                         reads=["r_oh2", "r_w2"], writes=["r_oh2"])
                    S.op("dve", lambda: V.tensor_add(out=gates[:], in0=r_oh1[:], in1=r_oh2[:]), reads=["r_oh1", "r_oh2"], writes=["gates"])

                    hnT_res = [("hnT", i) for i in range(NTI)]
                    hi = 0
                    di = 0
                    for e in range(NE):
                        if nxt_w[0] is None:
                            nxt_w[0] = load_expert(e)
                        wb = nxt_w[0]
                        if e + 1 < NE:
                            nxt_w[0] = load_expert(e + 1)
                        elif st + 1 < NST:
                            nxt_w[0] = load_expert(0)
                        else:
                            nxt_w[0] = None
                        for hf in range(2):
                            hb = hi % 2
                            hi += 1
                            for c in range(4):
                                pGU = pA if c % 2 == 0 else pB
                                pGUr = "pA" if c % 2 == 0 else "pB"
                                for k in range(8):
                                    S.op("pe", lambda k=k, c=c: T.matmul(pGU[:, 0:512], wgb[wb][:, k, c * 128:(c + 1) * 128], hnT[:, k, hf * 512:(hf + 1) * 512],
                                                                        start=(k == 0), stop=(k == 7)),
                                         reads=[("wgb", wb)] + hnT_res, writes=[(pGUr, 0)])
                                for k in range(8):
                                    S.op("pe", lambda k=k, c=c: T.matmul(pGU[:, 512:1024], wub[wb][:, k, c * 128:(c + 1) * 128], hnT[:, k, hf * 512:(hf + 1) * 512],
                                                                        start=(k == 0), stop=(k == 7)),
                                         reads=[("wub", wb)] + hnT_res, writes=[(pGUr, 1)])
                                S.op("act", lambda c=c: A.activation(out=sgt[c % 2][:], in_=pGU[:, 0:512], func=AF.Silu), reads=[(pGUr, 0)], writes=[("sgD", c % 2)])
                                S.op("dve", lambda c=c: V.tensor_tensor(out=hid[hb][:, c, :], in0=pGU[:, 512:1024], in1=sgt[c % 2][:], op=ALU.mult),
                                     reads=[(pGUr, 1), ("sgD", c % 2)], writes=[("hid", hb, c)])
                            for jj in range(4):
                                i = hf * 4 + jj
                                pDn = pC if di % 2 == 0 else pD_
                                pDr = "pC" if di % 2 == 0 else "pD"
                                di += 1
                                for half in range(2):
                                    for c in range(4):
                                        S.op("pe", lambda c=c, half=half, jj=jj: T.matmul(pDn[:, half * 512:(half + 1) * 512], hid[hb][:, c, jj * 128:(jj + 1) * 128],
                                                                                       wdb[wb][:, c, half * 512:(half + 1) * 512], start=(c == 0), stop=(c == 3)),
                                             reads=[("hid", hb, c), ("wdb", wb)], writes=[(pDr, half)])
                                S.op("dve", lambda i=i, e=e: V.scalar_tensor_tensor(out=x1[:, i, :], in0=pDn[:, :], scalar=gates[:, i, e:e + 1], in1=x1[:, i, :],
                                                                                   op0=ALU.mult, op1=ALU.add),
                                     reads=[(pDr, 0), (pDr, 1), "gates", ("x1", i)], writes=[("x1", i)])
                    for i in range(NTI):
                        sumsq(i, ssq, "ssqD")
                    rstd_from_ssq(ssq[:], rstd[:], D, ["ssqD"], ["rstdD"])
                    for i in range(NTI):
                        b2 = i % 2
                        pbk = pCb[:, b2 * 1024:(b2 + 1) * 1024]
                        norm_T(x1[:, i, :], [("x1", i)], rstd[:, i:i + 1], "rstdD", gple, "gple",
                               hpT[b2][:], ("hpT", b2), pbk, ("pC", b2), b2)
                        S.op("act", lambda i=i: A.copy(out=pbf[b2][:], in_=pld[:, i, :]), reads=["pld"], writes=[("pbf", b2)])
                        for k in range(2):
                            S.op("pe", lambda k=k: T.transpose(pDb[:, b2 * 1024 + k * 128: b2 * 1024 + (k + 1) * 128], pbf[b2][:, k * 128:(k + 1) * 128], ident[:]),
                                 reads=[("pbf", b2), "ident"], writes=[("pD", b2)])
                        S.op("act", lambda: A.copy(out=ptT[b2][:], in_=pDb[:, b2 * 1024: b2 * 1024 + 256].rearrange("p (k t) -> p k t", k=2)),
                             reads=[("pD", b2)], writes=[("ptT", b2)])
                        for half in range(2):
                            for k in range(8):
                                S.op("pe", lambda k=k, half=half: T.matmul(pA[:, half * 512:(half + 1) * 512], hpT[b2][:, k, :], wpgb[:, k, half * 512:(half + 1) * 512],
                                                                          start=(k == 0), stop=(k == 7)),
                                     reads=[("hpT", b2), "wpgb"], writes=[("pA", half)])
                        for half in range(2):
                            for k in range(2):
                                S.op("pe", lambda k=k, half=half: T.matmul(pB[:, half * 512:(half + 1) * 512], ptT[b2][:, k, :], wppb[:, k, half * 512:(half + 1) * 512],
                                                                          start=(k == 0), stop=(k == 1)),
                                     reads=[("ptT", b2), "wppb"], writes=[("pB", half)])
                        S.op("act", lambda: A.activation(out=sig[b2][:], in_=pA[:, :], func=AF.Sigmoid), reads=[("pA", 0), ("pA", 1)], writes=[("sigD", b2)])
                        S.op("dve", lambda: V.tensor_tensor(out=sig[b2][:], in0=pB[:, :], in1=sig[b2][:], op=ALU.mult),
                             reads=[("pB", 0), ("pB", 1), ("sigD", b2)], writes=[("sigD", b2)])
                        S.op("dve", lambda i=i: V.tensor_tensor(out=x1[:, i, :], in0=sig[b2][:], in1=x1[:, i, :], op=ALU.add),
                             reads=[("sigD", b2), ("x1", i)], writes=[("x1", i)])
                        sumsq(i, ssq3, "ssq3D")
                    rstd_from_ssq(ssq3[:], rstd3[:], D, ["ssq3D"], ["rstd3D"])
                    for i in range(NTI):
                        b2 = i % 2
                        S.op("dve", lambda i=i: V.scalar_tensor_tensor(out=sig[b2][:], in0=x1[:, i, :], scalar=rstd3[:, i:i + 1], in1=gfin[:], op0=ALU.mult, op1=ALU.mult),
                             reads=[("x1", i), "rstd3D", "gfin"], writes=[("sigD", b2)])
                        S.dma("sp", y[T0 + i * 128:T0 + (i + 1) * 128, :], sig[b2][:], reads=[("sigD", b2)], writes=[("y", st, i)])
                S.barrier()

        if "E" in phases:
            esub = [c for c in phases if c.isdigit()] or ["0", "1", "2", "3"]
            TS = 1024
            NTI = TS // 128
            NTILE = NTOK // 128
            NST = NTOK // TS
            with ExitStack() as phE:
                def sbE(name, shape, dt):
                    return phE.enter_context(nc.sbuf_tensor(name, shape, dt))

                OH1 = sbE("OH1", [128, NTILE, NE], F32)
                OH2 = sbE("OH2", [128, NTILE, NE], F32)
                W12 = sbE("W12", [128, NTILE, 2], F32)
                WI = sbE("WI", [128, NSLOT, 6], I32)
                S.dma("sp", rec_s, c_recpad, writes=["rec_s"])

                with ExitStack() as ph:
                    if "0" in esub:
                        def sb(name, shape, dt):
                            return ph.enter_context(nc.sbuf_tensor(name + '_e0', shape, dt))

                        def ps(name, shape, dt=F32):
                            return ph.enter_context(nc.psum_tensor(name + '_e0', shape, dt))

                        woutb = sb("woutb", [128, 8, D], BF16)
                        wrb = sb("wrb", [128, 8, 36], BF16)
                        gffb = sb("gffb", [128, D], F32)
                        x1 = sb("x1", [128, 2 * NTI, D], F32)
                        mt = [sb(f"mtD{i}", [128, 8, 512], BF16) for i in range(4)]
                        hnb = [sb(f"hnb{i}", [128, D], BF16) for i in range(2)]
                        hnT = [sb(f"hnTt{i}", [128, 8, 128], BF16) for i in range(2)]
                        junk = sb("junkD", [128, D], BF16)
                        ssq = sb("ssqD", [128, NTI], F32)
                        rstd = sb("rstdD", [128, NTI], F32)
                        rl = sb("rlD", [128, NTI, 36], F32)
                        r_mg = sb("r_mg", [128, NTI], F32)
                        r_gm = sb("r_gm", [128, NTI, 4], F32)
                        r_eg = sb("r_eg", [128, NTI, 4], F32)
                        r_sg = sb("r_sg", [128, NTI], F32)
                        r_lem = sb("r_lem", [128, NTI, NE], F32)
                        r_lem2 = sb("r_lem2", [128, NTI, NE], F32)
                        r_m1 = sb("r_m1", [128, NTI], F32)
                        r_m2 = sb("r_m2", [128, NTI], F32)
                        r_w1 = sb("r_w1", [128, NTI], F32)
                        pA = ps("pA", [128, 1024], F32)
                        pB = ps("pB", [128, 1024], F32)
                        pC = ps("pC", [128, 1024], F32)
                        pD_ = ps("pD", [128, 1024], F32)
                        pCb = pC[:, :].bitcast(BF16)
                        S.dma("pool", woutb[:], w_out.rearrange("(k p) n -> p k n", p=128), writes=["woutb"])
                        S.dma("pool", wrb[:, :, 0:4], w_rg.rearrange("(k p) n -> p k n", p=128), writes=["wrb"])
                        S.dma("pool", wrb[:, :, 4:36], w_re.rearrange("(k p) n -> p k n", p=128), writes=["wrb"])
                        S.dma("sp", gffb[:], norm_ffn.partition_broadcast(128), writes=["gffb"])
                        def e0_loads(st_):
                            T0_ = st_ * TS
                            xo_ = (st_ % 2) * NTI
                            for hf_ in range(2):
                                S.dma("pool", mt[(st_ % 2) * 2 + hf_][:], mixT_s[:, T0_ + hf_ * 512:T0_ + (hf_ + 1) * 512].rearrange("(k p) t -> p k t", p=128),
                                      writes=[("mtD", (st_ % 2) * 2 + hf_)])
                            for i_ in range(NTI):
                                S.dma("pool", x1[:, xo_ + i_, :], x[T0_ + i_ * 128:T0_ + (i_ + 1) * 128, :], writes=[("x1", xo_ + i_)])

                        e0_loads(0)
                        for st in range(NST):
                            T0 = st * TS
                            xo = (st % 2) * NTI
                            mo = (st % 2) * 2
                            if st + 1 < NST:
                                e0_loads(st + 1)
                            for i in range(NTI):
                                pO = pA if i % 2 == 0 else pB
                                pOr = "pA" if i % 2 == 0 else "pB"
                                hf = i // 4
                                jj = i % 4
                                for half in range(2):
                                    for k in range(8):
                                        S.op("pe", lambda k=k, half=half: T.matmul(pO[:, half * 512:(half + 1) * 512], mt[mo + hf][:, k, jj * 128:(jj + 1) * 128],
                                                                                  woutb[:, k, half * 512:(half + 1) * 512], start=(k == 0), stop=(k == 7)),
                                             reads=[("mtD", mo + hf), "woutb"], writes=[(pOr, half)])
                                S.op("dve", lambda i=i: V.tensor_tensor(out=x1[:, xo + i, :], in0=pO[:, :], in1=x1[:, xo + i, :], op=ALU.add),
                                     reads=[(pOr, 0), (pOr, 1), ("x1", xo + i)], writes=[("x1", xo + i)])
                                S.op("act", lambda i=i: A.activation(out=junk[:], in_=x1[:, xo + i, :], func=AF.Square, accum_out=ssq[:, i:i + 1]),
                                     reads=[("x1", xo + i)], writes=["junkD", "ssqD"])
                                S.dma("sp", x1_s[T0 + i * 128:T0 + (i + 1) * 128, :], x1[:, xo + i, :], reads=[("x1", xo + i)], writes=[("x1_s", st, i)])
                            rstd_from_ssq(ssq[:], rstd[:], D, ["ssqD"], ["rstdD"])
                            for i in range(NTI):
                                b2 = i % 2
                                S.op("dve", lambda i=i: V.scalar_tensor_tensor(out=hnb[b2][:], in0=x1[:, xo + i, :], scalar=rstd[:, i:i + 1], in1=gffb[:], op0=ALU.mult, op1=ALU.mult),
                                     reads=[("x1", xo + i), "rstdD", "gffb"], writes=[("hnb", b2)])
                                S.dma("sp", hn_s[T0 + i * 128:T0 + (i + 1) * 128, :], hnb[b2][:], reads=[("hnb", b2)], writes=[("hn_s", st, i)])
                                pbk = pCb[:, b2 * 1024:(b2 + 1) * 1024]
                                for k in range(8):
                                    S.op("pe", lambda k=k: T.transpose(pbk[:, k * 128:(k + 1) * 128], hnb[b2][:, k * 128:(k + 1) * 128], ident[:]),
                                         reads=[("hnb", b2), "ident"], writes=[("pC", b2)])
                                S.op("act", lambda: A.copy(out=hnT[b2][:], in_=pbk.rearrange("p (k t) -> p k t", k=8)), reads=[("pC", b2)], writes=[("hnTt", b2)])
                                for k in range(8):
                                    S.op("pe", lambda k=k: T.matmul(pD_[:, b2 * 512:b2 * 512 + 36], hnT[b2][:, k, :], wrb[:, k, :], start=(k == 0), stop=(k == 7)),
                                         reads=[("hnTt", b2), "wrb"], writes=[("pD", b2)])
                                S.op("act", lambda i=i: A.copy(out=rl[:, i, :], in_=pD_[:, b2 * 512:b2 * 512 + 36]), reads=[("pD", b2)], writes=[("rl", i)])
                            tsl = slice(st * NTI, (st + 1) * NTI)
                            oh1 = OH1[:, tsl, :]
                            oh2 = OH2[:, tsl, :]
                            rl_res = [("rl", i) for i in range(NTI)]
                            lg = rl[:, :, 0:4]
                            S.op("dve", lambda: V.tensor_reduce(out=r_mg[:], in_=lg, axis=AX.X, op=ALU.max), reads=rl_res, writes=["r_mg"])
                            mgb = r_mg[:, :].unsqueeze(2).to_broadcast([128, NTI, 4])
                            S.op("dve", lambda: V.tensor_tensor(out=r_gm[:], in0=lg, in1=mgb, op=ALU.is_equal), reads=rl_res + ["r_mg"], writes=["r_gm"])
                            S.op("dve", lambda: V.tensor_tensor(out=r_eg[:], in0=lg, in1=mgb, op=ALU.subtract), reads=rl_res + ["r_mg"], writes=["r_eg"])
                            S.op("act", lambda: A.activation(out=r_eg[:], in_=r_eg[:], func=AF.Exp), reads=["r_eg"], writes=["r_eg"])
                            S.op("dve", lambda: V.tensor_reduce(out=r_sg[:], in_=r_eg[:], axis=AX.X, op=ALU.add), reads=["r_eg"], writes=["r_sg"])
                            S.op("dve", lambda: V.reciprocal(out=r_sg[:], in_=r_sg[:]), reads=["r_sg"], writes=["r_sg"])
                            S.op("dve", lambda: V.tensor_scalar(out=r_gm[:], in0=r_gm[:], scalar1=1.0, scalar2=BIG, op0=ALU.subtract, op1=ALU.mult), reads=["r_gm"], writes=["r_gm"])
                            for i in range(NTI):
                                S.op("dve", lambda i=i: V.tensor_tensor(out=r_lem[:, i, :].rearrange("p (g j) -> p g j", g=4),
                                                                        in0=rl[:, i, 4:36].rearrange("p (g j) -> p g j", g=4),
                                                                        in1=r_gm[:, i, :].unsqueeze(2).to_broadcast([128, 4, 8]), op=ALU.add),
                                     reads=rl_res + ["r_gm"], writes=["r_lem"])
                            S.op("dve", lambda: V.tensor_reduce(out=r_m1[:], in_=r_lem[:], axis=AX.X, op=ALU.max), reads=["r_lem"], writes=["r_m1"])
                            S.op("dve", lambda: V.tensor_tensor(out=oh1, in0=r_lem[:], in1=r_m1[:, :].unsqueeze(2).to_broadcast([128, NTI, NE]), op=ALU.is_equal),
                                 reads=["r_lem", "r_m1"], writes=[("OH1", st)])
                            S.op("dve", lambda: V.scalar_tensor_tensor(out=r_lem2[:], in0=oh1, scalar=-BIG, in1=r_lem[:], op0=ALU.mult, op1=ALU.add),
                                 reads=[("OH1", st), "r_lem"], writes=["r_lem2"])
                            S.op("dve", lambda: V.tensor_reduce(out=r_m2[:], in_=r_lem2[:], axis=AX.X, op=ALU.max), reads=["r_lem2"], writes=["r_m2"])
                            S.op("dve", lambda: V.tensor_tensor(out=oh2, in0=r_lem2[:], in1=r_m2[:, :].unsqueeze(2).to_broadcast([128, NTI, NE]), op=ALU.is_equal),
                                 reads=["r_lem2", "r_m2"], writes=[("OH2", st)])
                            S.op("dve", lambda: V.tensor_sub(out=r_m2[:], in0=r_m2[:], in1=r_m1[:]), reads=["r_m2", "r_m1"], writes=["r_m2"])
                            S.op("act", lambda: A.activation(out=r_m2[:], in_=r_m2[:], func=AF.Exp), reads=["r_m2"], writes=["r_m2"])
                            S.op("dve", lambda: V.tensor_scalar_add(out=r_w1[:], in0=r_m2[:], scalar1=1.0), reads=["r_m2"], writes=["r_w1"])
                            S.op("dve", lambda: V.reciprocal(out=r_w1[:], in_=r_w1[:]), reads=["r_w1"], writes=["r_w1"])
                            S.op("dve", lambda: V.tensor_mul(out=W12[:, tsl, 0], in0=r_w1[:], in1=r_sg[:]), reads=["r_w1", "r_sg"], writes=[("W12", st, 0)])
                            S.op("dve", lambda: V.tensor_mul(out=W12[:, tsl, 1], in0=W12[:, tsl, 0], in1=r_m2[:]), reads=[("W12", st, 0), "r_m2"], writes=[("W12", st, 1)])
                        S.barrier()

                with ExitStack() as ph:
                    if "1" in esub:
                        def sb(name, shape, dt):
                            return ph.enter_context(nc.sbuf_tensor(name + '_e1', shape, dt))

                        def ps(name, shape, dt=F32):
                            return ph.enter_context(nc.psum_tensor(name + '_e1', shape, dt))

                        NTHR = NTOK // SLOT
                        Lb = sb("Lb", [128, 128], BF16)
                        onesb = sb("onesb", [128, 128], BF16)
                        mask96 = sb("mask96", [128, NE * NTILE], F32)
                        tokid = sb("tokid", [128, NTILE], F32)
                        slotst = sb("slotst", [128, NSLOT], F32)
                        thr = sb("thr", [128, NTHR], F32)
                        addc = sb("addc", [128, 6], F32)
                        mulc = sb("mulc", [128, 6], F32)
                        ones32 = sb("ones32", [128, NE], F32)
                        ohE = sb("ohE", [128, NE, NTILE], F32)
                        incl = sb("incl", [128, NE, NTILE], F32)
                        ohb = sb("ohb", [128, NTILE, NE], BF16)
                        ohcb = sb("ohcb", [128, NE, NTILE], BF16)
                        inclb = sb("inclb", [128, NE], BF16)
                        C_all = sb("C_all", [128, NTILE, NE], F32)
                        prod = sb("prodE", [128, NTILE, NE], F32)
                        cnt = sb("cntE", [128, NE], F32)
                        cmpt = sb("cmpt", [128, NE, NTHR], F32)
                        padded = sb("padded", [128, NE], F32)
                        inclp = sb("inclp", [128, NE], F32)
                        base = sb("baseE", [128, NE], F32)
                        POS = sb("POS", [128, NTILE, 2], F32)
                        POSI = sb("POSI", [128, NTILE, 2], I32)
                        REC = sb("REC", [128, NTILE, 2, 4], I32)
                        cmps = sb("cmps", [128, NSLOT, NE], F32)
                        slot_e = sb("slot_e", [128, NSLOT], F32)
                        WIf = sb("WIf", [128, NSLOT, 6], F32)
                        pS = [ps(f"pA{i}", [128, 512], F32) if False else ps(f"pSx{i}", [128, 512], F32) for i in range(2)]
                        pS2 = ps("pSy", [128, 512], F32)
                        for (t_, src, nm) in ((Lb, c_L, "Lb"), (onesb, c_ones, "onesb"), (mask96, c_mask96, "mask96"), (tokid, c_tokid, "tokid"),
                                              (slotst, c_slotstart, "slotst"), (thr, c_thr, "thr"), (addc, c_addc, "addc"), (mulc, c_mulc, "mulc")):
                            S.dma("sp", t_[:], src, writes=[nm])
                        S.op("dve", lambda: V.memset(ones32[:], 1.0), writes=["ones32"])
                        S.op("dve", lambda: V.tensor_tensor(out=ohE[:].rearrange("p e t -> p t e"), in0=OH1[:], in1=OH2[:], op=ALU.add), writes=["ohE"])
                        S.op("dve", lambda: V.tensor_tensor(out=ohb[:], in0=OH1[:], in1=OH2[:], op=ALU.add), writes=["ohb"])
                        S.op("dve", lambda: V.tensor_tensor_scan(out=incl[:].rearrange("p e t -> p (e t)"), data0=mask96[:], data1=ohE[:].rearrange("p e t -> p (e t)"),
                                                                 initial=0.0, op0=ALU.mult, op1=ALU.add), reads=["ohE", "mask96"], writes=["incl"])
                        S.op("dve", lambda: V.tensor_tensor(out=ohcb[:], in0=incl[:], in1=ohE[:], op=ALU.subtract), reads=["incl", "ohE"], writes=["ohcb"])
                        S.op("dve", lambda: V.tensor_copy(out=inclb[:], in_=incl[:, :, NTILE - 1]), reads=["incl"], writes=["inclb"])
                        for i in range(NTILE):
                            pb = (i // 16) % 2
                            sl = (i % 16) * NE
                            S.op("pe", lambda i=i, pb=pb, sl=sl: T.matmul(pS[pb][:, sl:sl + NE], Lb[:], ohb[:, i, :], start=True, stop=False),
                                 reads=["Lb", "ohb"], writes=[("pSx", pb)])
                            S.op("pe", lambda i=i, pb=pb, sl=sl: T.matmul(pS[pb][:, sl:sl + NE], onesb[:], ohcb[:, :, i], start=False, stop=True),
                                 reads=["onesb", "ohcb"], writes=[("pSx", pb)])
                            if i % 16 == 15 or i == NTILE - 1:
                                i0 = (i // 16) * 16
                                n_ = i - i0 + 1
                                S.op("dve", lambda i0=i0, n_=n_, pb=pb: V.tensor_copy(out=C_all[:, i0:i0 + n_, :], in_=pS[pb][:, 0:n_ * NE].rearrange("p (t e) -> p t e", e=NE)),
                                     reads=[("pSx", pb)], writes=["C_all"])
                        S.op("pe", lambda: T.matmul(pS2[:, 0:NE], onesb[:], inclb[:], start=True, stop=True), reads=["onesb", "inclb"], writes=["pSy"])
                        S.op("dve", lambda: V.tensor_copy(out=cnt[:], in_=pS2[:, 0:NE]), reads=["pSy"], writes=["cntE"])
                        S.op("dve", lambda: V.tensor_tensor(out=cmpt[:], in0=cnt[:, :].unsqueeze(2).to_broadcast([128, NE, NTHR]),
                                                            in1=thr[:, :].unsqueeze(1).to_broadcast([128, NE, NTHR]), op=ALU.is_gt),
                             reads=["cntE", "thr"], writes=["cmpt"])
                        S.op("dve", lambda: V.tensor_reduce(out=padded[:], in_=cmpt[:], axis=AX.X, op=ALU.add), reads=["cmpt"], writes=["padded"])
                        S.op("dve", lambda: V.tensor_scalar_mul(out=padded[:], in0=padded[:], scalar1=float(SLOT)), reads=["padded"], writes=["padded"])
                        S.op("dve", lambda: V.tensor_tensor_scan(out=inclp[:], data0=ones32[:], data1=padded[:], initial=0.0, op0=ALU.mult, op1=ALU.add),
                             reads=["ones32", "padded"], writes=["inclp"])
                        S.op("dve", lambda: V.tensor_sub(out=base[:], in0=inclp[:], in1=padded[:]), reads=["inclp", "padded"], writes=["baseE"])
                        S.op("dve", lambda: V.tensor_tensor(out=C_all[:], in0=C_all[:], in1=base[:, :].unsqueeze(1).to_broadcast([128, NTILE, NE]), op=ALU.add),
                             reads=["C_all", "baseE"], writes=["C_all"])
                        for r, OH in enumerate((OH1, OH2)):
                            S.op("dve", lambda OH=OH: V.tensor_tensor(out=prod[:], in0=C_all[:], in1=OH[:], op=ALU.mult), reads=["C_all"], writes=["prodE"])
                            S.op("dve", lambda r=r: V.tensor_reduce(out=POS[:, :, r], in_=prod[:], axis=AX.X, op=ALU.add), reads=["prodE"], writes=["POS"])
                        S.op("dve", lambda: V.tensor_copy(out=POSI[:], in_=POS[:]), reads=["POS"], writes=["POSI"])
                        S.op("dve", lambda: V.memset(REC[:], 0), writes=["REC"])
                        for r in range(2):
                            S.op("dve", lambda r=r: V.tensor_copy(out=REC[:, :, r, 0], in_=tokid[:]), reads=["tokid", "REC"], writes=["REC"])
                            S.op("dve", lambda r=r: V.tensor_scalar_add(out=POS[:, :, r], in0=tokid[:], scalar1=float(r * NTOK)), reads=["tokid", "POSI"], writes=["POS"])
                            S.op("dve", lambda r=r: V.tensor_copy(out=REC[:, :, r, 1], in_=POS[:, :, r]), reads=["POS", "REC"], writes=["REC"])
                            S.op("dve", lambda r=r: V.tensor_copy(out=REC[:, :, r, 2].bitcast(F32), in_=W12[:, :, r]), reads=["REC"], writes=["REC"])
                        ev_sc = []
                        for i in range(NTILE):
                            for r in range(2):
                                S.idma("pool", lambda: P.indirect_dma_start(out=rec_s, out_offset=bass.IndirectOffsetOnAxis(ap=POSI[:, i, r:r + 1], axis=0), in_=REC[:, i, r, :], in_offset=None),
                                       reads=["REC", "POSI", "rec_s"], writes=[("rec_sc", i, r)])
                        S.op("dve", lambda: V.tensor_tensor(out=cmps[:], in0=inclp[:, :].unsqueeze(1).to_broadcast([128, NSLOT, NE]),
                                                            in1=slotst[:, :].unsqueeze(2).to_broadcast([128, NSLOT, NE]), op=ALU.is_le),
                             reads=["inclp", "slotst"], writes=["cmps"])
                        S.op("dve", lambda: V.tensor_reduce(out=slot_e[:], in_=cmps[:], axis=AX.X, op=ALU.add), reads=["cmps"], writes=["slot_e"])
                        S.op("dve", lambda: V.tensor_scalar_min(out=slot_e[:], in0=slot_e[:], scalar1=float(NE - 1)), reads=["slot_e"], writes=["slot_e"])
                        S.op("dve", lambda: V.tensor_tensor(out=WIf[:], in0=slot_e[:, :].unsqueeze(2).to_broadcast([128, NSLOT, 6]),
                                                            in1=mulc[:, :].unsqueeze(1).to_broadcast([128, NSLOT, 6]), op=ALU.mult),
                             reads=["slot_e", "mulc"], writes=["WIf"])
                        S.op("dve", lambda: V.tensor_tensor(out=WIf[:], in0=WIf[:], in1=addc[:, :].unsqueeze(1).to_broadcast([128, NSLOT, 6]), op=ALU.add),
                             reads=["WIf", "addc"], writes=["WIf"])
                        S.op("dve", lambda: V.tensor_copy(out=WI[:], in_=WIf[:]), reads=["WIf"], writes=["WI"])
                        S.barrier()

                with ExitStack() as ph:
                    if "2" in esub:
                        def sb(name, shape, dt):
                            return ph.enter_context(nc.sbuf_tensor(name + '_e2', shape, dt))

                        def ps(name, shape, dt=F32):
                            return ph.enter_context(nc.psum_tensor(name + '_e2', shape, dt))

                        NB2 = 2
                        recb = [sb(f"recb{i}", [128, 4, 4], I32) for i in range(NB2)]
                        hng = [[sb(f"hng{i}_{j}", [128, D], BF16) for j in range(4)] for i in range(NB2)]
                        hnTs = [sb(f"hnTs{i}", [128, 8, 512], BF16) for i in range(NB2)]
                        wgb = [sb(f"wgb{i}", [128, 8, DE], BF16) for i in range(NB2)]
                        wub = [sb(f"wub{i}", [128, 8, DE], BF16) for i in range(NB2)]
                        wdb = [sb(f"wdb{i}", [128, 4, D], BF16) for i in range(NB2)]
                        hid = [sb(f"hidD{i}", [128, 4, 512], BF16) for i in range(2)]
                        sgt = [sb(f"sgD{i}", [128, 512], F32) for i in range(2)]
                        yrow = [sb(f"yrow{i}", [128, D], BF16) for i in range(4)]
                        pA = ps("pA", [128, 1024], F32)
                        pB = ps("pB", [128, 1024], F32)
                        pC = ps("pC", [128, 1024], F32)
                        pD_ = ps("pD", [128, 1024], F32)
                        pCb = pC[:, :].bitcast(BF16)
                        pDb = pD_[:, :].bitcast(BF16)
                        wg_v = w_eg.rearrange("e (p a k) n -> (e p a) (k n)", p=128, a=2)
                        wu_v = w_eu.rearrange("e (p a k) n -> (e p a) (k n)", p=128, a=2)
                        wd_v = w_ed.rearrange("e f n -> (e f) n")
                        rec_scatter_res = [("rec_sc", i, r) for i in range(NTILE) for r in range(2)]

                        def load_slot(s):
                            b = s % NB2
                            S.dma("sp", recb[b][:], rec_s[s * SLOT:(s + 1) * SLOT, :].rearrange("(j p) c -> p j c", p=128), writes=[("recb", b)])
                            for j in range(4):
                                S.idma("pool", lambda: P.indirect_dma_start(out=hng[b][j][:], out_offset=None, in_=hn_s, in_offset=bass.IndirectOffsetOnAxis(ap=recb[b][:, j, 0:1], axis=0)),
                                       reads=[("recb", b)], writes=[("hng", b, j)])
                            for (wt_, src_, nm_) in ((wgb, wbg_s, "wgb"), (wub, wbu_s, "wub"), (wdb, wbd_s, "wdb")):
                                S.idma("pool", lambda wt_=wt_, src_=src_: P.indirect_dma_start(out=wt_[b][:, :, :].rearrange("p k n -> p (k n)"), out_offset=None, in_=src_,
                                                                                              in_offset=bass.IndirectOffsetOnAxis(ap=WI[:, s, 0:1], axis=0)),
                                       reads=["WI"], writes=[(nm_, b)])

                        load_slot(0)
                        yi = 0
                        for s in range(NSLOT):
                            b = s % NB2
                            if s + 1 < NSLOT:
                                load_slot(s + 1)
                            for j in range(4):
                                pbk = pCb[:, (j % 2) * 1024:(j % 2 + 1) * 1024]
                                for k in range(8):
                                    S.op("pe", lambda k=k, j=j, pbk=pbk: T.transpose(pbk[:, k * 128:(k + 1) * 128],
                                                                                    hng[b][j][:, :].rearrange("p (q k) -> p k q", k=8)[:, k, :], ident[:]),
                                         reads=[("hng", b, j), "ident"], writes=[("pC", j % 2)])
                                if j % 2 == 0:
                                    S.op("act", lambda j=j, pbk=pbk: A.copy(out=hnTs[b][:, :, j * 128:(j + 1) * 128], in_=pbk.rearrange("p (k t) -> p k t", k=8)),
                                         reads=[("pC", j % 2)], writes=[("hnTs", b, j)])
                                else:
                                    S.op("dve", lambda j=j, pbk=pbk: V.tensor_copy(out=hnTs[b][:, :, j * 128:(j + 1) * 128], in_=pbk.rearrange("p (k t) -> p k t", k=8)),
                                         reads=[("pC", j % 2)], writes=[("hnTs", b, j)])
                            hnTs_res = [("hnTs", b, j) for j in range(4)]
                            hb = s % 2
                            for c in range(4):
                                pGU = pA if c % 2 == 0 else pB
                                pGUr = "pA" if c % 2 == 0 else "pB"
                                for k in range(8):
                                    S.op("pe", lambda k=k, c=c: T.matmul(pGU[:, 0:512], wgb[b][:, k, c * 128:(c + 1) * 128], hnTs[b][:, k, :], start=(k == 0), stop=(k == 7)),
                                         reads=[("wgb", b)] + hnTs_res, writes=[(pGUr, 0)])
                                for k in range(8):
                                    S.op("pe", lambda k=k, c=c: T.matmul(pGU[:, 512:1024], wub[b][:, k, c * 128:(c + 1) * 128], hnTs[b][:, k, :], start=(k == 0), stop=(k == 7)),
                                         reads=[("wub", b)] + hnTs_res, writes=[(pGUr, 1)])
                                S.op("act", lambda c=c: A.activation(out=sgt[c % 2][:], in_=pGU[:, 0:512], func=AF.Silu), reads=[(pGUr, 0)], writes=[("sgD", c % 2)])
                                S.op("dve", lambda c=c: V.tensor_tensor(out=hid[hb][:, c, :], in0=pGU[:, 512:1024], in1=sgt[c % 2][:], op=ALU.mult),
                                     reads=[(pGUr, 1), ("sgD", c % 2)], writes=[("hid", hb, c)])
                            for j in range(4):
                                pDn = pC if j % 2 == 0 else pD_
                                pDr = "pC" if j % 2 == 0 else "pD"
                                for half in range(2):
                                    for c in range(4):
                                        S.op("pe", lambda c=c, half=half, j=j: T.matmul(pDn[:, half * 512:(half + 1) * 512], hid[hb][:, c, j * 128:(j + 1) * 128],
                                                                                     wdb[b][:, c, half * 512:(half + 1) * 512], start=(c == 0), stop=(c == 3)),
                                             reads=[("hid", hb, c), ("wdb", b)], writes=[(pDr, half)])
                                yb = yi % 4
                                yi += 1
                                if j % 2 == 0:
                                    S.op("act", lambda j=j, yb=yb: A.activation(out=yrow[yb][:], in_=pDn[:, :], func=AF.Copy, scale=recb[b][:, j, 2:3].bitcast(F32)),
                                         reads=[(pDr, 0), (pDr, 1), ("recb", b)], writes=[("yrow", yb)])
                                else:
                                    S.op("dve", lambda j=j, yb=yb: V.tensor_scalar(out=yrow[yb][:], in0=pDn[:, :], scalar1=recb[b][:, j, 2:3].bitcast(F32), scalar2=None, op0=ALU.mult),
                                         reads=[(pDr, 0), (pDr, 1), ("recb", b)], writes=[("yrow", yb)])
                                S.idma("pool", lambda: P.indirect_dma_start(out=ybuf, out_offset=bass.IndirectOffsetOnAxis(ap=recb[b][:, j, 1:2], axis=0), in_=yrow[yb][:], in_offset=None),
                                       reads=[("yrow", yb), ("recb", b)], writes=[("ybuf", s, j)])
                        S.barrier()

                with ExitStack() as ph:
                    if "3" in esub:
                        def sb(name, shape, dt):
                            return ph.enter_context(nc.sbuf_tensor(name + '_e3', shape, dt))

                        def ps(name, shape, dt=F32):
                            return ph.enter_context(nc.psum_tensor(name + '_e3', shape, dt))

                        wpgb = sb("wpgb", [128, 8, D], BF16)
                        wppb = sb("wppb", [128, 2, D], BF16)
                        x1 = sb("x1", [128, 2 * NTI, D], F32)
                        ya = [sb(f"yaE{i}", [128, 2, D], BF16) for i in range(4)]
                        xsb = [sb(f"xsD{i}", [128, D], BF16) for i in range(2)]
                        junk = sb("junkD", [128, D], BF16)
                        ssq = sb("ssqD", [128, 2 * NTI], F32)
                        rstd = sb("rstdD", [128, 2 * NTI], F32)
                        ssq3 = sb("ssq3D", [128, NTI], F32)
                        rstd3 = sb("rstd3D", [128, NTI], F32)
                        pld = sb("pld", [128, 2 * NTI, PD], F32)
                        pbf = [sb(f"pbf{i}", [128, PD], BF16) for i in range(2)]
                        ptT = [sb(f"ptT{i}", [128, 2, 128], BF16) for i in range(2)]
                        hpT = [sb(f"hpT{i}", [128, 8, 128], BF16) for i in range(2)]
                        sig = [sb(f"sigD{i}", [128, D], F32) for i in range(2)]
                        pA = ps("pA", [128, 1024], F32)
                        pB = ps("pB", [128, 1024], F32)
                        pC = ps("pC", [128, 1024], F32)
                        pD_ = ps("pD", [128, 1024], F32)
                        pCb = pC[:, :].bitcast(BF16)
                        pDb = pD_[:, :].bitcast(BF16)
                        S.dma("pool", wpgb[:], w_pg.rearrange("(k p) n -> p k n", p=128), writes=["wpgb"])
                        S.dma("pool", wppb[:], w_pp.rearrange("(k p) n -> p k n", p=128), writes=["wppb"])

                        def sumsq(xi, col_t, ci, res):
                            S.op("act", lambda: A.activation(out=junk[:], in_=x1[:, xi, :], func=AF.Square, accum_out=col_t[:, ci:ci + 1]),
                                 reads=[("x1", xi)], writes=["junkD", res])

                        def norm_T(xin_ap, xin_res, rstd_col, rstd_res, gain, gain_res, out_ap, out_res, pbank, pbank_res, xsi):
                            S.op("act", lambda: A.activation(out=xsb[xsi][:], in_=xin_ap, func=AF.Copy, scale=rstd_col),
                                 reads=xin_res + [rstd_res], writes=[("xsD", xsi)])
                            for k in range(8):
                                S.op("pe", lambda k=k: T.transpose(pbank[:, k * 128:(k + 1) * 128], xsb[xsi][:, k * 128:(k + 1) * 128], ident[:]),
                                     reads=[("xsD", xsi), "ident"], writes=[pbank_res])
                            S.op("dve", lambda: V.tensor_tensor(out=out_ap, in0=pbank.rearrange("p (k t) -> p k t", k=8),
                                                                in1=gain[:, :].unsqueeze(2).to_broadcast([128, 8, 128]), op=ALU.mult),
                                 reads=[pbank_res, gain_res], writes=[out_res])

                        yb_v = ybuf[0:2 * NTOK, :].rearrange("(r t) d -> t r d", r=2)

                        def e3_loads(st_):
                            T0_ = st_ * TS
                            xo_ = (st_ % 2) * NTI
                            S.dma("pool", pld[:, xo_:xo_ + NTI, :], p_in[T0_:T0_ + TS, :].rearrange("(j p) n -> p j n", p=128), writes=[("pld", st_ % 2)])
                            for i_ in range(NTI):
                                S.dma("pool", x1[:, xo_ + i_, :], x1_s[T0_ + i_ * 128:T0_ + (i_ + 1) * 128, :], writes=[("x1", xo_ + i_)])

                        def ya_load(g_):
                            S.dma("pool", ya[g_ % 4][:], yb_v[g_ * 128:(g_ + 1) * 128, :, :], writes=[("yaE", g_ % 4)])

                        def e3_combine(st, i):
                            xo = (st % 2) * NTI
                            yq = (st * NTI + i) % 4
                            S.op("dve", lambda: V.tensor_tensor(out=x1[:, xo + i, :], in0=x1[:, xo + i, :], in1=ya[yq][:, 0, :], op=ALU.add),
                                 reads=[("x1", xo + i), ("yaE", yq)], writes=[("x1", xo + i)])
                            S.op("dve", lambda: V.tensor_tensor(out=x1[:, xo + i, :], in0=x1[:, xo + i, :], in1=ya[yq][:, 1, :], op=ALU.add),
                                 reads=[("x1", xo + i), ("yaE", yq)], writes=[("x1", xo + i)])
                            if st * NTI + i + 4 < NTOK // 128:
                                ya_load(st * NTI + i + 4)
                            sumsq(xo + i, ssq, xo + i, ("ssqD", st % 2))

                        def e3_rstd1(st):
                            xo = (st % 2) * NTI
                            rstd_from_ssq(ssq[:, xo:xo + NTI], rstd[:, xo:xo + NTI], D, [("ssqD", st % 2)], [("rstdD", st % 2)])

                        def e3_front(st, i):
                            b2 = i % 2
                            xo = (st % 2) * NTI
                            ro = (st % 2) * NTI
                            pbk = pCb[:, b2 * 1024:(b2 + 1) * 1024]
                            norm_T(x1[:, xo + i, :], [("x1", xo + i)], rstd[:, ro + i:ro + i + 1], ("rstdD", st % 2), gple, "gple",
                                   hpT[b2][:], ("hpT", b2), pbk, ("pC", b2), b2)
                            S.op("act", lambda i=i: A.copy(out=pbf[b2][:], in_=pld[:, xo + i, :]), reads=[("pld", st % 2)], writes=[("pbf", b2)])
                            for k in range(2):
                                S.op("pe", lambda k=k: T.transpose(pDb[:, b2 * 1024 + k * 128: b2 * 1024 + (k + 1) * 128], pbf[b2][:, k * 128:(k + 1) * 128], ident[:]),
                                     reads=[("pbf", b2), "ident"], writes=[("pD", b2)])
                            S.op("act", lambda: A.copy(out=ptT[b2][:], in_=pDb[:, b2 * 1024: b2 * 1024 + 256].rearrange("p (k t) -> p k t", k=2)),
                                 reads=[("pD", b2)], writes=[("ptT", b2)])

                        def e3_back(st, i):
                            b2 = i % 2
                            xo = (st % 2) * NTI
                            ro = (st % 2) * NTI
                            for half in range(2):
                                for k in range(8):
                                    S.op("pe", lambda k=k, half=half: T.matmul(pA[:, half * 512:(half + 1) * 512], hpT[b2][:, k, :], wpgb[:, k, half * 512:(half + 1) * 512],
                                                                              start=(k == 0), stop=(k == 7)),
                                         reads=[("hpT", b2), "wpgb"], writes=[("pA", half)])
                            for half in range(2):
                                for k in range(2):
                                    S.op("pe", lambda k=k, half=half: T.matmul(pB[:, half * 512:(half + 1) * 512], ptT[b2][:, k, :], wppb[:, k, half * 512:(half + 1) * 512],
                                                                              start=(k == 0), stop=(k == 1)),
                                         reads=[("ptT", b2), "wppb"], writes=[("pB", half)])
                            for half in range(2):
                                hs = slice(half * 512, (half + 1) * 512)
                                S.op("act", lambda hs=hs: A.activation(out=sig[b2][:, hs], in_=pA[:, hs], func=AF.Sigmoid), reads=[("pA", half)], writes=[("sigD", b2, half)])
                                S.op("dve", lambda hs=hs: V.tensor_tensor(out=sig[b2][:, hs], in0=pB[:, hs], in1=sig[b2][:, hs], op=ALU.mult),
                                     reads=[("pB", half), ("sigD", b2, half)], writes=[("sigD", b2, half)])
                            S.op("dve", lambda i=i: V.tensor_tensor(out=x1[:, xo + i, :], in0=sig[b2][:], in1=x1[:, xo + i, :], op=ALU.add),
                                 reads=[("sigD", b2, 0), ("sigD", b2, 1), ("x1", xo + i)], writes=[("x1", xo + i)])
                            sumsq(xo + i, ssq3, i, "ssq3D")


                        e3_loads(0)
                        for g_ in range(4):
                            ya_load(g_)
                        for i in range(NTI):
                            e3_combine(0, i)
                        e3_rstd1(0)
                        for st in range(NST):
                            T0 = st * TS
                            xo = (st % 2) * NTI
                            if st + 1 < NST:
                                e3_loads(st + 1)
                            e3_front(st, 0)
                            for i in range(NTI):
                                if i + 1 < NTI:
                                    e3_front(st, i + 1)
                                e3_back(st, i)
                                if st + 1 < NST:
                                    e3_combine(st + 1, i)
                            if st + 1 < NST:
                                e3_rstd1(st + 1)
                            rstd_from_ssq(ssq3[:], rstd3[:], D, ["ssq3D"], ["rstd3D"])
                            for i in range(NTI):
                                S.op("dve", lambda i=i: V.scalar_tensor_tensor(out=x1[:, xo + i, :], in0=x1[:, xo + i, :], scalar=rstd3[:, i:i + 1], in1=gfin[:], op0=ALU.mult, op1=ALU.mult),
                                     reads=[("x1", xo + i), "rstd3D", "gfin"], writes=[("x1", xo + i)])
                                S.dma("sp", y[T0 + i * 128:T0 + (i + 1) * 128, :], x1[:, xo + i, :], reads=[("x1", xo + i)], writes=[("y", st, i)])
                        S.barrier()

        S.finish("sp")
        build.stats = dict(cnt=dict(S.cnt), waits=S.n_wait)
    return nc


_CONST = {}


def _consts():
    if _CONST:
        return _CONST
    bf = ml_dtypes.bfloat16
    idx = np.arange(128)
    same = (idx[:, None] // 64) == (idx[None, :] // 64)
    _CONST["c_ident"] = np.eye(128, dtype=np.float32).astype(bf)
    _CONST["c_maskf"] = (same & (idx[:, None] <= idx[None, :])).astype(np.float32).astype(bf)
    _CONST["c_maskb"] = (same & (idx[:, None] >= idx[None, :])).astype(np.float32).astype(bf)
    ang = 2.0 * np.pi * ((idx[:, None] * idx[None, :]) % 128) / 128.0
    _CONST["c_cc"] = np.concatenate([np.cos(ang), np.sin(ang)], axis=1).astype(np.float32).astype(bf)
    n = np.arange(NTAB, dtype=np.int64)
    m = (n[:, None] * n[None, :]) % NTAB
    tab = np.cos(2.0 * np.pi * np.arange(NTAB) / NTAB)
    tabs = -np.sin(2.0 * np.pi * np.arange(NTAB) / NTAB)
    _CONST["c_ctab"] = tab[m].astype(np.float32).astype(bf)
    _CONST["c_stab"] = tabs[m].astype(np.float32).astype(bf)
    return _CONST


def _consts_n(NTOK):
    bf = ml_dtypes.bfloat16
    NSLOT = (2 * NTOK) // SLOT + NE
    NPOS = NSLOT * SLOT
    NTILE = NTOK // 128
    c = {}
    pos = np.arange(NPOS)
    rp = np.zeros((NPOS, 4), np.int32)
    rp[:, 1] = 2 * NTOK + (pos % SLOT)
    c["c_recpad"] = rp
    idx = np.arange(128)
    c["c_L"] = (idx[:, None] < idx[None, :]).astype(np.float32).astype(bf)
    c["c_ones"] = np.ones((128, 128), np.float32).astype(bf)
    m = np.ones((NE, NTILE), np.float32)
    m[:, 0] = 0.0
    c["c_mask96"] = np.ascontiguousarray(np.broadcast_to(m.reshape(1, -1), (128, NE * NTILE)))
    c["c_tokid"] = (np.arange(NTILE)[None, :] * 128 + idx[:, None]).astype(np.float32)
    c["c_slotstart"] = np.ascontiguousarray(np.broadcast_to((np.arange(NSLOT) * SLOT).astype(np.float32)[None, :], (128, NSLOT)))
    c["c_thr"] = np.ascontiguousarray(np.broadcast_to((np.arange(NTOK // SLOT) * SLOT).astype(np.float32)[None, :], (128, NTOK // SLOT)))
    addc = np.stack([idx, idx, idx, idx, idx, idx], axis=1).astype(np.float32)
    c["c_addc"] = addc
    c["c_mulc"] = np.ascontiguousarray(np.broadcast_to(np.array([128, 128, 128, 128, 128, 128], np.float32)[None, :], (128, 6)))
    return c


_WNAMES = ["norm_mix", "w_in", "w_fourier", "lb_logits", "norm_o", "w_out", "norm_ffn", "w_route_group", "w_route_expert",
           "w_exp_gate", "w_exp_up", "w_exp_down", "norm_ple", "w_ple_gate", "w_ple_proj", "norm_final"]


def _prep_weights(inp):
    f = lambda a: np.ascontiguousarray(np.asarray(a, dtype=np.float32))
    return {
        "norm_mix": f(inp["norm_mix"]).reshape(D),
        "w_in": f(inp["w_in"]).reshape(D, INW),
        "w_fourier": f(inp["w_fourier"]).reshape(4, 128, 128),
        "lb_logits": f(inp["lb_logits"]).reshape(2, 1024),
        "norm_o": f(inp["norm_o"]).reshape(HD),
        "w_out": f(inp["w_out"]).reshape(D, D),
        "norm_ffn": f(inp["norm_ffn"]).reshape(D),
        "w_route_group": f(inp["w_route_group"]).reshape(D, 4),
        "w_route_expert": f(inp["w_route_expert"]).reshape(D, NE),
        "w_exp_gate": f(inp["w_exp_gate"]).reshape(NE, D, DE),
        "w_exp_up": f(inp["w_exp_up"]).reshape(NE, D, DE),
        "w_exp_down": f(inp["w_exp_down"]).reshape(NE, DE, D),
        "norm_ple": f(inp["norm_ple"]).reshape(D),
        "w_ple_gate": f(inp["w_ple_gate"]).reshape(D, D),
        "w_ple_proj": f(inp["w_ple_proj"]).reshape(PD, D),
        "norm_final": f(inp["norm_final"]).reshape(D),
    }


_NC_CACHE = {}


def kernel(**inputs):
    ncores = 8
    xp = np.asarray(inputs["x_prompt"], dtype=np.float32)
    xs = np.asarray(inputs["x_sample"], dtype=np.float32)
    pp = np.asarray(inputs["p_prompt"], dtype=np.float32)[0]
    psm = np.asarray(inputs["p_sample"], dtype=np.float32)[0]
    B, SQ, _ = xp.shape
    DB, DS, _ = xs.shape
    npc = B // ncores
    nsc = DB // ncores
    seq_lens = tuple([SQ] * npc + [DS] * nsc)
    if seq_lens not in _NC_CACHE:
        _NC_CACHE[seq_lens] = build(list(seq_lens))
    nc = _NC_CACHE[seq_lens]
    w = _prep_weights(inputs)
    cst = dict(_consts())
    cst.update(_consts_n(sum(seq_lens)))
    in_maps = []
    for c in range(ncores):
        xc = np.concatenate([xp[c * npc:(c + 1) * npc].reshape(-1, D), xs[c * nsc:(c + 1) * nsc].reshape(-1, D)], axis=0)
        pc = np.concatenate([pp[c * npc:(c + 1) * npc].reshape(-1, PD), psm[c * nsc:(c + 1) * nsc].reshape(-1, PD)], axis=0)
        m = {"x": np.ascontiguousarray(xc), "p": np.ascontiguousarray(pc)}
        m.update(w)
        m.update(cst)
        in_maps.append(m)
    res = run_bass_kernel_spmd(nc, in_maps, core_ids=list(range(ncores)))
    yp = np.empty((B, SQ, D), np.float32)
    ys = np.empty((DB, DS, D), np.float32)
    for c in range(ncores):
        yc = np.asarray(res.results[c]["y"], dtype=np.float32)
        yp[c * npc:(c + 1) * npc] = yc[:npc * SQ].reshape(npc, SQ, D)
        ys[c * nsc:(c + 1) * nsc] = yc[npc * SQ:].reshape(nsc, DS, D)
    return (yp, ys)
```

```python
import numpy as np
import ml_dtypes
from contextlib import ExitStack
import concourse.bass as bass
import concourse.mybir as mybir
from concourse.bass_utils import run_bass_kernel_spmd

F32 = mybir.dt.float32
BF16 = mybir.dt.bfloat16
I32 = mybir.dt.int32
AF = mybir.ActivationFunctionType
ALU = mybir.AluOpType
AX = mybir.AxisListType

D = 1024
NH = 4
HD = 128
NE = 32
DE = 512
PD = 256
INW = 3072
EPS = 1e-6
NTAB = 4096
BIG = 1.0e30


class Syn:
    EPOCH = 30000

    def __init__(self, nc, es, n_dma_sems=32):
        self.nc = nc
        self.es = es
        self.eng = {"pe": nc.tensor, "dve": nc.vector, "act": nc.scalar, "pool": nc.gpsimd, "sp": nc.sync}
        self.cnt = {e: 0 for e in self.eng}
        self.sems = {e: [] for e in self.eng}
        self.know = {e: {} for e in self.eng}
        self.snap = {}
        self.last_w = {}
        self.readers = {}
        self.dma_sems = [es.enter_context(nc.semaphore(f"dq{i}")) for i in range(n_dma_sems)]
        self.dma_val = [0] * n_dma_sems
        self.dma_last_ev = [None] * n_dma_sems
        self.dma_rr = 0
        self.n_wait = 0

    def _sem_for(self, e, count):
        ep = (count - 1) // self.EPOCH
        while len(self.sems[e]) <= ep:
            self.sems[e].append(self.es.enter_context(self.nc.semaphore(f"c_{e}_{len(self.sems[e])}")))
        return self.sems[e][ep], count - ep * self.EPOCH

    def _known(self, e, ev):
        return self.know[e].get((ev[0], ev[1]), 0) >= ev[2]

    def _learn(self, e, ev):
        k = self.know[e]
        key = (ev[0], ev[1])
        if k.get(key, 0) < ev[2]:
            k[key] = ev[2]
        sn = self.snap.get(ev)
        if sn:
            for kk, vv in sn.items():
                if k.get(kk, 0) < vv:
                    k[kk] = vv

    def _wait(self, e, ev):
        if ev is None or self._known(e, ev):
            return
        if ev[0] == "e":
            sem, val = self._sem_for(ev[1], ev[2])
            self.eng[e].wait_ge(sem, val)
        else:
            self.eng[e].wait_ge(self.dma_sems[ev[1]], ev[2])
        self.n_wait += 1
        self._learn(e, ev)

    PSUM_NAMES = {"pSx", "pSy", "pj", "ptrA", "pkA", "pfB", "pscC", "poC", "pkvC", "ptrC", "pA", "pB", "pC", "pD"}

    def _is_ps(self, r):
        return (r[0] if isinstance(r, tuple) else r) in self.PSUM_NAMES

    def _deps(self, e, reads, writes):
        evs = []
        for r in reads:
            ev = self.last_w.get(r)
            ps = self._is_ps(r)
            if ev is not None and not (ps and ev[1] == e):
                evs.append(ev)
            if ps:
                evs.extend(x for x in self.readers.get(r, ()) if x[1] != e)
        for w in writes:
            ps = self._is_ps(w)
            ev = self.last_w.get(w)
            if ev is not None and not (ps and ev[1] == e):
                evs.append(ev)
            evs.extend(x for x in self.readers.get(w, ()) if not (ps and x[1] == e))
        for ev in evs:
            if ev[0] == "e" and ev[1] == "pe" and e == "pe":
                continue
            self._wait(e, ev)

    def _record(self, ev, reads, writes):
        for r in reads:
            self.readers.setdefault(r, []).append(ev)
        for w in writes:
            self.last_w[w] = ev
            self.readers[w] = []

    def op(self, e, fn, reads=(), writes=()):
        self._deps(e, reads, writes)
        inst = fn()
        self.cnt[e] += 1
        c = self.cnt[e]
        sem, _ = self._sem_for(e, c)
        inst.then_inc(sem, 1)
        ev = ("e", e, c)
        self.snap[ev] = dict(self.know[e])
        self._record(ev, reads, writes)
        return ev

    def dma(self, q, out, in_, reads=(), writes=(), **kw):
        self._deps(q, reads, writes)
        i = self.dma_rr
        self.dma_rr = (self.dma_rr + 1) % len(self.dma_sems)
        self._wait(q, self.dma_last_ev[i])
        inst = self.eng[q].dma_start(out=out, in_=in_, **kw)
        self.dma_val[i] += 16
        inst.then_inc(self.dma_sems[i], 16)
        ev = ("d", i, self.dma_val[i])
        self.snap[ev] = dict(self.know[q])
        self.dma_last_ev[i] = ev
        self._record(ev, reads, writes)
        return ev

    def idma(self, q, fn, reads=(), writes=()):
        self._deps(q, reads, writes)
        i = self.dma_rr
        self.dma_rr = (self.dma_rr + 1) % len(self.dma_sems)
        self._wait(q, self.dma_last_ev[i])
        inst = fn()
        self.dma_val[i] += 16
        inst.then_inc(self.dma_sems[i], 16)
        ev = ("d", i, self.dma_val[i])
        self.snap[ev] = dict(self.know[q])
        self.dma_last_ev[i] = ev
        self._record(ev, reads, writes)
        return ev

    def barrier(self):
        for e in self.eng:
            for ev in self.dma_last_ev:
                self._wait(e, ev)
            for f in self.eng:
                if f != e and self.cnt[f] > 0:
                    self._wait(e, ("e", f, self.cnt[f]))
        self.last_w.clear()
        self.readers.clear()

    def finish(self, q="sp"):
        for ev in self.dma_last_ev:
            self._wait(q, ev)
        for e in self.eng:
            if self.cnt[e] > 0 and e != q:
                self._wait(q, ("e", e, self.cnt[e]))


SLOT = 512


def build(seq_lens, debug=False, phases="ABCE"):
    NTOK = sum(seq_lens)
    NSLOT = (2 * NTOK) // SLOT + NE
    NPOS = NSLOT * SLOT
    NTILE_ = NTOK // 128
    assert NTOK % 1024 == 0 and all(s % 512 == 0 for s in seq_lens)
    offs = [sum(seq_lens[:i]) for i in range(len(seq_lens))]
    nc = bass.Bass("TRN2", target_bir_lowering=False)

    def din(name, shape, dt=F32):
        return nc.dram_tensor(name, shape, dt, kind="ExternalInput").ap()

    x = din("x", [NTOK, D])
    p_in = din("p", [NTOK, PD])
    norm_mix = din("norm_mix", [D])
    w_in = din("w_in", [D, INW])
    w_four = din("w_fourier", [4, 128, 128])
    lb_logits = din("lb_logits", [2, 2 * 512])
    norm_o = din("norm_o", [HD])
    w_out = din("w_out", [D, D])
    norm_ffn = din("norm_ffn", [D])
    w_rg = din("w_route_group", [D, 4])
    w_re = din("w_route_expert", [D, NE])
    w_eg = din("w_exp_gate", [NE, D, DE])
    w_eu = din("w_exp_up", [NE, D, DE])
    w_ed = din("w_exp_down", [NE, DE, D])
    norm_ple = din("norm_ple", [D])
    w_pg = din("w_ple_gate", [D, D])
    w_pp = din("w_ple_proj", [PD, D])
    norm_final = din("norm_final", [D])
    c_ident = din("c_ident", [128, 128], BF16)
    c_maskf = din("c_maskf", [128, 128], BF16)
    c_maskb = din("c_maskb", [128, 128], BF16)
    c_cc = din("c_cc", [128, 256], BF16)
    c_ctab = din("c_ctab", [NTAB, NTAB], BF16)
    c_stab = din("c_stab", [NTAB, NTAB], BF16)
    c_recpad = din("c_recpad", [NPOS, 4], I32)
    c_L = din("c_L", [128, 128], BF16)
    c_ones = din("c_ones", [128, 128], BF16)
    c_mask96 = din("c_mask96", [128, NE * NTILE_], F32)
    c_tokid = din("c_tokid", [128, NTILE_], F32)
    c_slotstart = din("c_slotstart", [128, NSLOT], F32)
    c_thr = din("c_thr", [128, NTOK // SLOT], F32)
    c_addc = din("c_addc", [128, 6], F32)
    c_mulc = din("c_mulc", [128, 6], F32)

    y = nc.dram_tensor("y", [NTOK, D], F32, kind="ExternalOutput").ap()

    skind = "ExternalOutput" if debug else "Internal"

    def dscr(name, shape, dt=BF16):
        return nc.dram_tensor(name, shape, dt, kind=skind).ap()

    ab_s = dscr("ab_s", [NTOK, 1024])
    qd_s = dscr("qd_s", [2, NH, 128, NTOK])
    kd_s = dscr("kd_s", [2, NH, 128, NTOK])
    k2_s = dscr("k2_s", [NTOK, 1024])
    v_s = dscr("v_s", [NTOK, 512])
    og_s = dscr("og_s", [NTOK, 512])
    mixT_s = dscr("mixT_s", [D, NTOK])
    x1_s = dscr("x1_s", [NTOK, D], F32)
    hn_s = dscr("hn_s", [NTOK, D])
    rec_s = dscr("rec_s", [NPOS, 4], I32)
    ybuf = dscr("ybuf", [2 * NTOK + SLOT, D])
    wbg_s = dscr("wbg_s", [NE * 128, 8 * DE])
    wbu_s = dscr("wbu_s", [NE * 128, 8 * DE])
    wbd_s = dscr("wbd_s", [NE * 128, 4 * D])
    dbg_ofwd = dscr("dbg_ofwd", [NTOK, 512]) if debug else None
    dbg_scm = dscr("dbg_scm", [128, 512]) if debug else None
    dbg_kv = nc.dram_tensor("dbg_kv", [128, 512], F32, kind="ExternalOutput").ap() if debug else None

    NCH = NTOK // 64

    with ExitStack() as es:
        def sbg(name, shape, dt):
            return es.enter_context(nc.sbuf_tensor(name, shape, dt))

        es.enter_context(nc.Block())
        S = Syn(nc, es)
        V, A, P, T = nc.vector, nc.scalar, nc.gpsimd, nc.tensor

        ident = sbg("ident", [128, 128], BF16)
        maskf = sbg("maskf", [128, 128], BF16)
        maskb = sbg("maskb", [128, 128], BF16)
        gmix = sbg("gmix", [128, 8], F32)
        gffn = sbg("gffn", [128, 8], F32)
        gple = sbg("gple", [128, 8], F32)
        gfin = sbg("gfin", [128, D], F32)
        normo = sbg("normo", [128, 1], F32)
        lbt = sbg("lbt", [128, 8], F32)
        clb = sbg("clb", [128, 8], F32)
        lbtmp = sbg("lbtmp", [128, 16], F32)
        decS = sbg("decS", [128, 2, NH, NCH], F32)
        S.dma("sp", ident[:], c_ident, writes=["ident"])
        S.dma("sp", maskf[:], c_maskf, writes=["maskf"])
        S.dma("sp", maskb[:], c_maskb, writes=["maskb"])
        for (t_, src, nm) in ((gmix, norm_mix, "gmix"), (gffn, norm_ffn, "gffn"), (gple, norm_ple, "gple")):
            S.dma("sp", t_[:], src.rearrange("(k p) -> p k", p=128), writes=[nm], allow_slow_non_contiguous=True)
        S.dma("sp", gfin[:], norm_final.partition_broadcast(128), writes=["gfin"])
        S.dma("sp", normo[:], norm_o.rearrange("(p o) -> p o", o=1), writes=["normo"])
        S.dma("sp", lbtmp[:, 0:8], lb_logits[0].rearrange("(a p) -> p a", p=128), writes=["lbtmp"], allow_slow_non_contiguous=True)
        S.dma("sp", lbtmp[:, 8:16], lb_logits[1].rearrange("(a p) -> p a", p=128), writes=["lbtmp"], allow_slow_non_contiguous=True)
        S.op("dve", lambda: V.tensor_sub(out=lbt[:], in0=lbtmp[:, 8:16], in1=lbtmp[:, 0:8]), reads=["lbtmp"], writes=["lbt"])
        S.op("act", lambda: A.activation(out=lbt[:], in_=lbt[:], func=AF.Exp), reads=["lbt"], writes=["lbt"])
        S.op("dve", lambda: V.tensor_scalar_add(out=lbt[:], in0=lbt[:], scalar1=1.0), reads=["lbt"], writes=["lbt"])
        S.op("dve", lambda: V.reciprocal(out=lbt[:], in_=lbt[:]), reads=["lbt"], writes=["lbt"])
        S.op("act", lambda: A.activation(out=clb[:], in_=lbt[:], func=AF.Ln, scale=-1.0, bias=1.0), reads=["lbt"], writes=["clb"])

        def rstd_from_ssq(ssq_ap, out_ap, n, res_r, res_w):
            S.op("act", lambda: A.activation(out=out_ap, in_=ssq_ap, func=AF.Ln, scale=1.0 / n, bias=EPS), reads=res_r, writes=res_w)
            S.op("act", lambda: A.activation(out=out_ap, in_=out_ap, func=AF.Exp, scale=-0.5), reads=res_w, writes=res_w)

        if "A" in phases:
            with ExitStack() as ph:
                def sb(name, shape, dt):
                    return ph.enter_context(nc.sbuf_tensor(name, shape, dt))

                def ps(name, shape, dt=F32):
                    return ph.enter_context(nc.psum_tensor(name, shape, dt))

                winb = sb("winb", [128, 8, INW], BF16)
                mcs = sb("mcs", [128, 4, 256], BF16)
                wfb = sb("wfb", [128, 4, 128], BF16)
                ccb = sb("ccb", [128, 256], BF16)
                scanmask = sb("scanmask", [128, 512], F32)
                xg = [sb(f"xg{i}", [128, 4, D], F32) for i in range(2)]
                xs = [sb(f"xs{i}", [128, 4, D], BF16) for i in range(2)]
                junk = sb("junkA", [128, D], BF16)
                ssq = sb("ssqA", [128, 8], F32)
                rstd = sb("rstdA", [128, 8], F32)
                hT = [sb(f"hT{i}", [128, 8, 512], BF16) for i in range(2)]
                uT = sb("uT", [128, 4, 512], BF16)
                abst = sb("abst", [128, 4, 1024], BF16)
                vst = sb("vst", [128, 4, 512], BF16)
                ogst = sb("ogst", [128, 4, 512], BF16)
                k2T = sb("k2T", [128, 8, 512], BF16)
                k2st = sb("k2st", [128, 4, 1024], BF16)
                qdst = [sb(f"qdst{i}", [128, 512], BF16) for i in range(4)]
                kdst = [sb(f"kdst{i}", [128, 512], BF16) for i in range(4)]
                NTMP = 2
                tm = [{n: sb(f"t_{n}{i}", [128, 512], F32) for n in ("e", "L1", "L2", "P", "lk", "a1", "a2", "Eb")} for i in range(NTMP)]
                ptr = [ps(f"ptrA{i}", [128, 1024], BF16) for i in range(2)]
                pj = [ps(f"pjA{i}", [128, 512], F32) for i in range(5)]
                pk = ps("pkA", [128, 1024], BF16)

                for c in range(4):
                    S.dma("pool", winb[:, :, c * 768:(c + 1) * 768],
                          w_in[:, c * 768:(c + 1) * 768].rearrange("(k p) n -> p k n", p=128), writes=[("winb", c)])
                winb_res = [("winb", c) for c in range(4)]
                S.dma("pool", wfb[:], w_four.rearrange("g c d -> c g d"), writes=["wfb"])
                S.dma("sp", ccb[:], c_cc, writes=["ccb"])
                for g in range(4):
                    for hh in range(2):
                        S.op("pe", lambda g=g, hh=hh: T.matmul(pj[hh][:, g * 128:(g + 1) * 128],
                                                                ccb[:, hh * 128:(hh + 1) * 128], wfb[:, g, :], start=True, stop=True),
                             reads=["ccb", "wfb"], writes=[("pj", hh)])
                for hh in range(2):
                    S.op("dve", lambda hh=hh: V.tensor_copy(out=mcs[:, :, hh * 128:(hh + 1) * 128],
                                                            in_=pj[hh][:, :].rearrange("p (g d) -> p g d", g=4)),
                         reads=[("pj", hh)], writes=["mcs"])
                S.op("dve", lambda: V.memset(scanmask[:], 1.0), writes=["scanmask"])
                S.op("dve", lambda: V.memset(scanmask[:].rearrange("p (n c) -> p n c", c=64)[:, :, 0:1], 0.0), reads=["scanmask"], writes=["scanmask"])

                conv_jobs = []
                if "E" in phases:
                    for e in range(NE):
                        conv_jobs.append((wbg_s[e * 128:(e + 1) * 128, :], w_eg[e].rearrange("(p k) n -> p (k n)", p=128), ("wbg_s", e)))
                        conv_jobs.append((wbu_s[e * 128:(e + 1) * 128, :], w_eu[e].rearrange("(p k) n -> p (k n)", p=128), ("wbu_s", e)))
                        conv_jobs.append((wbd_s[e * 128:(e + 1) * 128, :].rearrange("p (k n) -> p k n", k=4), w_ed[e].rearrange("(k p) n -> p k n", p=128), ("wbd_s", e)))

                def emit_conv(n_):
                    for _ in range(n_):
                        if conv_jobs:
                            o_, i_, r_ = conv_jobs.pop(0)
                            S.dma("pool", o_, i_, writes=[r_])

                pj_rr = [0]

                def next_pj():
                    i = pj_rr[0]
                    pj_rr[0] = (i + 1) % 5
                    return i

                NG = NTOK // 512
                ev_i = 0

                def a_load(G_):
                    S.dma("sp", xg[G_ % 2][:], x[G_ * 512:(G_ + 1) * 512, :].rearrange("(j p) d -> p j d", p=128), writes=[("xg", G_ % 2)])

                def a_front(G_):
                    b_ = G_ % 2
                    for j in range(4):
                        S.op("act", lambda j=j: A.activation(out=junk[:], in_=xg[b_][:, j, :], func=AF.Square, accum_out=ssq[:, b_ * 4 + j:b_ * 4 + j + 1]),
                             reads=[("xg", b_)], writes=["junkA", ("ssqA", b_)])
                    rstd_from_ssq(ssq[:, b_ * 4:(b_ + 1) * 4], rstd[:, b_ * 4:(b_ + 1) * 4], D, [("ssqA", b_)], [("rstdA", b_)])
                    for j in range(4):
                        if j % 2 == 0:
                            S.op("act", lambda j=j: A.activation(out=xs[b_][:, j, :], in_=xg[b_][:, j, :], func=AF.Copy, scale=rstd[:, b_ * 4 + j:b_ * 4 + j + 1]),
                                 reads=[("xg", b_), ("rstdA", b_)], writes=[("xs", b_, j)])
                        else:
                            S.op("dve", lambda j=j: V.tensor_scalar(out=xs[b_][:, j, :], in0=xg[b_][:, j, :], scalar1=rstd[:, b_ * 4 + j:b_ * 4 + j + 1], scalar2=None, op0=ALU.mult),
                                 reads=[("xg", b_), ("rstdA", b_)], writes=[("xs", b_, j)])
                    for k in range(8):
                        pt = ptr[(k // 2) % 2]
                        half = k % 2
                        for j in range(4):
                            S.op("pe", lambda j=j, k=k, pt=pt, half=half: T.transpose(pt[:, half * 512 + j * 128: half * 512 + (j + 1) * 128],
                                                                                      xs[b_][:, j, k * 128:(k + 1) * 128], ident[:]),
                                 reads=[("xs", b_, j), "ident"], writes=[("ptrA", (k // 2) % 2)])
                        if k % 2 == 0:
                            S.op("dve", lambda k=k, pt=pt, half=half: V.tensor_scalar(out=hT[b_][:, k, :], in0=pt[:, half * 512:(half + 1) * 512],
                                                                                      scalar1=gmix[:, k:k + 1], scalar2=None, op0=ALU.mult),
                                 reads=[("ptrA", (k // 2) % 2), "gmix"], writes=[("hT", b_, k)])
                        else:
                            S.op("act", lambda k=k, pt=pt, half=half: A.activation(out=hT[b_][:, k, :], in_=pt[:, half * 512:(half + 1) * 512],
                                                                                   func=AF.Copy, scale=gmix[:, k:k + 1]),
                                 reads=[("ptrA", (k // 2) % 2), "gmix"], writes=[("hT", b_, k)])

                a_load(0)
                if NG > 1:
                    a_load(1)
                a_front(0)
                for G in range(NG):
                    t0 = G * 512
                    b = G % 2
                    if G + 1 < NG:
                        a_front(G + 1)
                    if G + 2 < NG:
                        a_load(G + 2)
                    emit_conv((3 * NE + NG - 1) // NG if G > 0 else 0)
                    hT_res = [("hT", b, k) for k in range(8)]

                    def proj_fm(col0, pi):
                        for k in range(8):
                            S.op("pe", lambda k=k: T.matmul(pj[pi][:, :], winb[:, k, col0:col0 + 128], hT[b][:, k, :], start=(k == 0), stop=(k == 7)),
                                 reads=winb_res + hT_res, writes=[("pj", pi)])

                    for g in range(4):
                        pi = next_pj()
                        proj_fm(g * 128, pi)
                        eng = "act" if g % 2 else "dve"
                        if eng == "act":
                            S.op("act", lambda g=g, pi=pi: A.copy(out=uT[:, g, :], in_=pj[pi][:, :]), reads=[("pj", pi)], writes=[("uT", g)])
                        else:
                            S.op("dve", lambda g=g, pi=pi: V.tensor_copy(out=uT[:, g, :], in_=pj[pi][:, :]), reads=[("pj", pi)], writes=[("uT", g)])
                    for j in range(4):
                        for half in range(2):
                            pi = next_pj()
                            for gg in range(2):
                                g = half * 2 + gg
                                S.op("pe", lambda g=g, gg=gg, j=j, pi=pi: T.matmul(pj[pi][:, gg * 256:(gg + 1) * 256], uT[:, g, j * 128:(j + 1) * 128],
                                                                                mcs[:, g, :], start=True, stop=True),
                                     reads=[("uT", g), "mcs"], writes=[("pj", pi)])
                            if half == 0:
                                S.op("act", lambda j=j, half=half, pi=pi: A.copy(out=abst[:, j, half * 512:(half + 1) * 512], in_=pj[pi][:, :]),
                                     reads=[("pj", pi)], writes=[("abst", j, half)])
                            else:
                                S.op("dve", lambda j=j, half=half, pi=pi: V.tensor_copy(out=abst[:, j, half * 512:(half + 1) * 512], in_=pj[pi][:, :]),
                                     reads=[("pj", pi)], writes=[("abst", j, half)])
                    S.dma("sp", ab_s[t0:t0 + 512, :].rearrange("(j p) n -> p j n", p=128), abst[:],
                          reads=[("abst", j, hf) for j in range(4) for hf in range(2)], writes=[("ab_s", G)])

                    for (col0, st, nm) in ((1024, vst, "vst"), (2560, ogst, "ogst")):
                        for j in range(4):
                            pi = next_pj()
                            for k in range(8):
                                S.op("pe", lambda k=k, j=j, pi=pi, col0=col0: T.matmul(pj[pi][:, :], hT[b][:, k, j * 128:(j + 1) * 128],
                                                                                      winb[:, k, col0:col0 + 512], start=(k == 0), stop=(k == 7)),
                                     reads=winb_res + hT_res, writes=[("pj", pi)])
                            if j % 2 == 0:
                                S.op("act", lambda j=j, pi=pi, st=st: A.copy(out=st[:, j, :], in_=pj[pi][:, :]), reads=[("pj", pi)], writes=[(nm, j)])
                            else:
                                S.op("dve", lambda j=j, pi=pi, st=st: V.tensor_copy(out=st[:, j, :], in_=pj[pi][:, :]), reads=[("pj", pi)], writes=[(nm, j)])
                    S.dma("sp", v_s[t0:t0 + 512, :].rearrange("(j p) n -> p j n", p=128), vst[:],
                          reads=[("vst", j) for j in range(4)], writes=[("v_s", G)])
                    S.dma("sp", og_s[t0:t0 + 512, :].rearrange("(j p) n -> p j n", p=128), ogst[:],
                          reads=[("ogst", j) for j in range(4)], writes=[("og_s", G)])

                    for h in range(NH):
                        pq = next_pj()
                        proj_fm(512 + h * 128, pq)
                        pfr = [None, None]
                        pfr[0] = next_pj()
                        proj_fm(1536 + h * 128, pfr[0])
                        pfr[1] = next_pj()
                        proj_fm(2048 + h * 128, pfr[1])
                        def chain(dr):
                            tt = tm[dr]
                            ti = dr
                            R = lambda n: ("tm", ti, n)
                            fr = pj[pfr[dr]]
                            frr = ("pj", pfr[dr])
                            col = dr * 4 + h
                            qs = qdst[(2 * h + dr) % 4]
                            ks = kdst[(2 * h + dr) % 4]
                            qsr = ("qdst", (2 * h + dr) % 4)
                            ksr = ("kdst", (2 * h + dr) % 4)
                            S.op("act", lambda: A.activation(out=tt["e"][:], in_=fr[:, :], func=AF.Exp, scale=-1.0), reads=[frr], writes=[R("e")])
                            yield
                            S.op("act", lambda: A.activation(out=tt["L1"][:], in_=tt["e"][:], func=AF.Ln, bias=1.0), reads=[R("e")], writes=[R("L1")])
                            yield
                            S.op("act", lambda: A.activation(out=tt["L2"][:], in_=tt["e"][:], func=AF.Ln, scale=lbt[:, col:col + 1], bias=1.0),
                                 reads=[R("e"), "lbt"], writes=[R("L2")])
                            yield
                            S.op("dve", lambda: V.tensor_sub(out=tt["L2"][:], in0=tt["L2"][:], in1=tt["L1"][:]), reads=[R("L2"), R("L1")], writes=[R("L2")])
                            yield
                            S.op("dve", lambda: V.tensor_tensor_scan(out=tt["P"][:], data0=scanmask[:], data1=tt["L2"][:], initial=0.0, op0=ALU.mult, op1=ALU.add),
                                 reads=[R("L2"), "scanmask"], writes=[R("P")])
                            yield
                            S.op("dve", lambda: V.scalar_tensor_tensor(out=tt["lk"][:], in0=fr[:, :], scalar=-1.0, in1=tt["L1"][:], op0=ALU.mult, op1=ALU.subtract),
                                 reads=[frr, R("L1")], writes=[R("lk")])
                            yield
                            P3 = tt["P"][:].rearrange("p (n c) -> p n c", c=64)
                            Tb = P3[:, :, 63:64].to_broadcast([128, 8, 64])
                            v3 = lambda n: tt[n][:].rearrange("p (n c) -> p n c", c=64)
                            ch0 = t0 // 64
                            if dr == 0:
                                S.op("dve", lambda: V.tensor_sub(out=tt["a1"][:], in0=tt["lk"][:], in1=tt["P"][:]), reads=[R("lk"), R("P")], writes=[R("a1")])
                                yield
                                S.op("dve", lambda: V.tensor_tensor(out=v3("a2"), in0=v3("a1"), in1=Tb, op=ALU.add), reads=[R("a1"), R("P")], writes=[R("a2")])
                                yield
                                S.op("act", lambda: A.activation(out=tt["Eb"][:], in_=tt["P"][:], func=AF.Exp), reads=[R("P")], writes=[R("Eb")])
                                yield
                            else:
                                S.op("dve", lambda: V.tensor_sub(out=tt["a2"][:], in0=tt["P"][:], in1=tt["L2"][:]), reads=[R("P"), R("L2")], writes=[R("a2")])
                                yield
                                S.op("dve", lambda: V.tensor_tensor(out=v3("Eb"), in0=Tb, in1=v3("a2"), op=ALU.subtract), reads=[R("P"), R("a2")], writes=[R("Eb")])
                                yield
                                S.op("dve", lambda: V.tensor_sub(out=tt["a1"][:], in0=tt["lk"][:], in1=tt["Eb"][:]), reads=[R("lk"), R("Eb")], writes=[R("a1")])
                                yield
                                S.op("dve", lambda: V.tensor_tensor(out=tt["a2"][:], in0=tt["a2"][:], in1=tt["lk"][:], op=ALU.add), reads=[R("a2"), R("lk")], writes=[R("a2")])
                                yield
                                S.op("act", lambda: A.activation(out=tt["Eb"][:], in_=tt["Eb"][:], func=AF.Exp), reads=[R("Eb")], writes=[R("Eb")])
                                yield
                            S.op("act", lambda: A.activation(out=ks[:], in_=tt["a1"][:], func=AF.Exp, bias=clb[:, col:col + 1]), reads=[R("a1"), "clb"], writes=[ksr])
                            yield
                            S.op("act", lambda: A.activation(out=k2T[:, col, :], in_=tt["a2"][:], func=AF.Exp, bias=clb[:, col:col + 1]),
                                 reads=[R("a2"), "clb"], writes=[("k2T", col)])
                            yield
                            S.op("dve", lambda: V.scalar_tensor_tensor(out=qs[:], in0=pj[pq][:, :], scalar=float(HD) ** -0.5, in1=tt["Eb"][:], op0=ALU.mult, op1=ALU.mult),
                                 reads=[("pj", pq), R("Eb")], writes=[qsr])
                            yield
                            S.op("act", lambda: A.activation(out=decS[:, dr, h, ch0:ch0 + 8], in_=P3[:, :, 63], func=AF.Exp), reads=[R("P")], writes=[("decS", G)])
                            yield
                            S.dma("sp", qd_s[dr, h, :, t0:t0 + 512], qs[:], reads=[qsr], writes=[("qd_s", G)])
                            S.dma("sp", kd_s[dr, h, :, t0:t0 + 512], ks[:], reads=[ksr], writes=[("kd_s", G)])

                        gens_ = [chain(0), chain(1)]
                        alive_ = [True, True]
                        while any(alive_):
                            for d_ in range(2):
                                if alive_[d_]:
                                    try:
                                        next(gens_[d_])
                                    except StopIteration:
                                        alive_[d_] = False
                    for j in range(4):
                        for col in range(8):
                            S.op("pe", lambda j=j, col=col: T.transpose(pk[:, col * 128:(col + 1) * 128], k2T[:, col, j * 128:(j + 1) * 128], ident[:]),
                                 reads=[("k2T", col), "ident"], writes=["pkA"])
                        if j % 2 == 0:
                            S.op("dve", lambda j=j: V.tensor_copy(out=k2st[:, j, :], in_=pk[:, :]), reads=["pkA"], writes=[("k2st", j)])
                        else:
                            S.op("act", lambda j=j: A.copy(out=k2st[:, j, :], in_=pk[:, :]), reads=["pkA"], writes=[("k2st", j)])
                    S.dma("sp", k2_s[t0:t0 + 512, :].rearrange("(j p) n -> p j n", p=128), k2st[:],
                          reads=[("k2st", j) for j in range(4)], writes=[("k2_s", G)])
                emit_conv(3 * NE)
                S.barrier()

        if "B" in phases:
            with ExitStack() as ph:
                def sb(name, shape, dt):
                    return ph.enter_context(nc.sbuf_tensor(name, shape, dt))

                def ps(name, shape, dt=F32):
                    return ph.enter_context(nc.psum_tensor(name, shape, dt))

                NTmax = max(seq_lens) // 128
                abS = sb("abS", [128, NTmax, 1024], BF16)
                ctile = [sb(f"ctile{i}", [128, 16, 512], BF16) for i in range(2)]
                stile = [sb(f"stile{i}", [128, 16, 512], BF16) for i in range(2)]
                yst = [sb(f"ystB{i}", [128, 4, 512], BF16) for i in range(2)]
                pf = [ps(f"pfB{i}", [128, 512], F32) for i in range(8)]
                ti = 0
                cti = 0
                for si, (off, SL) in enumerate(zip(offs, seq_lens)):
                    NT = SL // 128
                    rs = NTAB // SL
                    TC = min(16, NT)
                    nkc = NT // TC
                    for q4 in range(0, NT, 8):
                        n4 = min(8, NT - q4)
                        S.dma("sp", abS[:, q4:q4 + n4, :], ab_s[off + q4 * 128: off + (q4 + n4) * 128, :].rearrange("(j p) n -> p j n", p=128),
                              reads=[("ab_s", g) for g in range(NTOK // 512)] if "A" in phases else [], writes=[("abS", q4 // 8)])
                    abS_res = [("abS", i) for i in range((NT + 7) // 8)]
                    ctab_v = c_ctab.rearrange("(r m) c -> r m c", m=rs)
                    stab_v = c_stab.rearrange("(r m) c -> r m c", m=rs)
                    for ct in range(SL // 512):
                        pset = (cti % 2) * 4
                        for kc in range(nkc):
                            tb = ti % 2
                            ti += 1
                            r0 = kc * TC * 128
                            S.dma("sp", ctile[tb][:, 0:TC, :], ctab_v[r0:r0 + TC * 128, 0, ct * 512:(ct + 1) * 512].rearrange("(j p) n -> p j n", p=128),
                                  writes=[("ctile", tb)])
                            S.dma("pool", stile[tb][:, 0:TC, :], stab_v[r0:r0 + TC * 128, 0, ct * 512:(ct + 1) * 512].rearrange("(j p) n -> p j n", p=128),
                                  writes=[("stile", tb)])
                            for t in range(TC):
                                tt_ = kc * TC + t
                                for g in range(4):
                                    S.op("pe", lambda g=g, t=t, tt_=tt_, tb=tb: T.matmul(pf[pset + g][:, :], abS[:, tt_, g * 256:g * 256 + 128], ctile[tb][:, t, :],
                                                                                   start=(tt_ == 0), stop=False),
                                         reads=abS_res + [("ctile", tb)], writes=[("pfB", pset + g)])
                                    S.op("pe", lambda g=g, t=t, tt_=tt_, tb=tb: T.matmul(pf[pset + g][:, :], abS[:, tt_, g * 256 + 128:g * 256 + 256], stile[tb][:, t, :],
                                                                                   start=False, stop=(tt_ == NT - 1)),
                                         reads=abS_res + [("stile", tb)], writes=[("pfB", pset + g)])
                        yb = cti % 2
                        sc = float((SL * 128.0) ** -0.5)
                        for g in range(4):
                            if g % 2 == 0:
                                S.op("act", lambda g=g: A.activation(out=yst[yb][:, g, :], in_=pf[pset + g][:, :], func=AF.Copy, scale=sc),
                                     reads=[("pfB", pset + g)], writes=[("ystB", yb)])
                            else:
                                S.op("dve", lambda g=g: V.tensor_scalar(out=yst[yb][:, g, :], in0=pf[pset + g][:, :], scalar1=sc, scalar2=None, op0=ALU.mult),
                                     reads=[("pfB", pset + g)], writes=[("ystB", yb)])
                        S.dma("sp", mixT_s[0:512, off + ct * 512: off + (ct + 1) * 512].rearrange("(g p) t -> p g t", p=128), yst[yb][:],
                              reads=[("ystB", yb)], writes=[("mixT_s", "four", si, ct)])
                        cti += 1
                S.barrier()

        if "C" in phases:
            with ExitStack() as ph:
                def sb(name, shape, dt):
                    return ph.enter_context(nc.sbuf_tensor(name, shape, dt))

                def ps(name, shape, dt=F32):
                    return ph.enter_context(nc.psum_tensor(name, shape, dt))

                NTmax = max(seq_lens) // 128
                osto = sb("osto", [128, NTmax, 512], BF16)
                qdb = [[sb(f"qdb{d}_{i}", [128, NH, 512], BF16) for i in range(2)] for d in range(2)]
                kdb = [[sb(f"kdb{d}_{i}", [128, NH, 512], BF16) for i in range(2)] for d in range(2)]
                k2b = [[sb(f"k2b{d}_{i}", [128, 4, 512], BF16) for i in range(2)] for d in range(2)]
                vb = [[sb(f"vb{d}_{i}", [128, 4, 512], BF16) for i in range(2)] for d in range(2)]
                ogb = [[sb(f"ogb{d}_{i}", [128, 4, 512], BF16) for i in range(2)] for d in range(2)]
                S32 = [[[sb(f"S32_{d}_{h}_{i}", [128, 128], F32) for i in range(2)] for h in range(NH)] for d in range(2)]
                Sbf = [[sb(f"Sbf_{d}_{h}", [128, 128], BF16) for h in range(NH)] for d in range(2)]
                scm = [sb(f"scm{d}", [128, 512], BF16) for d in range(2)]
                o32 = [sb(f"o32_{d}", [128, 512], F32) for d in range(2)]
                sg = [sb(f"sgC{d}", [128, 512], F32) for d in range(2)]
                mb = [sb(f"mbC{d}", [128, 512], BF16) for d in range(2)]
                junk = sb("junkC", [128, 128], BF16)
                ssq4 = [sb(f"ssq4_{d}", [128, 4], F32) for d in range(2)]
                mst = [[sb(f"mstC{d}_{i}", [128, NH, 512], BF16) for i in range(2)] for d in range(2)]
                psc = [ps(f"pscC{d}", [128, 512], F32) for d in range(2)]
                po = [ps(f"poC{d}", [128, 512], F32) for d in range(2)]
                pkv = [ps(f"pkvC{d}", [128, 512], F32) for d in range(2)]
                ptr = [ps(f"ptrC{d}", [128, 1024], BF16) for d in range(2)]
                blk_cnt = [0, 0]

                def c_step(dr, off, SL, t, second, cur):
                    NT = SL // 128
                    bi, j = t // 4, t % 4
                    tok0 = off + bi * 512
                    G = tok0 // 512
                    first_in_blk = (j == 0) if dr == 0 else (j == 3)
                    last_in_blk = (j == 3) if dr == 0 else (j == 0)
                    if first_in_blk:
                        blk_cnt[dr] += 1
                        cur["bb"] = blk_cnt[dr] % 2
                        bb = cur["bb"]
                        S.dma("sp", qdb[dr][bb][:], qd_s[dr, :, :, tok0:tok0 + 512].rearrange("h d t -> d h t"), writes=[("qdb", dr, bb)])
                        S.dma("sp", kdb[dr][bb][:], kd_s[dr, :, :, tok0:tok0 + 512].rearrange("h d t -> d h t"), writes=[("kdb", dr, bb)])
                        S.dma("sp", k2b[dr][bb][:], k2_s[tok0:tok0 + 512, dr * 512:(dr + 1) * 512].rearrange("(j p) n -> p j n", p=128), writes=[("k2b", dr, bb)])
                        S.dma("sp", vb[dr][bb][:], v_s[tok0:tok0 + 512, :].rearrange("(j p) n -> p j n", p=128), writes=[("vb", dr, bb)])
                        if (bi * 4 + 3 >= NT // 2) if dr == 0 else (bi * 4 < NT // 2):
                            S.dma("sp", ogb[dr][bb][:], og_s[tok0:tok0 + 512, :].rearrange("(j p) n -> p j n", p=128), writes=[("ogb", dr, bb)])
                    bb = cur["bb"]
                    mask = maskf if dr == 0 else maskb
                    Q, Kd, K2, Vv, OG = qdb[dr][bb], kdb[dr][bb], k2b[dr][bb], vb[dr][bb], ogb[dr][bb]
                    rq, rk, rk2, rv, rog = ("qdb", dr, bb), ("kdb", dr, bb), ("k2b", dr, bb), ("vb", dr, bb), ("ogb", dr, bb)
                    for h in range(NH):
                        S.op("pe", lambda h=h: T.matmul(psc[dr][:, h * 128:(h + 1) * 128], Kd[:, h, j * 128:(j + 1) * 128], Q[:, h, j * 128:(j + 1) * 128], start=True, stop=True),
                             reads=[rk, rq], writes=[("pscC", dr)])
                    yield
                    S.op("dve", lambda: V.tensor_tensor(out=scm[dr][:].rearrange("p (h c) -> p h c", h=NH), in0=psc[dr][:, :].rearrange("p (h c) -> p h c", h=NH),
                                                        in1=mask[:, :].unsqueeze(1).to_broadcast([128, NH, 128]), op=ALU.mult),
                         reads=[("pscC", dr), "maskf", "maskb"], writes=[("scm", dr)])
                    yield
                    corder = (0, 1) if dr == 0 else (1, 0)
                    for ci, c in enumerate(corder):
                        rows = slice(c * 64, (c + 1) * 64)
                        chunk = (tok0 + j * 128 + c * 64) // 64
                        for h in range(NH):
                            S.op("pe", lambda h=h, rows=rows, c=c: T.matmul(po[dr][rows, h * 128:(h + 1) * 128], scm[dr][rows, h * 128 + c * 64: h * 128 + (c + 1) * 64],
                                                                           Vv[rows, j, h * 128:(h + 1) * 128], start=True, stop=False),
                                 reads=[("scm", dr), rv], writes=[("poC", dr)])
                            S.op("pe", lambda h=h, rows=rows, c=c: T.matmul(po[dr][rows, h * 128:(h + 1) * 128], Q[:, h, j * 128 + c * 64: j * 128 + (c + 1) * 64],
                                                                           Sbf[dr][h][:, :], start=False, stop=True),
                                 reads=[rq, ("Sbf", dr, h)], writes=[("poC", dr)])
                            S.op("pe", lambda h=h, rows=rows: T.matmul(pkv[dr][:, h * 128:(h + 1) * 128], K2[rows, j, h * 128:(h + 1) * 128],
                                                                      Vv[rows, j, h * 128:(h + 1) * 128], start=True, stop=True),
                                 reads=[rk2, rv], writes=[("pkvC", dr)])
                        yield
                        for h in range(NH):
                            a = cur["spp"][h]
                            S.op("dve", lambda h=h, a=a, chunk=chunk: V.scalar_tensor_tensor(
                                out=Sbf[dr][h][:], in0=S32[dr][h][a][:], scalar=decS[:, dr, h, chunk:chunk + 1],
                                in1=pkv[dr][:, h * 128:(h + 1) * 128], op0=ALU.mult, op1=ALU.add),
                                 reads=[("S32", dr, h, a), ("pkvC", dr)], writes=[("Sbf", dr, h)])
                            S.op("dve", lambda h=h, a=a, chunk=chunk: V.scalar_tensor_tensor(
                                out=S32[dr][h][1 - a][:], in0=S32[dr][h][a][:], scalar=decS[:, dr, h, chunk:chunk + 1],
                                in1=pkv[dr][:, h * 128:(h + 1) * 128], op0=ALU.mult, op1=ALU.add),
                                 reads=[("S32", dr, h, a), ("pkvC", dr)], writes=[("S32", dr, h, 1 - a)])
                            cur["spp"][h] = 1 - a
                        yield
                    if not second:
                        S.op("act", lambda: A.copy(out=osto[:, t, :], in_=po[dr][:, :]), reads=[("poC", dr)], writes=[("osto", t)])
                        return
                    mi = cur["bb"]
                    S.op("dve", lambda: V.tensor_tensor(out=o32[dr][:], in0=po[dr][:, :], in1=osto[:, t, :], op=ALU.add),
                         reads=[("poC", dr), ("osto", t)], writes=[("o32", dr)])
                    for h in range(NH):
                        S.op("act", lambda h=h: A.activation(out=junk[:], in_=o32[dr][:, h * 128:(h + 1) * 128], func=AF.Square, accum_out=ssq4[dr][:, h:h + 1]),
                             reads=[("o32", dr)], writes=["junkC", ("ssq4", dr)])
                    rstd_from_ssq(ssq4[dr][:], ssq4[dr][:], HD, [("ssq4", dr)], [("ssq4", dr)])
                    S.op("act", lambda: A.activation(out=sg[dr][:], in_=OG[:, j, :], func=AF.Exp, scale=-1.0), reads=[rog], writes=[("sgC", dr)])
                    yield
                    S.op("act", lambda: A.activation(out=sg[dr][:], in_=sg[dr][:], func=AF.Ln, bias=1.0), reads=[("sgC", dr)], writes=[("sgC", dr)])
                    S.op("act", lambda: A.activation(out=sg[dr][:], in_=sg[dr][:], func=AF.Exp, scale=-1.0), reads=[("sgC", dr)], writes=[("sgC", dr)])
                    S.op("dve", lambda: V.tensor_tensor(out=o32[dr][:], in0=o32[dr][:], in1=OG[:, j, :], op=ALU.mult), reads=[("o32", dr), rog], writes=[("o32", dr)])
                    S.op("dve", lambda: V.tensor_tensor(out=sg[dr][:], in0=sg[dr][:], in1=o32[dr][:], op=ALU.mult), reads=[("sgC", dr), ("o32", dr)], writes=[("sgC", dr)])
                    S.op("dve", lambda: V.tensor_tensor(out=mb[dr][:].rearrange("p (h c) -> p h c", h=NH), in0=sg[dr][:].rearrange("p (h c) -> p h c", h=NH),
                                                        in1=ssq4[dr][:, :].unsqueeze(2).to_broadcast([128, NH, 128]), op=ALU.mult),
                         reads=[("sgC", dr), ("ssq4", dr)], writes=[("mbC", dr)])
                    yield
                    for h in range(NH):
                        S.op("pe", lambda h=h: T.transpose(ptr[dr][:, h * 128:(h + 1) * 128], mb[dr][:, h * 128:(h + 1) * 128], ident[:]),
                             reads=[("mbC", dr), "ident"], writes=[("ptrC", dr)])
                    yield
                    S.op("act", lambda: A.activation(out=mst[dr][mi][:, :, j * 128:(j + 1) * 128], in_=ptr[dr][:, 0:512].rearrange("p (h t) -> p h t", h=NH),
                                                     func=AF.Copy, scale=normo[:, 0:1]),
                         reads=[("ptrC", dr), "normo"], writes=[("mstC", dr, mi)])
                    whole = (bi * 4 >= NT // 2) if dr == 0 else (bi * 4 + 3 < NT // 2)
                    if whole:
                        if last_in_blk:
                            S.dma("sp", mixT_s[512:1024, tok0:tok0 + 512].rearrange("(h p) t -> p h t", p=128), mst[dr][mi][:],
                                  reads=[("mstC", dr, mi)], writes=[("mixT_s", "hg", G)])
                    else:
                        S.dma("sp", mixT_s[512:1024, tok0 + j * 128:tok0 + (j + 1) * 128].rearrange("(h p) t -> p h t", p=128), mst[dr][mi][:, :, j * 128:(j + 1) * 128],
                              reads=[("mstC", dr, mi)], writes=[("mixT_s", "hg", G, j)])

                for si, (off, SL) in enumerate(zip(offs, seq_lens)):
                    NT = SL // 128
                    curs = [{"spp": [0] * NH, "bb": 0}, {"spp": [0] * NH, "bb": 0}]
                    for d in range(2):
                        for h in range(NH):
                            S.op("dve", lambda d=d, h=h: V.memset(S32[d][h][0][:], 0.0), writes=[("S32", d, h, 0)])
                            S.op("dve", lambda d=d, h=h: V.memset(Sbf[d][h][:], 0.0), writes=[("Sbf", d, h)])
                    for k in range(NT):
                        tf, tb_ = k, NT - 1 - k
                        gens = [c_step(0, off, SL, tf, tf >= NT // 2, curs[0]), c_step(1, off, SL, tb_, tb_ < NT // 2, curs[1])]
                        alive = [True, True]
                        while any(alive):
                            for d in range(2):
                                if alive[d]:
                                    try:
                                        next(gens[d])
                                    except StopIteration:
                                        alive[d] = False
                S.barrier()

        if "D" in phases:
            with ExitStack() as ph:
                def sb(name, shape, dt):
                    return ph.enter_context(nc.sbuf_tensor(name, shape, dt))

                def ps(name, shape, dt=F32):
                    return ph.enter_context(nc.psum_tensor(name, shape, dt))

                TS = 1024
                NTI = TS // 128
                woutb = sb("woutb", [128, 8, D], BF16)
                wpgb = sb("wpgb", [128, 8, D], BF16)
                wppb = sb("wppb", [128, 2, D], BF16)
                wrb = sb("wrb", [128, 8, 36], BF16)
                x1 = sb("x1", [128, NTI, D], F32)
                hnT = sb("hnT", [128, 8, TS], BF16)
                mt = [sb(f"mtD{i}", [128, 8, 512], BF16) for i in range(2)]
                NWB = 2
                wgb = [sb(f"wgb{i}", [128, 8, DE], BF16) for i in range(NWB)]
                wub = [sb(f"wub{i}", [128, 8, DE], BF16) for i in range(NWB)]
                wdb = [sb(f"wdb{i}", [128, 4, D], BF16) for i in range(NWB)]
                hid = [sb(f"hidD{i}", [128, 4, 512], BF16) for i in range(2)]
                sgt = [sb(f"sgD{i}", [128, 512], F32) for i in range(2)]
                xsb = [sb(f"xsD{i}", [128, D], BF16) for i in range(2)]
                junk = sb("junkD", [128, D], BF16)
                ssq = sb("ssqD", [128, NTI], F32)
                rstd = sb("rstdD", [128, NTI], F32)
                ssq3 = sb("ssq3D", [128, NTI], F32)
                rstd3 = sb("rstd3D", [128, NTI], F32)
                rl = sb("rlD", [128, NTI, 36], F32)
                gates = sb("gatesD", [128, NTI, NE], F32)
                r_mg = sb("r_mg", [128, NTI], F32)
                r_gm = sb("r_gm", [128, NTI, 4], F32)
                r_eg = sb("r_eg", [128, NTI, 4], F32)
                r_sg = sb("r_sg", [128, NTI], F32)
                r_lem = sb("r_lem", [128, NTI, NE], F32)
                r_lem2 = sb("r_lem2", [128, NTI, NE], F32)
                r_oh1 = sb("r_oh1", [128, NTI, NE], F32)
                r_oh2 = sb("r_oh2", [128, NTI, NE], F32)
                r_m1 = sb("r_m1", [128, NTI], F32)
                r_m2 = sb("r_m2", [128, NTI], F32)
                r_w1 = sb("r_w1", [128, NTI], F32)
                r_w2 = sb("r_w2", [128, NTI], F32)
                pld = sb("pld", [128, NTI, PD], F32)
                pbf = [sb(f"pbf{i}", [128, PD], BF16) for i in range(2)]
                ptT = [sb(f"ptT{i}", [128, 2, 128], BF16) for i in range(2)]
                hpT = [sb(f"hpT{i}", [128, 8, 128], BF16) for i in range(2)]
                sig = [sb(f"sigD{i}", [128, D], F32) for i in range(2)]
                pA = ps("pA", [128, 1024], F32)
                pB = ps("pB", [128, 1024], F32)
                pC = ps("pC", [128, 1024], F32)
                pD_ = ps("pD", [128, 1024], F32)
                pCb = pC[:, :].bitcast(BF16)
                pDb = pD_[:, :].bitcast(BF16)

                S.dma("pool", woutb[:], w_out.rearrange("(k p) n -> p k n", p=128), writes=["woutb"])
                S.dma("pool", wpgb[:], w_pg.rearrange("(k p) n -> p k n", p=128), writes=["wpgb"])
                S.dma("pool", wppb[:], w_pp.rearrange("(k p) n -> p k n", p=128), writes=["wppb"])
                S.dma("pool", wrb[:, :, 0:4], w_rg.rearrange("(k p) n -> p k n", p=128), writes=["wrb"])
                S.dma("pool", wrb[:, :, 4:36], w_re.rearrange("(k p) n -> p k n", p=128), writes=["wrb"])

                def norm_T(xin_ap, xin_res, rstd_col, rstd_res, gain, gain_res, out_ap, out_res, pbank, pbank_res, xsi):
                    S.op("act", lambda: A.activation(out=xsb[xsi][:], in_=xin_ap, func=AF.Copy, scale=rstd_col),
                         reads=xin_res + [rstd_res], writes=[("xsD", xsi)])
                    for k in range(8):
                        S.op("pe", lambda k=k: T.transpose(pbank[:, k * 128:(k + 1) * 128], xsb[xsi][:, k * 128:(k + 1) * 128], ident[:]),
                             reads=[("xsD", xsi), "ident"], writes=[pbank_res])
                    S.op("dve", lambda: V.tensor_tensor(out=out_ap, in0=pbank.rearrange("p (k t) -> p k t", k=8),
                                                        in1=gain[:, :].unsqueeze(2).to_broadcast([128, 8, 128]), op=ALU.mult),
                         reads=[pbank_res, gain_res], writes=[out_res])

                def sumsq(i, col_t, res):
                    S.op("act", lambda: A.activation(out=junk[:], in_=x1[:, i, :], func=AF.Square, accum_out=col_t[:, i:i + 1]),
                         reads=[("x1", i)], writes=["junkD", res])

                NST = NTOK // TS
                wload_i = [0]
                nxt_w = [None]

                def load_expert(e):
                    wb = wload_i[0] % NWB
                    wload_i[0] += 1
                    S.dma("pool", wgb[wb][:], w_eg[e].rearrange("(k p) n -> p k n", p=128), writes=[("wgb", wb)])
                    S.dma("pool", wub[wb][:], w_eu[e].rearrange("(k p) n -> p k n", p=128), writes=[("wub", wb)])
                    S.dma("pool", wdb[wb][:], w_ed[e].rearrange("(k p) n -> p k n", p=128), writes=[("wdb", wb)])
                    return wb

                for st in range(NST):
                    T0 = st * TS
                    G0 = T0 // 512
                    mix_reads = [("mixT_s", "hg", G0), ("mixT_s", "hg", G0 + 1)] if "C" in phases else []
                    S.dma("sp", pld[:], p_in[T0:T0 + TS, :].rearrange("(j p) n -> p j n", p=128), writes=["pld"])
                    for hf in range(2):
                        S.dma("sp", mt[hf][:], mixT_s[:, T0 + hf * 512:T0 + (hf + 1) * 512].rearrange("(k p) t -> p k t", p=128),
                              reads=mix_reads, writes=[("mtD", hf)])
                    for i in range(NTI):
                        S.dma("sp", x1[:, i, :], x[T0 + i * 128:T0 + (i + 1) * 128, :], writes=[("x1", i)])
                    for i in range(NTI):
                        pO = pA if i % 2 == 0 else pB
                        pOr = "pA" if i % 2 == 0 else "pB"
                        hf = i // 4
                        jj = i % 4
                        for half in range(2):
                            for k in range(8):
                                S.op("pe", lambda k=k, half=half: T.matmul(pO[:, half * 512:(half + 1) * 512], mt[hf][:, k, jj * 128:(jj + 1) * 128],
                                                                          woutb[:, k, half * 512:(half + 1) * 512], start=(k == 0), stop=(k == 7)),
                                     reads=[("mtD", hf), "woutb"], writes=[(pOr, half)])
                        S.op("dve", lambda i=i: V.tensor_tensor(out=x1[:, i, :], in0=pO[:, :], in1=x1[:, i, :], op=ALU.add),
                             reads=[(pOr, 0), (pOr, 1), ("x1", i)], writes=[("x1", i)])
                        sumsq(i, ssq, "ssqD")
                    rstd_from_ssq(ssq[:], rstd[:], D, ["ssqD"], ["rstdD"])
                    for i in range(NTI):
                        pbk = pCb[:, (i % 2) * 1024:(i % 2 + 1) * 1024]
                        norm_T(x1[:, i, :], [("x1", i)], rstd[:, i:i + 1], "rstdD", gffn, "gffn",
                               hnT[:, :, i * 128:(i + 1) * 128], ("hnT", i), pbk, ("pC", i % 2), i % 2)
                        for k in range(8):
                            S.op("pe", lambda k=k, i=i: T.matmul(pD_[:, (i % 2) * 512:(i % 2) * 512 + 36], hnT[:, k, i * 128:(i + 1) * 128], wrb[:, k, :],
                                                                start=(k == 0), stop=(k == 7)),
                                 reads=[("hnT", i), "wrb"], writes=[("pD", i % 2)])
                        S.op("act", lambda i=i: A.copy(out=rl[:, i, :], in_=pD_[:, (i % 2) * 512:(i % 2) * 512 + 36]), reads=[("pD", i % 2)], writes=[("rl", i)])
                    rl_res = [("rl", i) for i in range(NTI)]
                    lg = rl[:, :, 0:4]
                    le = rl[:, :, 4:36]
                    S.op("dve", lambda: V.tensor_reduce(out=r_mg[:], in_=lg, axis=AX.X, op=ALU.max), reads=rl_res, writes=["r_mg"])
                    mgb = r_mg[:, :].unsqueeze(2).to_broadcast([128, NTI, 4])
                    S.op("dve", lambda: V.tensor_tensor(out=r_gm[:], in0=lg, in1=mgb, op=ALU.is_equal), reads=rl_res + ["r_mg"], writes=["r_gm"])
                    S.op("dve", lambda: V.tensor_tensor(out=r_eg[:], in0=lg, in1=mgb, op=ALU.subtract), reads=rl_res + ["r_mg"], writes=["r_eg"])
                    S.op("act", lambda: A.activation(out=r_eg[:], in_=r_eg[:], func=AF.Exp), reads=["r_eg"], writes=["r_eg"])
                    S.op("dve", lambda: V.tensor_reduce(out=r_sg[:], in_=r_eg[:], axis=AX.X, op=ALU.add), reads=["r_eg"], writes=["r_sg"])
                    S.op("dve", lambda: V.reciprocal(out=r_sg[:], in_=r_sg[:]), reads=["r_sg"], writes=["r_sg"])
                    S.op("dve", lambda: V.tensor_scalar(out=r_gm[:], in0=r_gm[:], scalar1=1.0, scalar2=BIG, op0=ALU.subtract, op1=ALU.mult), reads=["r_gm"], writes=["r_gm"])
                    for i in range(NTI):
                        S.op("dve", lambda i=i: V.tensor_tensor(out=r_lem[:, i, :].rearrange("p (g j) -> p g j", g=4),
                                                                in0=rl[:, i, 4:36].rearrange("p (g j) -> p g j", g=4),
                                                                in1=r_gm[:, i, :].unsqueeze(2).to_broadcast([128, 4, 8]), op=ALU.add),
                             reads=rl_res + ["r_gm"], writes=["r_lem"])
                    S.op("dve", lambda: V.tensor_reduce(out=r_m1[:], in_=r_lem[:], axis=AX.X, op=ALU.max), reads=["r_lem"], writes=["r_m1"])
                    S.op("dve", lambda: V.tensor_tensor(out=r_oh1[:], in0=r_lem[:], in1=r_m1[:, :].unsqueeze(2).to_broadcast([128, NTI, NE]), op=ALU.is_equal),
                         reads=["r_lem", "r_m1"], writes=["r_oh1"])
                    S.op("dve", lambda: V.scalar_tensor_tensor(out=r_lem2[:], in0=r_oh1[:], scalar=-BIG, in1=r_lem[:], op0=ALU.mult, op1=ALU.add),
                         reads=["r_oh1", "r_lem"], writes=["r_lem2"])
                    S.op("dve", lambda: V.tensor_reduce(out=r_m2[:], in_=r_lem2[:], axis=AX.X, op=ALU.max), reads=["r_lem2"], writes=["r_m2"])
                    S.op("dve", lambda: V.tensor_tensor(out=r_oh2[:], in0=r_lem2[:], in1=r_m2[:, :].unsqueeze(2).to_broadcast([128, NTI, NE]), op=ALU.is_equal),
                         reads=["r_lem2", "r_m2"], writes=["r_oh2"])
                    S.op("dve", lambda: V.tensor_sub(out=r_m2[:], in0=r_m2[:], in1=r_m1[:]), reads=["r_m2", "r_m1"], writes=["r_m2"])
                    S.op("act", lambda: A.activation(out=r_m2[:], in_=r_m2[:], func=AF.Exp), reads=["r_m2"], writes=["r_m2"])
                    S.op("dve", lambda: V.tensor_scalar_add(out=r_w1[:], in0=r_m2[:], scalar1=1.0), reads=["r_m2"], writes=["r_w1"])
                    S.op("dve", lambda: V.reciprocal(out=r_w1[:], in_=r_w1[:]), reads=["r_w1"], writes=["r_w1"])
                    S.op("dve", lambda: V.tensor_mul(out=r_w1[:], in0=r_w1[:], in1=r_sg[:]), reads=["r_w1", "r_sg"], writes=["r_w1"])
                    S.op("dve", lambda: V.tensor_mul(out=r_w2[:], in0=r_w1[:], in1=r_m2[:]), reads=["r_w1", "r_m2"], writes=["r_w2"])
                    S.op("dve", lambda: V.tensor_tensor(out=r_oh1[:], in0=r_oh1[:], in1=r_w1[:, :].unsqueeze(2).to_broadcast([128, NTI, NE]), op=ALU.mult),
                         reads=["r_oh1", "r_w1"], writes=["r_oh1"])
                    S.op("dve", lambda: V.tensor_tensor(out=r_oh2[:], in0=r_oh2[:], in1=r_w2[:, :].unsqueeze(2).to_broadcast([128, NTI, NE]), op=ALU.mult),
                         reads=["r_oh2", "r_w2"], writes=["r_oh2"])
                    S.op("dve", lambda: V.tensor_add(out=gates[:], in0=r_oh1[:], in1=r_oh2[:]), reads=["r_oh1", "r_oh2"], writes=["gates"])

                    hnT_res = [("hnT", i) for i in range(NTI)]
                    hi = 0
                    di = 0
                    for e in range(NE):
                        if nxt_w[0] is None:
                            nxt_w[0] = load_expert(e)
                        wb = nxt_w[0]
                        if e + 1 < NE:
                            nxt_w[0] = load_expert(e + 1)
                        elif st + 1 < NST:
                            nxt_w[0] = load_expert(0)
                        else:
                            nxt_w[0] = None
                        for hf in range(2):
                            hb = hi % 2
                            hi += 1
                            for c in range(4):
                                pGU = pA if c % 2 == 0 else pB
                                pGUr = "pA" if c % 2 == 0 else "pB"
                                for k in range(8):
                                    S.op("pe", lambda k=k, c=c: T.matmul(pGU[:, 0:512], wgb[wb][:, k, c * 128:(c + 1) * 128], hnT[:, k, hf * 512:(hf + 1) * 512],
                                                                        start=(k == 0), stop=(k == 7)),
                                         reads=[("wgb", wb)] + hnT_res, writes=[(pGUr, 0)])
                                for k in range(8):
                                    S.op("pe", lambda k=k, c=c: T.matmul(pGU[:, 512:1024], wub[wb][:, k, c * 128:(c + 1) * 128], hnT[:, k, hf * 512:(hf + 1) * 512],
                                                                        start=(k == 0), stop=(k == 7)),
                                         reads=[("wub", wb)] + hnT_res, writes=[(pGUr, 1)])
                                S.op("act", lambda c=c: A.activation(out=sgt[c % 2][:], in_=pGU[:, 0:512], func=AF.Silu), reads=[(pGUr, 0)], writes=[("sgD", c % 2)])
                                S.op("dve", lambda c=c: V.tensor_tensor(out=hid[hb][:, c, :], in0=pGU[:, 512:1024], in1=sgt[c % 2][:], op=ALU.mult),
                                     reads=[(pGUr, 1), ("sgD", c % 2)], writes=[("hid", hb, c)])
                            for jj in range(4):
                                i = hf * 4 + jj
                                pDn = pC if di % 2 == 0 else pD_
                                pDr = "pC" if di % 2 == 0 else "pD"
                                di += 1
                                for half in range(2):
                                    for c in range(4):
                                        S.op("pe", lambda c=c, half=half, jj=jj: T.matmul(pDn[:, half * 512:(half + 1) * 512], hid[hb][:, c, jj * 128:(jj + 1) * 128],
                                                                                       wdb[wb][:, c, half * 512:(half + 1) * 512], start=(c == 0), stop=(c == 3)),
                                             reads=[("hid", hb, c), ("wdb", wb)], writes=[(pDr, half)])
                                S.op("dve", lambda i=i, e=e: V.scalar_tensor_tensor(out=x1[:, i, :], in0=pDn[:, :], scalar=gates[:, i, e:e + 1], in1=x1[:, i, :],
                                                                                   op0=ALU.mult, op1=ALU.add),
                                     reads=[(pDr, 0), (pDr, 1), "gates", ("x1", i)], writes=[("x1", i)])
                    for i in range(NTI):
                        sumsq(i, ssq, "ssqD")
                    rstd_from_ssq(ssq[:], rstd[:], D, ["ssqD"], ["rstdD"])
                    for i in range(NTI):
                        b2 = i % 2
                        pbk = pCb[:, b2 * 1024:(b2 + 1) * 1024]
                        norm_T(x1[:, i, :], [("x1", i)], rstd[:, i:i + 1], "rstdD", gple, "gple",
                               hpT[b2][:], ("hpT", b2), pbk, ("pC", b2), b2)
                        S.op("act", lambda i=i: A.copy(out=pbf[b2][:], in_=pld[:, i, :]), reads=["pld"], writes=[("pbf", b2)])
                        for k in range(2):
                            S.op("pe", lambda k=k: T.transpose(pDb[:, b2 * 1024 + k * 128: b2 * 1024 + (k + 1) * 128], pbf[b2][:, k * 128:(k + 1) * 128], ident[:]),
                                 reads=[("pbf", b2), "ident"], writes=[("pD", b2)])
                        S.op("act", lambda: A.copy(out=ptT[b2][:], in_=pDb[:, b2 * 1024: b2 * 1024 + 256].rearrange("p (k t) -> p k t", k=2)),
                             reads=[("pD", b2)], writes=[("ptT", b2)])
                        for half in range(2):
                            for k in range(8):
                                S.op("pe", lambda k=k, half=half: T.matmul(pA[:, half * 512:(half + 1) * 512], hpT[b2][:, k, :], wpgb[:, k, half * 512:(half + 1) * 512],
                                                                          start=(k == 0), stop=(k == 7)),
                                     reads=[("hpT", b2), "wpgb"], writes=[("pA", half)])
                        for half in range(2):
                            for k in range(2):
                                S.op("pe", lambda k=k, half=half: T.matmul(pB[:, half * 512:(half + 1) * 512], ptT[b2][:, k, :], wppb[:, k, half * 512:(half + 1) * 512],
                                                                          start=(k == 0), stop=(k == 1)),
                                     reads=[("ptT", b2), "wppb"], writes=[("pB", half)])
                        S.op("act", lambda: A.activation(out=sig[b2][:], in_=pA[:, :], func=AF.Sigmoid), reads=[("pA", 0), ("pA", 1)], writes=[("sigD", b2)])
                        S.op("dve", lambda: V.tensor_tensor(out=sig[b2][:], in0=pB[:, :], in1=sig[b2][:], op=ALU.mult),
                             reads=[("pB", 0), ("pB", 1), ("sigD", b2)], writes=[("sigD", b2)])
                        S.op("dve", lambda i=i: V.tensor_tensor(out=x1[:, i, :], in0=sig[b2][:], in1=x1[:, i, :], op=ALU.add),
                             reads=[("sigD", b2), ("x1", i)], writes=[("x1", i)])
                        sumsq(i, ssq3, "ssq3D")
                    rstd_from_ssq(ssq3[:], rstd3[:], D, ["ssq3D"], ["rstd3D"])
                    for i in range(NTI):
                        b2 = i % 2
                        S.op("dve", lambda i=i: V.scalar_tensor_tensor(out=sig[b2][:], in0=x1[:, i, :], scalar=rstd3[:, i:i + 1], in1=gfin[:], op0=ALU.mult, op1=ALU.mult),
                             reads=[("x1", i), "rstd3D", "gfin"], writes=[("sigD", b2)])
                        S.dma("sp", y[T0 + i * 128:T0 + (i + 1) * 128, :], sig[b2][:], reads=[("sigD", b2)], writes=[("y", st, i)])
                S.barrier()

        if "E" in phases:
            esub = [c for c in phases if c.isdigit()] or ["0", "1", "2", "3"]
            TS = 1024
            NTI = TS // 128
            NTILE = NTOK // 128
            NST = NTOK // TS
            with ExitStack() as phE:
                def sbE(name, shape, dt):
                    return phE.enter_context(nc.sbuf_tensor(name, shape, dt))

                OH1 = sbE("OH1", [128, NTILE, NE], F32)
                OH2 = sbE("OH2", [128, NTILE, NE], F32)
                W12 = sbE("W12", [128, NTILE, 2], F32)
                WI = sbE("WI", [128, NSLOT, 6], I32)
                S.dma("sp", rec_s, c_recpad, writes=["rec_s"])

                with ExitStack() as ph:
                    if "0" in esub:
                        def sb(name, shape, dt):
                            return ph.enter_context(nc.sbuf_tensor(name + '_e0', shape, dt))

                        def ps(name, shape, dt=F32):
                            return ph.enter_context(nc.psum_tensor(name + '_e0', shape, dt))

                        woutb = sb("woutb", [128, 8, D], BF16)
                        wrb = sb("wrb", [128, 8, 36], BF16)
                        gffb = sb("gffb", [128, D], F32)
                        x1 = sb("x1", [128, 2 * NTI, D], F32)
                        mt = [sb(f"mtD{i}", [128, 8, 512], BF16) for i in range(4)]
                        hnb = [sb(f"hnb{i}", [128, D], BF16) for i in range(2)]
                        hnT = [sb(f"hnTt{i}", [128, 8, 128], BF16) for i in range(2)]
                        junk = sb("junkD", [128, D], BF16)
                        ssq = sb("ssqD", [128, NTI], F32)
                        rstd = sb("rstdD", [128, NTI], F32)
                        rl = sb("rlD", [128, NTI, 36], F32)
                        r_mg = sb("r_mg", [128, NTI], F32)
                        r_gm = sb("r_gm", [128, NTI, 4], F32)
                        r_eg = sb("r_eg", [128, NTI, 4], F32)
                        r_sg = sb("r_sg", [128, NTI], F32)
                        r_lem = sb("r_lem", [128, NTI, NE], F32)
                        r_lem2 = sb("r_lem2", [128, NTI, NE], F32)
                        r_m1 = sb("r_m1", [128, NTI], F32)
                        r_m2 = sb("r_m2", [128, NTI], F32)
                        r_w1 = sb("r_w1", [128, NTI], F32)
                        pA = ps("pA", [128, 1024], F32)
                        pB = ps("pB", [128, 1024], F32)
                        pC = ps("pC", [128, 1024], F32)
                        pD_ = ps("pD", [128, 1024], F32)
                        pCb = pC[:, :].bitcast(BF16)
                        S.dma("pool", woutb[:], w_out.rearrange("(k p) n -> p k n", p=128), writes=["woutb"])
                        S.dma("pool", wrb[:, :, 0:4], w_rg.rearrange("(k p) n -> p k n", p=128), writes=["wrb"])
                        S.dma("pool", wrb[:, :, 4:36], w_re.rearrange("(k p) n -> p k n", p=128), writes=["wrb"])
                        S.dma("sp", gffb[:], norm_ffn.partition_broadcast(128), writes=["gffb"])
                        def e0_loads(st_):
                            T0_ = st_ * TS
                            xo_ = (st_ % 2) * NTI
                            for hf_ in range(2):
                                S.dma("pool", mt[(st_ % 2) * 2 + hf_][:], mixT_s[:, T0_ + hf_ * 512:T0_ + (hf_ + 1) * 512].rearrange("(k p) t -> p k t", p=128),
                                      writes=[("mtD", (st_ % 2) * 2 + hf_)])
                            for i_ in range(NTI):
                                S.dma("pool", x1[:, xo_ + i_, :], x[T0_ + i_ * 128:T0_ + (i_ + 1) * 128, :], writes=[("x1", xo_ + i_)])

                        e0_loads(0)
                        for st in range(NST):
                            T0 = st * TS
                            xo = (st % 2) * NTI
                            mo = (st % 2) * 2
                            if st + 1 < NST:
                                e0_loads(st + 1)
                            for i in range(NTI):
                                pO = pA if i % 2 == 0 else pB
                                pOr = "pA" if i % 2 == 0 else "pB"
                                hf = i // 4
                                jj = i % 4
                                for half in range(2):
                                    for k in range(8):
                                        S.op("pe", lambda k=k, half=half: T.matmul(pO[:, half * 512:(half + 1) * 512], mt[mo + hf][:, k, jj * 128:(jj + 1) * 128],
                                                                                  woutb[:, k, half * 512:(half + 1) * 512], start=(k == 0), stop=(k == 7)),
                                             reads=[("mtD", mo + hf), "woutb"], writes=[(pOr, half)])
                                S.op("dve", lambda i=i: V.tensor_tensor(out=x1[:, xo + i, :], in0=pO[:, :], in1=x1[:, xo + i, :], op=ALU.add),
                                     reads=[(pOr, 0), (pOr, 1), ("x1", xo + i)], writes=[("x1", xo + i)])
                                S.op("act", lambda i=i: A.activation(out=junk[:], in_=x1[:, xo + i, :], func=AF.Square, accum_out=ssq[:, i:i + 1]),
                                     reads=[("x1", xo + i)], writes=["junkD", "ssqD"])
                                S.dma("sp", x1_s[T0 + i * 128:T0 + (i + 1) * 128, :], x1[:, xo + i, :], reads=[("x1", xo + i)], writes=[("x1_s", st, i)])
                            rstd_from_ssq(ssq[:], rstd[:], D, ["ssqD"], ["rstdD"])
                            for i in range(NTI):
                                b2 = i % 2
                                S.op("dve", lambda i=i: V.scalar_tensor_tensor(out=hnb[b2][:], in0=x1[:, xo + i, :], scalar=rstd[:, i:i + 1], in1=gffb[:], op0=ALU.mult, op1=ALU.mult),
                                     reads=[("x1", xo + i), "rstdD", "gffb"], writes=[("hnb", b2)])
                                S.dma("sp", hn_s[T0 + i * 128:T0 + (i + 1) * 128, :], hnb[b2][:], reads=[("hnb", b2)], writes=[("hn_s", st, i)])
                                pbk = pCb[:, b2 * 1024:(b2 + 1) * 1024]
                                for k in range(8):
                                    S.op("pe", lambda k=k: T.transpose(pbk[:, k * 128:(k + 1) * 128], hnb[b2][:, k * 128:(k + 1) * 128], ident[:]),
                                         reads=[("hnb", b2), "ident"], writes=[("pC", b2)])
                                S.op("act", lambda: A.copy(out=hnT[b2][:], in_=pbk.rearrange("p (k t) -> p k t", k=8)), reads=[("pC", b2)], writes=[("hnTt", b2)])
                                for k in range(8):
                                    S.op("pe", lambda k=k: T.matmul(pD_[:, b2 * 512:b2 * 512 + 36], hnT[b2][:, k, :], wrb[:, k, :], start=(k == 0), stop=(k == 7)),
                                         reads=[("hnTt", b2), "wrb"], writes=[("pD", b2)])
                                S.op("act", lambda i=i: A.copy(out=rl[:, i, :], in_=pD_[:, b2 * 512:b2 * 512 + 36]), reads=[("pD", b2)], writes=[("rl", i)])
                            tsl = slice(st * NTI, (st + 1) * NTI)
                            oh1 = OH1[:, tsl, :]
                            oh2 = OH2[:, tsl, :]
                            rl_res = [("rl", i) for i in range(NTI)]
                            lg = rl[:, :, 0:4]
                            S.op("dve", lambda: V.tensor_reduce(out=r_mg[:], in_=lg, axis=AX.X, op=ALU.max), reads=rl_res, writes=["r_mg"])
                            mgb = r_mg[:, :].unsqueeze(2).to_broadcast([128, NTI, 4])
                            S.op("dve", lambda: V.tensor_tensor(out=r_gm[:], in0=lg, in1=mgb, op=ALU.is_equal), reads=rl_res + ["r_mg"], writes=["r_gm"])
                            S.op("dve", lambda: V.tensor_tensor(out=r_eg[:], in0=lg, in1=mgb, op=ALU.subtract), reads=rl_res + ["r_mg"], writes=["r_eg"])
                            S.op("act", lambda: A.activation(out=r_eg[:], in_=r_eg[:], func=AF.Exp), reads=["r_eg"], writes=["r_eg"])
                            S.op("dve", lambda: V.tensor_reduce(out=r_sg[:], in_=r_eg[:], axis=AX.X, op=ALU.add), reads=["r_eg"], writes=["r_sg"])
                            S.op("dve", lambda: V.reciprocal(out=r_sg[:], in_=r_sg[:]), reads=["r_sg"], writes=["r_sg"])
                            S.op("dve", lambda: V.tensor_scalar(out=r_gm[:], in0=r_gm[:], scalar1=1.0, scalar2=BIG, op0=ALU.subtract, op1=ALU.mult), reads=["r_gm"], writes=["r_gm"])
                            for i in range(NTI):
                                S.op("dve", lambda i=i: V.tensor_tensor(out=r_lem[:, i, :].rearrange("p (g j) -> p g j", g=4),
                                                                        in0=rl[:, i, 4:36].rearrange("p (g j) -> p g j", g=4),
                                                                        in1=r_gm[:, i, :].unsqueeze(2).to_broadcast([128, 4, 8]), op=ALU.add),
                                     reads=rl_res + ["r_gm"], writes=["r_lem"])
                            S.op("dve", lambda: V.tensor_reduce(out=r_m1[:], in_=r_lem[:], axis=AX.X, op=ALU.max), reads=["r_lem"], writes=["r_m1"])
                            S.op("dve", lambda: V.tensor_tensor(out=oh1, in0=r_lem[:], in1=r_m1[:, :].unsqueeze(2).to_broadcast([128, NTI, NE]), op=ALU.is_equal),
                                 reads=["r_lem", "r_m1"], writes=[("OH1", st)])
                            S.op("dve", lambda: V.scalar_tensor_tensor(out=r_lem2[:], in0=oh1, scalar=-BIG, in1=r_lem[:], op0=ALU.mult, op1=ALU.add),
                                 reads=[("OH1", st), "r_lem"], writes=["r_lem2"])
                            S.op("dve", lambda: V.tensor_reduce(out=r_m2[:], in_=r_lem2[:], axis=AX.X, op=ALU.max), reads=["r_lem2"], writes=["r_m2"])
                            S.op("dve", lambda: V.tensor_tensor(out=oh2, in0=r_lem2[:], in1=r_m2[:, :].unsqueeze(2).to_broadcast([128, NTI, NE]), op=ALU.is_equal),
                                 reads=["r_lem2", "r_m2"], writes=[("OH2", st)])
                            S.op("dve", lambda: V.tensor_sub(out=r_m2[:], in0=r_m2[:], in1=r_m1[:]), reads=["r_m2", "r_m1"], writes=["r_m2"])
                            S.op("act", lambda: A.activation(out=r_m2[:], in_=r_m2[:], func=AF.Exp), reads=["r_m2"], writes=["r_m2"])
                            S.op("dve", lambda: V.tensor_scalar_add(out=r_w1[:], in0=r_m2[:], scalar1=1.0), reads=["r_m2"], writes=["r_w1"])
                            S.op("dve", lambda: V.reciprocal(out=r_w1[:], in_=r_w1[:]), reads=["r_w1"], writes=["r_w1"])
                            S.op("dve", lambda: V.tensor_mul(out=W12[:, tsl, 0], in0=r_w1[:], in1=r_sg[:]), reads=["r_w1", "r_sg"], writes=[("W12", st, 0)])
                            S.op("dve", lambda: V.tensor_mul(out=W12[:, tsl, 1], in0=W12[:, tsl, 0], in1=r_m2[:]), reads=[("W12", st, 0), "r_m2"], writes=[("W12", st, 1)])
                        S.barrier()

                with ExitStack() as ph:
                    if "1" in esub:
                        def sb(name, shape, dt):
                            return ph.enter_context(nc.sbuf_tensor(name + '_e1', shape, dt))

                        def ps(name, shape, dt=F32):
                            return ph.enter_context(nc.psum_tensor(name + '_e1', shape, dt))

                        NTHR = NTOK // SLOT
                        Lb = sb("Lb", [128, 128], BF16)
                        onesb = sb("onesb", [128, 128], BF16)
                        mask96 = sb("mask96", [128, NE * NTILE], F32)
                        tokid = sb("tokid", [128, NTILE], F32)
                        slotst = sb("slotst", [128, NSLOT], F32)
                        thr = sb("thr", [128, NTHR], F32)
                        addc = sb("addc", [128, 6], F32)
                        mulc = sb("mulc", [128, 6], F32)
                        ones32 = sb("ones32", [128, NE], F32)
                        ohE = sb("ohE", [128, NE, NTILE], F32)
                        incl = sb("incl", [128, NE, NTILE], F32)
                        ohb = sb("ohb", [128, NTILE, NE], BF16)
                        ohcb = sb("ohcb", [128, NE, NTILE], BF16)
                        inclb = sb("inclb", [128, NE], BF16)
                        C_all = sb("C_all", [128, NTILE, NE], F32)
                        prod = sb("prodE", [128, NTILE, NE], F32)
                        cnt = sb("cntE", [128, NE], F32)
                        cmpt = sb("cmpt", [128, NE, NTHR], F32)
                        padded = sb("padded", [128, NE], F32)
                        inclp = sb("inclp", [128, NE], F32)
                        base = sb("baseE", [128, NE], F32)
                        POS = sb("POS", [128, NTILE, 2], F32)
                        POSI = sb("POSI", [128, NTILE, 2], I32)
                        REC = sb("REC", [128, NTILE, 2, 4], I32)
                        cmps = sb("cmps", [128, NSLOT, NE], F32)
                        slot_e = sb("slot_e", [128, NSLOT], F32)
                        WIf = sb("WIf", [128, NSLOT, 6], F32)
                        pS = [ps(f"pA{i}", [128, 512], F32) if False else ps(f"pSx{i}", [128, 512], F32) for i in range(2)]
                        pS2 = ps("pSy", [128, 512], F32)
                        for (t_, src, nm) in ((Lb, c_L, "Lb"), (onesb, c_ones, "onesb"), (mask96, c_mask96, "mask96"), (tokid, c_tokid, "tokid"),
                                              (slotst, c_slotstart, "slotst"), (thr, c_thr, "thr"), (addc, c_addc, "addc"), (mulc, c_mulc, "mulc")):
                            S.dma("sp", t_[:], src, writes=[nm])
                        S.op("dve", lambda: V.memset(ones32[:], 1.0), writes=["ones32"])
                        S.op("dve", lambda: V.tensor_tensor(out=ohE[:].rearrange("p e t -> p t e"), in0=OH1[:], in1=OH2[:], op=ALU.add), writes=["ohE"])
                        S.op("dve", lambda: V.tensor_tensor(out=ohb[:], in0=OH1[:], in1=OH2[:], op=ALU.add), writes=["ohb"])
                        S.op("dve", lambda: V.tensor_tensor_scan(out=incl[:].rearrange("p e t -> p (e t)"), data0=mask96[:], data1=ohE[:].rearrange("p e t -> p (e t)"),
                                                                 initial=0.0, op0=ALU.mult, op1=ALU.add), reads=["ohE", "mask96"], writes=["incl"])
                        S.op("dve", lambda: V.tensor_tensor(out=ohcb[:], in0=incl[:], in1=ohE[:], op=ALU.subtract), reads=["incl", "ohE"], writes=["ohcb"])
                        S.op("dve", lambda: V.tensor_copy(out=inclb[:], in_=incl[:, :, NTILE - 1]), reads=["incl"], writes=["inclb"])
                        for i in range(NTILE):
                            pb = (i // 16) % 2
                            sl = (i % 16) * NE
                            S.op("pe", lambda i=i, pb=pb, sl=sl: T.matmul(pS[pb][:, sl:sl + NE], Lb[:], ohb[:, i, :], start=True, stop=False),
                                 reads=["Lb", "ohb"], writes=[("pSx", pb)])
                            S.op("pe", lambda i=i, pb=pb, sl=sl: T.matmul(pS[pb][:, sl:sl + NE], onesb[:], ohcb[:, :, i], start=False, stop=True),
                                 reads=["onesb", "ohcb"], writes=[("pSx", pb)])
                            if i % 16 == 15 or i == NTILE - 1:
                                i0 = (i // 16) * 16
                                n_ = i - i0 + 1
                                S.op("dve", lambda i0=i0, n_=n_, pb=pb: V.tensor_copy(out=C_all[:, i0:i0 + n_, :], in_=pS[pb][:, 0:n_ * NE].rearrange("p (t e) -> p t e", e=NE)),
                                     reads=[("pSx", pb)], writes=["C_all"])
                        S.op("pe", lambda: T.matmul(pS2[:, 0:NE], onesb[:], inclb[:], start=True, stop=True), reads=["onesb", "inclb"], writes=["pSy"])
                        S.op("dve", lambda: V.tensor_copy(out=cnt[:], in_=pS2[:, 0:NE]), reads=["pSy"], writes=["cntE"])
                        S.op("dve", lambda: V.tensor_tensor(out=cmpt[:], in0=cnt[:, :].unsqueeze(2).to_broadcast([128, NE, NTHR]),
                                                            in1=thr[:, :].unsqueeze(1).to_broadcast([128, NE, NTHR]), op=ALU.is_gt),
                             reads=["cntE", "thr"], writes=["cmpt"])
                        S.op("dve", lambda: V.tensor_reduce(out=padded[:], in_=cmpt[:], axis=AX.X, op=ALU.add), reads=["cmpt"], writes=["padded"])
                        S.op("dve", lambda: V.tensor_scalar_mul(out=padded[:], in0=padded[:], scalar1=float(SLOT)), reads=["padded"], writes=["padded"])
                        S.op("dve", lambda: V.tensor_tensor_scan(out=inclp[:], data0=ones32[:], data1=padded[:], initial=0.0, op0=ALU.mult, op1=ALU.add),
                             reads=["ones32", "padded"], writes=["inclp"])
                        S.op("dve", lambda: V.tensor_sub(out=base[:], in0=inclp[:], in1=padded[:]), reads=["inclp", "padded"], writes=["baseE"])
                        S.op("dve", lambda: V.tensor_tensor(out=C_all[:], in0=C_all[:], in1=base[:, :].unsqueeze(1).to_broadcast([128, NTILE, NE]), op=ALU.add),
                             reads=["C_all", "baseE"], writes=["C_all"])
                        for r, OH in enumerate((OH1, OH2)):
                            S.op("dve", lambda OH=OH: V.tensor_tensor(out=prod[:], in0=C_all[:], in1=OH[:], op=ALU.mult), reads=["C_all"], writes=["prodE"])
                            S.op("dve", lambda r=r: V.tensor_reduce(out=POS[:, :, r], in_=prod[:], axis=AX.X, op=ALU.add), reads=["prodE"], writes=["POS"])
                        S.op("dve", lambda: V.tensor_copy(out=POSI[:], in_=POS[:]), reads=["POS"], writes=["POSI"])
                        S.op("dve", lambda: V.memset(REC[:], 0), writes=["REC"])
                        for r in range(2):
                            S.op("dve", lambda r=r: V.tensor_copy(out=REC[:, :, r, 0], in_=tokid[:]), reads=["tokid", "REC"], writes=["REC"])
                            S.op("dve", lambda r=r: V.tensor_scalar_add(out=POS[:, :, r], in0=tokid[:], scalar1=float(r * NTOK)), reads=["tokid", "POSI"], writes=["POS"])
                            S.op("dve", lambda r=r: V.tensor_copy(out=REC[:, :, r, 1], in_=POS[:, :, r]), reads=["POS", "REC"], writes=["REC"])
                            S.op("dve", lambda r=r: V.tensor_copy(out=REC[:, :, r, 2].bitcast(F32), in_=W12[:, :, r]), reads=["REC"], writes=["REC"])
                        ev_sc = []
                        for i in range(NTILE):
                            for r in range(2):
                                S.idma("pool", lambda: P.indirect_dma_start(out=rec_s, out_offset=bass.IndirectOffsetOnAxis(ap=POSI[:, i, r:r + 1], axis=0), in_=REC[:, i, r, :], in_offset=None),
                                       reads=["REC", "POSI", "rec_s"], writes=[("rec_sc", i, r)])
                        S.op("dve", lambda: V.tensor_tensor(out=cmps[:], in0=inclp[:, :].unsqueeze(1).to_broadcast([128, NSLOT, NE]),
                                                            in1=slotst[:, :].unsqueeze(2).to_broadcast([128, NSLOT, NE]), op=ALU.is_le),
                             reads=["inclp", "slotst"], writes=["cmps"])
                        S.op("dve", lambda: V.tensor_reduce(out=slot_e[:], in_=cmps[:], axis=AX.X, op=ALU.add), reads=["cmps"], writes=["slot_e"])
                        S.op("dve", lambda: V.tensor_scalar_min(out=slot_e[:], in0=slot_e[:], scalar1=float(NE - 1)), reads=["slot_e"], writes=["slot_e"])
                        S.op("dve", lambda: V.tensor_tensor(out=WIf[:], in0=slot_e[:, :].unsqueeze(2).to_broadcast([128, NSLOT, 6]),
                                                            in1=mulc[:, :].unsqueeze(1).to_broadcast([128, NSLOT, 6]), op=ALU.mult),
                             reads=["slot_e", "mulc"], writes=["WIf"])
                        S.op("dve", lambda: V.tensor_tensor(out=WIf[:], in0=WIf[:], in1=addc[:, :].unsqueeze(1).to_broadcast([128, NSLOT, 6]), op=ALU.add),
                             reads=["WIf", "addc"], writes=["WIf"])
                        S.op("dve", lambda: V.tensor_copy(out=WI[:], in_=WIf[:]), reads=["WIf"], writes=["WI"])
                        S.barrier()

                with ExitStack() as ph:
                    if "2" in esub:
                        def sb(name, shape, dt):
                            return ph.enter_context(nc.sbuf_tensor(name + '_e2', shape, dt))

                        def ps(name, shape, dt=F32):
                            return ph.enter_context(nc.psum_tensor(name + '_e2', shape, dt))

                        NB2 = 2
                        recb = [sb(f"recb{i}", [128, 4, 4], I32) for i in range(NB2)]
                        hng = [[sb(f"hng{i}_{j}", [128, D], BF16) for j in range(4)] for i in range(NB2)]
                        hnTs = [sb(f"hnTs{i}", [128, 8, 512], BF16) for i in range(NB2)]
                        wgb = [sb(f"wgb{i}", [128, 8, DE], BF16) for i in range(NB2)]
                        wub = [sb(f"wub{i}", [128, 8, DE], BF16) for i in range(NB2)]
                        wdb = [sb(f"wdb{i}", [128, 4, D], BF16) for i in range(NB2)]
                        hid = [sb(f"hidD{i}", [128, 4, 512], BF16) for i in range(2)]
                        sgt = [sb(f"sgD{i}", [128, 512], F32) for i in range(2)]
                        yrow = [sb(f"yrow{i}", [128, D], BF16) for i in range(4)]
                        pA = ps("pA", [128, 1024], F32)
                        pB = ps("pB", [128, 1024], F32)
                        pC = ps("pC", [128, 1024], F32)
                        pD_ = ps("pD", [128, 1024], F32)
                        pCb = pC[:, :].bitcast(BF16)
                        pDb = pD_[:, :].bitcast(BF16)
                        wg_v = w_eg.rearrange("e (p a k) n -> (e p a) (k n)", p=128, a=2)
                        wu_v = w_eu.rearrange("e (p a k) n -> (e p a) (k n)", p=128, a=2)
                        wd_v = w_ed.rearrange("e f n -> (e f) n")
                        rec_scatter_res = [("rec_sc", i, r) for i in range(NTILE) for r in range(2)]

                        def load_slot(s):
                            b = s % NB2
                            S.dma("sp", recb[b][:], rec_s[s * SLOT:(s + 1) * SLOT, :].rearrange("(j p) c -> p j c", p=128), writes=[("recb", b)])
                            for j in range(4):
                                S.idma("pool", lambda: P.indirect_dma_start(out=hng[b][j][:], out_offset=None, in_=hn_s, in_offset=bass.IndirectOffsetOnAxis(ap=recb[b][:, j, 0:1], axis=0)),
                                       reads=[("recb", b)], writes=[("hng", b, j)])
                            for (wt_, src_, nm_) in ((wgb, wbg_s, "wgb"), (wub, wbu_s, "wub"), (wdb, wbd_s, "wdb")):
                                S.idma("pool", lambda wt_=wt_, src_=src_: P.indirect_dma_start(out=wt_[b][:, :, :].rearrange("p k n -> p (k n)"), out_offset=None, in_=src_,
                                                                                              in_offset=bass.IndirectOffsetOnAxis(ap=WI[:, s, 0:1], axis=0)),
                                       reads=["WI"], writes=[(nm_, b)])

                        load_slot(0)
                        yi = 0
                        for s in range(NSLOT):
                            b = s % NB2
                            if s + 1 < NSLOT:
                                load_slot(s + 1)
                            for j in range(4):
                                pbk = pCb[:, (j % 2) * 1024:(j % 2 + 1) * 1024]
                                for k in range(8):
                                    S.op("pe", lambda k=k, j=j, pbk=pbk: T.transpose(pbk[:, k * 128:(k + 1) * 128],
                                                                                    hng[b][j][:, :].rearrange("p (q k) -> p k q", k=8)[:, k, :], ident[:]),
                                         reads=[("hng", b, j), "ident"], writes=[("pC", j % 2)])
                                if j % 2 == 0:
                                    S.op("act", lambda j=j, pbk=pbk: A.copy(out=hnTs[b][:, :, j * 128:(j + 1) * 128], in_=pbk.rearrange("p (k t) -> p k t", k=8)),
                                         reads=[("pC", j % 2)], writes=[("hnTs", b, j)])
                                else:
                                    S.op("dve", lambda j=j, pbk=pbk: V.tensor_copy(out=hnTs[b][:, :, j * 128:(j + 1) * 128], in_=pbk.rearrange("p (k t) -> p k t", k=8)),
                                         reads=[("pC", j % 2)], writes=[("hnTs", b, j)])
                            hnTs_res = [("hnTs", b, j) for j in range(4)]
                            hb = s % 2
                            for c in range(4):
                                pGU = pA if c % 2 == 0 else pB
                                pGUr = "pA" if c % 2 == 0 else "pB"
                                for k in range(8):
                                    S.op("pe", lambda k=k, c=c: T.matmul(pGU[:, 0:512], wgb[b][:, k, c * 128:(c + 1) * 128], hnTs[b][:, k, :], start=(k == 0), stop=(k == 7)),
                                         reads=[("wgb", b)] + hnTs_res, writes=[(pGUr, 0)])
                                for k in range(8):
                                    S.op("pe", lambda k=k, c=c: T.matmul(pGU[:, 512:1024], wub[b][:, k, c * 128:(c + 1) * 128], hnTs[b][:, k, :], start=(k == 0), stop=(k == 7)),
                                         reads=[("wub", b)] + hnTs_res, writes=[(pGUr, 1)])
                                S.op("act", lambda c=c: A.activation(out=sgt[c % 2][:], in_=pGU[:, 0:512], func=AF.Silu), reads=[(pGUr, 0)], writes=[("sgD", c % 2)])
                                S.op("dve", lambda c=c: V.tensor_tensor(out=hid[hb][:, c, :], in0=pGU[:, 512:1024], in1=sgt[c % 2][:], op=ALU.mult),
                                     reads=[(pGUr, 1), ("sgD", c % 2)], writes=[("hid", hb, c)])
                            for j in range(4):
                                pDn = pC if j % 2 == 0 else pD_
                                pDr = "pC" if j % 2 == 0 else "pD"
                                for half in range(2):
                                    for c in range(4):
                                        S.op("pe", lambda c=c, half=half, j=j: T.matmul(pDn[:, half * 512:(half + 1) * 512], hid[hb][:, c, j * 128:(j + 1) * 128],
                                                                                     wdb[b][:, c, half * 512:(half + 1) * 512], start=(c == 0), stop=(c == 3)),
                                             reads=[("hid", hb, c), ("wdb", b)], writes=[(pDr, half)])
                                yb = yi % 4
                                yi += 1
                                if j % 2 == 0:
                                    S.op("act", lambda j=j, yb=yb: A.activation(out=yrow[yb][:], in_=pDn[:, :], func=AF.Copy, scale=recb[b][:, j, 2:3].bitcast(F32)),
                                         reads=[(pDr, 0), (pDr, 1), ("recb", b)], writes=[("yrow", yb)])
                                else:
                                    S.op("dve", lambda j=j, yb=yb: V.tensor_scalar(out=yrow[yb][:], in0=pDn[:, :], scalar1=recb[b][:, j, 2:3].bitcast(F32), scalar2=None, op0=ALU.mult),
                                         reads=[(pDr, 0), (pDr, 1), ("recb", b)], writes=[("yrow", yb)])
                                S.idma("pool", lambda: P.indirect_dma_start(out=ybuf, out_offset=bass.IndirectOffsetOnAxis(ap=recb[b][:, j, 1:2], axis=0), in_=yrow[yb][:], in_offset=None),
                                       reads=[("yrow", yb), ("recb", b)], writes=[("ybuf", s, j)])
                        S.barrier()

                with ExitStack() as ph:
                    if "3" in esub:
                        def sb(name, shape, dt):
                            return ph.enter_context(nc.sbuf_tensor(name + '_e3', shape, dt))

                        def ps(name, shape, dt=F32):
                            return ph.enter_context(nc.psum_tensor(name + '_e3', shape, dt))

                        wpgb = sb("wpgb", [128, 8, D], BF16)
                        wppb = sb("wppb", [128, 2, D], BF16)
                        x1 = sb("x1", [128, 2 * NTI, D], F32)
                        ya = [sb(f"yaE{i}", [128, 2, D], BF16) for i in range(4)]
                        xsb = [sb(f"xsD{i}", [128, D], BF16) for i in range(2)]
                        junk = sb("junkD", [128, D], BF16)
                        ssq = sb("ssqD", [128, 2 * NTI], F32)
                        rstd = sb("rstdD", [128, 2 * NTI], F32)
                        ssq3 = sb("ssq3D", [128, NTI], F32)
                        rstd3 = sb("rstd3D", [128, NTI], F32)
                        pld = sb("pld", [128, 2 * NTI, PD], F32)
                        pbf = [sb(f"pbf{i}", [128, PD], BF16) for i in range(2)]
                        ptT = [sb(f"ptT{i}", [128, 2, 128], BF16) for i in range(2)]
                        hpT = [sb(f"hpT{i}", [128, 8, 128], BF16) for i in range(2)]
                        sig = [sb(f"sigD{i}", [128, D], F32) for i in range(2)]
                        pA = ps("pA", [128, 1024], F32)
                        pB = ps("pB", [128, 1024], F32)
                        pC = ps("pC", [128, 1024], F32)
                        pD_ = ps("pD", [128, 1024], F32)
                        pCb = pC[:, :].bitcast(BF16)
                        pDb = pD_[:, :].bitcast(BF16)
                        S.dma("pool", wpgb[:], w_pg.rearrange("(k p) n -> p k n", p=128), writes=["wpgb"])
                        S.dma("pool", wppb[:], w_pp.rearrange("(k p) n -> p k n", p=128), writes=["wppb"])

                        def sumsq(xi, col_t, ci, res):
                            S.op("act", lambda: A.activation(out=junk[:], in_=x1[:, xi, :], func=AF.Square, accum_out=col_t[:, ci:ci + 1]),
                                 reads=[("x1", xi)], writes=["junkD", res])

                        def norm_T(xin_ap, xin_res, rstd_col, rstd_res, gain, gain_res, out_ap, out_res, pbank, pbank_res, xsi):
                            S.op("act", lambda: A.activation(out=xsb[xsi][:], in_=xin_ap, func=AF.Copy, scale=rstd_col),
                                 reads=xin_res + [rstd_res], writes=[("xsD", xsi)])
                            for k in range(8):
                                S.op("pe", lambda k=k: T.transpose(pbank[:, k * 128:(k + 1) * 128], xsb[xsi][:, k * 128:(k + 1) * 128], ident[:]),
                                     reads=[("xsD", xsi), "ident"], writes=[pbank_res])
                            S.op("dve", lambda: V.tensor_tensor(out=out_ap, in0=pbank.rearrange("p (k t) -> p k t", k=8),
                                                                in1=gain[:, :].unsqueeze(2).to_broadcast([128, 8, 128]), op=ALU.mult),
                                 reads=[pbank_res, gain_res], writes=[out_res])

                        yb_v = ybuf[0:2 * NTOK, :].rearrange("(r t) d -> t r d", r=2)

                        def e3_loads(st_):
                            T0_ = st_ * TS
                            xo_ = (st_ % 2) * NTI
                            S.dma("pool", pld[:, xo_:xo_ + NTI, :], p_in[T0_:T0_ + TS, :].rearrange("(j p) n -> p j n", p=128), writes=[("pld", st_ % 2)])
                            for i_ in range(NTI):
                                S.dma("pool", x1[:, xo_ + i_, :], x1_s[T0_ + i_ * 128:T0_ + (i_ + 1) * 128, :], writes=[("x1", xo_ + i_)])

                        def ya_load(g_):
                            S.dma("pool", ya[g_ % 4][:], yb_v[g_ * 128:(g_ + 1) * 128, :, :], writes=[("yaE", g_ % 4)])

                        def e3_combine(st, i):
                            xo = (st % 2) * NTI
                            yq = (st * NTI + i) % 4
                            S.op("dve", lambda: V.tensor_tensor(out=x1[:, xo + i, :], in0=x1[:, xo + i, :], in1=ya[yq][:, 0, :], op=ALU.add),
                                 reads=[("x1", xo + i), ("yaE", yq)], writes=[("x1", xo + i)])
                            S.op("dve", lambda: V.tensor_tensor(out=x1[:, xo + i, :], in0=x1[:, xo + i, :], in1=ya[yq][:, 1, :], op=ALU.add),
                                 reads=[("x1", xo + i), ("yaE", yq)], writes=[("x1", xo + i)])
                            if st * NTI + i + 4 < NTOK // 128:
                                ya_load(st * NTI + i + 4)
                            sumsq(xo + i, ssq, xo + i, ("ssqD", st % 2))

                        def e3_rstd1(st):
                            xo = (st % 2) * NTI
                            rstd_from_ssq(ssq[:, xo:xo + NTI], rstd[:, xo:xo + NTI], D, [("ssqD", st % 2)], [("rstdD", st % 2)])

                        def e3_front(st, i):
                            b2 = i % 2
                            xo = (st % 2) * NTI
                            ro = (st % 2) * NTI
                            pbk = pCb[:, b2 * 1024:(b2 + 1) * 1024]
                            norm_T(x1[:, xo + i, :], [("x1", xo + i)], rstd[:, ro + i:ro + i + 1], ("rstdD", st % 2), gple, "gple",
                                   hpT[b2][:], ("hpT", b2), pbk, ("pC", b2), b2)
                            S.op("act", lambda i=i: A.copy(out=pbf[b2][:], in_=pld[:, xo + i, :]), reads=[("pld", st % 2)], writes=[("pbf", b2)])
                            for k in range(2):
                                S.op("pe", lambda k=k: T.transpose(pDb[:, b2 * 1024 + k * 128: b2 * 1024 + (k + 1) * 128], pbf[b2][:, k * 128:(k + 1) * 128], ident[:]),
                                     reads=[("pbf", b2), "ident"], writes=[("pD", b2)])
                            S.op("act", lambda: A.copy(out=ptT[b2][:], in_=pDb[:, b2 * 1024: b2 * 1024 + 256].rearrange("p (k t) -> p k t", k=2)),
                                 reads=[("pD", b2)], writes=[("ptT", b2)])

                        def e3_back(st, i):
                            b2 = i % 2
                            xo = (st % 2) * NTI
                            ro = (st % 2) * NTI
                            for half in range(2):
                                for k in range(8):
                                    S.op("pe", lambda k=k, half=half: T.matmul(pA[:, half * 512:(half + 1) * 512], hpT[b2][:, k, :], wpgb[:, k, half * 512:(half + 1) * 512],
                                                                              start=(k == 0), stop=(k == 7)),
                                         reads=[("hpT", b2), "wpgb"], writes=[("pA", half)])
                            for half in range(2):
                                for k in range(2):
                                    S.op("pe", lambda k=k, half=half: T.matmul(pB[:, half * 512:(half + 1) * 512], ptT[b2][:, k, :], wppb[:, k, half * 512:(half + 1) * 512],
                                                                              start=(k == 0), stop=(k == 1)),
                                         reads=[("ptT", b2), "wppb"], writes=[("pB", half)])
                            for half in range(2):
                                hs = slice(half * 512, (half + 1) * 512)
                                S.op("act", lambda hs=hs: A.activation(out=sig[b2][:, hs], in_=pA[:, hs], func=AF.Sigmoid), reads=[("pA", half)], writes=[("sigD", b2, half)])
                                S.op("dve", lambda hs=hs: V.tensor_tensor(out=sig[b2][:, hs], in0=pB[:, hs], in1=sig[b2][:, hs], op=ALU.mult),
                                     reads=[("pB", half), ("sigD", b2, half)], writes=[("sigD", b2, half)])
                            S.op("dve", lambda i=i: V.tensor_tensor(out=x1[:, xo + i, :], in0=sig[b2][:], in1=x1[:, xo + i, :], op=ALU.add),
                                 reads=[("sigD", b2, 0), ("sigD", b2, 1), ("x1", xo + i)], writes=[("x1", xo + i)])
                            sumsq(xo + i, ssq3, i, "ssq3D")


                        e3_loads(0)
                        for g_ in range(4):
                            ya_load(g_)
                        for i in range(NTI):
                            e3_combine(0, i)
                        e3_rstd1(0)
                        for st in range(NST):
                            T0 = st * TS
                            xo = (st % 2) * NTI
                            if st + 1 < NST:
                                e3_loads(st + 1)
                            e3_front(st, 0)
                            for i in range(NTI):
                                if i + 1 < NTI:
                                    e3_front(st, i + 1)
                                e3_back(st, i)
                                if st + 1 < NST:
                                    e3_combine(st + 1, i)
                            if st + 1 < NST:
                                e3_rstd1(st + 1)
                            rstd_from_ssq(ssq3[:], rstd3[:], D, ["ssq3D"], ["rstd3D"])
                            for i in range(NTI):
                                S.op("dve", lambda i=i: V.scalar_tensor_tensor(out=x1[:, xo + i, :], in0=x1[:, xo + i, :], scalar=rstd3[:, i:i + 1], in1=gfin[:], op0=ALU.mult, op1=ALU.mult),
                                     reads=[("x1", xo + i), "rstd3D", "gfin"], writes=[("x1", xo + i)])
                                S.dma("sp", y[T0 + i * 128:T0 + (i + 1) * 128, :], x1[:, xo + i, :], reads=[("x1", xo + i)], writes=[("y", st, i)])
                        S.barrier()

        S.finish("sp")
        build.stats = dict(cnt=dict(S.cnt), waits=S.n_wait)
    return nc


_CONST = {}


def _consts():
    if _CONST:
        return _CONST
    bf = ml_dtypes.bfloat16
    idx = np.arange(128)
    same = (idx[:, None] // 64) == (idx[None, :] // 64)
    _CONST["c_ident"] = np.eye(128, dtype=np.float32).astype(bf)
    _CONST["c_maskf"] = (same & (idx[:, None] <= idx[None, :])).astype(np.float32).astype(bf)
    _CONST["c_maskb"] = (same & (idx[:, None] >= idx[None, :])).astype(np.float32).astype(bf)
    ang = 2.0 * np.pi * ((idx[:, None] * idx[None, :]) % 128) / 128.0
    _CONST["c_cc"] = np.concatenate([np.cos(ang), np.sin(ang)], axis=1).astype(np.float32).astype(bf)
    n = np.arange(NTAB, dtype=np.int64)
    m = (n[:, None] * n[None, :]) % NTAB
    tab = np.cos(2.0 * np.pi * np.arange(NTAB) / NTAB)
    tabs = -np.sin(2.0 * np.pi * np.arange(NTAB) / NTAB)
    _CONST["c_ctab"] = tab[m].astype(np.float32).astype(bf)
    _CONST["c_stab"] = tabs[m].astype(np.float32).astype(bf)
    return _CONST


def _consts_n(NTOK):
    bf = ml_dtypes.bfloat16
    NSLOT = (2 * NTOK) // SLOT + NE
    NPOS = NSLOT * SLOT
    NTILE = NTOK // 128
    c = {}
    pos = np.arange(NPOS)
    rp = np.zeros((NPOS, 4), np.int32)
    rp[:, 1] = 2 * NTOK + (pos % SLOT)
    c["c_recpad"] = rp
    idx = np.arange(128)
    c["c_L"] = (idx[:, None] < idx[None, :]).astype(np.float32).astype(bf)
    c["c_ones"] = np.ones((128, 128), np.float32).astype(bf)
    m = np.ones((NE, NTILE), np.float32)
    m[:, 0] = 0.0
    c["c_mask96"] = np.ascontiguousarray(np.broadcast_to(m.reshape(1, -1), (128, NE * NTILE)))
    c["c_tokid"] = (np.arange(NTILE)[None, :] * 128 + idx[:, None]).astype(np.float32)
    c["c_slotstart"] = np.ascontiguousarray(np.broadcast_to((np.arange(NSLOT) * SLOT).astype(np.float32)[None, :], (128, NSLOT)))
    c["c_thr"] = np.ascontiguousarray(np.broadcast_to((np.arange(NTOK // SLOT) * SLOT).astype(np.float32)[None, :], (128, NTOK // SLOT)))
    addc = np.stack([idx, idx, idx, idx, idx, idx], axis=1).astype(np.float32)
    c["c_addc"] = addc
    c["c_mulc"] = np.ascontiguousarray(np.broadcast_to(np.array([128, 128, 128, 128, 128, 128], np.float32)[None, :], (128, 6)))
    return c


_WNAMES = ["norm_mix", "w_in", "w_fourier", "lb_logits", "norm_o", "w_out", "norm_ffn", "w_route_group", "w_route_expert",
           "w_exp_gate", "w_exp_up", "w_exp_down", "norm_ple", "w_ple_gate", "w_ple_proj", "norm_final"]


def _prep_weights(inp):
    f = lambda a: np.ascontiguousarray(np.asarray(a, dtype=np.float32))
    return {
        "norm_mix": f(inp["norm_mix"]).reshape(D),
        "w_in": f(inp["w_in"]).reshape(D, INW),
        "w_fourier": f(inp["w_fourier"]).reshape(4, 128, 128),
        "lb_logits": f(inp["lb_logits"]).reshape(2, 1024),
        "norm_o": f(inp["norm_o"]).reshape(HD),
        "w_out": f(inp["w_out"]).reshape(D, D),
        "norm_ffn": f(inp["norm_ffn"]).reshape(D),
        "w_route_group": f(inp["w_route_group"]).reshape(D, 4),
        "w_route_expert": f(inp["w_route_expert"]).reshape(D, NE),
        "w_exp_gate": f(inp["w_exp_gate"]).reshape(NE, D, DE),
        "w_exp_up": f(inp["w_exp_up"]).reshape(NE, D, DE),
        "w_exp_down": f(inp["w_exp_down"]).reshape(NE, DE, D),
        "norm_ple": f(inp["norm_ple"]).reshape(D),
        "w_ple_gate": f(inp["w_ple_gate"]).reshape(D, D),
        "w_ple_proj": f(inp["w_ple_proj"]).reshape(PD, D),
        "norm_final": f(inp["norm_final"]).reshape(D),
    }


_NC_CACHE = {}


def kernel(**inputs):
    ncores = 8
    xp = np.asarray(inputs["x_prompt"], dtype=np.float32)
    xs = np.asarray(inputs["x_sample"], dtype=np.float32)
    pp = np.asarray(inputs["p_prompt"], dtype=np.float32)[0]
    psm = np.asarray(inputs["p_sample"], dtype=np.float32)[0]
    B, SQ, _ = xp.shape
    DB, DS, _ = xs.shape
    npc = B // ncores
    nsc = DB // ncores
    seq_lens = tuple([SQ] * npc + [DS] * nsc)
    if seq_lens not in _NC_CACHE:
        _NC_CACHE[seq_lens] = build(list(seq_lens))
    nc = _NC_CACHE[seq_lens]
    w = _prep_weights(inputs)
    cst = dict(_consts())
    cst.update(_consts_n(sum(seq_lens)))
    in_maps = []
    for c in range(ncores):
        xc = np.concatenate([xp[c * npc:(c + 1) * npc].reshape(-1, D), xs[c * nsc:(c + 1) * nsc].reshape(-1, D)], axis=0)
        pc = np.concatenate([pp[c * npc:(c + 1) * npc].reshape(-1, PD), psm[c * nsc:(c + 1) * nsc].reshape(-1, PD)], axis=0)
        m = {"x": np.ascontiguousarray(xc), "p": np.ascontiguousarray(pc)}
        m.update(w)
        m.update(cst)
        in_maps.append(m)
    res = run_bass_kernel_spmd(nc, in_maps, core_ids=list(range(ncores)))
    yp = np.empty((B, SQ, D), np.float32)
    ys = np.empty((DB, DS, D), np.float32)
    for c in range(ncores):
        yc = np.asarray(res.results[c]["y"], dtype=np.float32)
        yp[c * npc:(c + 1) * npc] = yc[:npc * SQ].reshape(npc, SQ, D)
        ys[c * nsc:(c + 1) * nsc] = yc[npc * SQ:].reshape(nsc, DS, D)
    return (yp, ys)
```
